# Optimizing a Trainium2 kernel written in Bass

```python
import math
import jax
import jax.numpy as jnp
from jax import lax
import numpy as np

D_MODEL = 1024
BATCH = 8
SEQ = 4096
DEPTH = 4

CHUNK = 64
Q_BLOCK = 128

ATTN_WIDTH = D_MODEL // 2
RWKV_WIDTH = D_MODEL - ATTN_WIDTH

ATTN_HEADS = 4
ATTN_HEAD_DIM = ATTN_WIDTH // (2 * ATTN_HEADS)
ROT_DIM = ATTN_HEAD_DIM // 4
ROPE_THETA = 500000.0
SUBLN_EPS = 1e-5

RWKV_HEAD_SIZE = 64
RWKV_HEADS = RWKV_WIDTH // RWKV_HEAD_SIZE
W_LORA = max(32, int(round(1.8 * RWKV_WIDTH ** 0.5 / 32)) * 32)
A_LORA = max(32, int(round(1.8 * RWKV_WIDTH ** 0.5 / 32)) * 32)
G_LORA = max(32, int(round(0.6 * RWKV_WIDTH ** 0.8 / 32)) * 32)
GN_EPS = 64e-5
RWKV_IN = 3 * RWKV_WIDTH + W_LORA + A_LORA + G_LORA
IN_W = 3 * ATTN_WIDTH + RWKV_IN

N_EXPERT_GROUPS = 4
EXPERTS_PER_GROUP = 8
N_EXPERTS = N_EXPERT_GROUPS * EXPERTS_PER_GROUP
TOP_K = 2
D_EXPERT = D_MODEL // 4
ROW_BLOCK = 128

NORM_EPS = 1e-6

kernel_name = "hymba_diffattn_rwkv7_hmoe_adaln"


def rms_norm(x, g, eps=NORM_EPS):
    xf = x.astype(jnp.float32)
    y = xf * lax.rsqrt(jnp.mean(xf * xf, axis=-1, keepdims=True) + eps)
    return (y * g.astype(jnp.float32)).astype(x.dtype)


def lambda_init_of(layer):
    return 0.8 - 0.6 * math.exp(-0.3 * layer)


def partial_rotary(t, cos, sin):
    half = ROT_DIM // 2
    t1 = t[..., :half]
    t2 = t[..., half:ROT_DIM]
    return jnp.concatenate([t1 * cos - t2 * sin, t2 * cos + t1 * sin, t[..., ROT_DIM:]], axis=-1)


def diff_attention(q, k, v, lam_vecs, subln_g, lambda_init):
    bsz, seq = q.shape[0], q.shape[1]
    qh = jnp.transpose(q, (0, 2, 3, 1, 4))
    kh = jnp.transpose(k, (0, 2, 3, 1, 4))
    vh = jnp.transpose(v, (0, 2, 1, 3))
    lv = lam_vecs.astype(jnp.float32)
    lam = jnp.exp(jnp.sum(lv[0] * lv[1])) - jnp.exp(jnp.sum(lv[2] * lv[3])) + lambda_init
    chunk_id = jnp.arange(seq) // CHUNK
    scale = ATTN_HEAD_DIM ** -0.5
    outs = []
    for blk in range(seq // Q_BLOCK):
        lo, hi = blk * Q_BLOCK, (blk + 1) * Q_BLOCK
        s = jnp.einsum('bhmqd,bhmkd->bhmqk', qh[:, :, :, lo:hi], kh[:, :, :, :hi]).astype(jnp.float32) * scale
        visible = chunk_id[None, :hi] <= chunk_id[lo:hi, None]
        p = jax.nn.softmax(jnp.where(visible, s, -jnp.inf), axis=-1)
        weights = p[:, :, 0] - lam * p[:, :, 1]
        outs.append(jnp.einsum('bhqk,bhkv->bhqv', weights.astype(v.dtype), vh[:, :, :hi]))
    o = jnp.concatenate(outs, axis=2)
    o = rms_norm(o, subln_g, eps=SUBLN_EPS) * (1.0 - lambda_init)
    return jnp.transpose(o, (0, 2, 1, 3)).reshape(bsz, seq, ATTN_HEADS * 2 * ATTN_HEAD_DIM)


def wkv7_scan(r, w, k, v, a, b):
    bsz, _, heads, n = r.shape

    def step(state, inp):
        r_t, w_t, k_t, v_t, a_t, b_t = inp
        sa = jnp.einsum('bhvk,bhk->bhv', state, a_t)
        state = (state * w_t[:, :, None, :] + sa[..., None] * b_t[:, :, None, :]
                 + v_t[..., None] * k_t[:, :, None, :])
        return state, jnp.einsum('bhvk,bhk->bhv', state, r_t)

    xs = tuple(jnp.moveaxis(t, 1, 0) for t in (r, w, k, v, a, b))
    s0 = jnp.zeros((bsz, heads, n, n), jnp.float32)
    _, ys = lax.scan(step, s0, xs)
    return jnp.moveaxis(ys, 0, 1)


def head_group_norm(y, g, b):
    mu = jnp.mean(y, axis=-1, keepdims=True)
    var = jnp.mean(jnp.square(y - mu), axis=-1, keepdims=True)
    yn = ((y - mu) * lax.rsqrt(var + GN_EPS)).reshape(y.shape[0], y.shape[1], -1)
    return yn * g.astype(jnp.float32) + b.astype(jnp.float32)


def rwkv7_time_mix(p, mu, w0, w_up, a0, a_up, g_up, k_k, k_a, r_k, lnx_g, lnx_b):
    bsz, seq, _ = p.shape
    f32 = jnp.float32
    C = RWKV_WIDTH
    p_prev = jnp.pad(p, ((0, 0), (1, 0), (0, 0)))[:, :seq]
    p = p + (p_prev - p) * mu
    r, k, v = p[..., :C], p[..., C:2 * C], p[..., 2 * C:3 * C]
    o = 3 * C
    w_lo = p[..., o:o + W_LORA]
    a_lo = p[..., o + W_LORA:o + W_LORA + A_LORA]
    g_lo = p[..., o + W_LORA + A_LORA:]
    w = -jax.nn.softplus(-(w0 + jnp.tanh(w_lo) @ w_up).astype(f32)) - 0.5
    decay = jnp.exp(-jnp.exp(w))
    a = jax.nn.sigmoid((a0 + a_lo @ a_up).astype(f32))
    g = (jax.nn.sigmoid(g_lo) @ g_up).astype(f32)
    hs = (bsz, seq, RWKV_HEADS, RWKV_HEAD_SIZE)
    kk = (k * k_k).astype(f32).reshape(hs)
    kk = kk / jnp.maximum(jnp.sqrt(jnp.sum(kk * kk, axis=-1, keepdims=True)), 1e-12)
    kf = k.astype(f32) * (1.0 + (a - 1.0) * k_a.astype(f32))
    rh, kh, vh, ah = rf = r.astype(f32).reshape(hs), kf.reshape(hs), v.astype(f32).reshape(hs), a.reshape(hs)
    y = wkv7_scan(rh, decay.reshape(hs), kh, vh, -kk, kk * ah)
    y = head_group_norm(y, lnx_g, lnx_b)
    bonus = jnp.sum(rh * kh * r_k.astype(f32), axis=-1, keepdims=True) * vh
    out = (y + bonus.reshape(bsz, seq, C)) * g
    return out.astype(p.dtype)


def routed_experts(ht, gate_w, expert_id, w_gate, w_up, w_down):
    T, D = ht.shape
    M = T * TOP_K
    flat_e = expert_id.reshape(M)
    flat_tok = jnp.repeat(jnp.arange(T, dtype=jnp.int32), TOP_K)
    flat_w = gate_w.reshape(M)
    order = jnp.argsort(flat_e)
    se = flat_e[order]
    counts = jnp.bincount(flat_e, length=N_EXPERTS)
    padded = (counts + ROW_BLOCK - 1) // ROW_BLOCK * ROW_BLOCK
    pad_end = jnp.cumsum(padded)
    pad_start = pad_end - padded
    start = jnp.cumsum(counts) - counts
    dest = pad_start[se] + (jnp.arange(M) - start[se])
    R = -(-M // ROW_BLOCK) * ROW_BLOCK + N_EXPERTS * ROW_BLOCK
    n_blocks = R // ROW_BLOCK
    row_tok = jnp.full((R,), T, dtype=jnp.int32).at[dest].set(flat_tok[order])
    row_w = jnp.zeros((R,), ht.dtype).at[dest].set(flat_w[order])
    block_e = jnp.minimum(jnp.searchsorted(pad_end, jnp.arange(n_blocks) * ROW_BLOCK, side='right'),
                          N_EXPERTS - 1)
    h_pad = jnp.concatenate([ht, jnp.zeros((1, D), ht.dtype)], axis=0)

    def block_mlp(args):
        tok, e = args
        xb = h_pad[tok]
        hid = jax.nn.silu(xb @ w_gate[e]) * (xb @ w_up[e])
        return hid @ w_down[e]

    yb = lax.map(block_mlp, (row_tok.reshape(n_blocks, ROW_BLOCK), block_e))
    y = jax.ops.segment_sum(yb.reshape(R, D) * row_w[:, None], row_tok, num_segments=T + 1)
    return y[:T]


def hier_moe(h, w_group, b_group, w_router, b_router, w_gate, w_up, w_down):
    bsz, seq, D = h.shape
    T = bsz * seq
    ht = h.reshape(T, D)
    g_prob = jax.nn.softmax((ht @ w_group + b_group).astype(jnp.float32), axis=-1)
    g_top, g_idx = lax.top_k(g_prob, 1)
    e_logits = (ht @ w_router + b_router).astype(jnp.float32).reshape(T, N_EXPERT_GROUPS, EXPERTS_PER_GROUP)
    in_group = e_logits[jnp.arange(T), g_idx[:, 0]]
    top_v, top_i = lax.top_k(in_group, TOP_K)
    gate_w = jax.nn.softmax(top_v, axis=-1) * g_top
    expert_id = g_idx * EXPERTS_PER_GROUP + top_i
    y = routed_experts(ht, gate_w.astype(h.dtype), expert_id, w_gate, w_up, w_down)
    return y.reshape(bsz, seq, D)


def setup_inputs(seed: int = 0) -> dict:
    key = jax.random.key(seed)
    ks = list(jax.random.split(key, 40))
    L, D = DEPTH, D_MODEL

    def nrm(i, shape, std):
        return std * jax.random.normal(ks[i], shape, jnp.float32)

    def uni(i, shape, lo, hi):
        return jax.random.uniform(ks[i], shape, jnp.float32, lo, hi)

    positions = (jax.random.randint(ks[2], (BATCH, 1), 0, 8192, dtype=jnp.int32)
                 + jnp.arange(SEQ, dtype=jnp.int32)[None, :])
    return {
        "x": nrm(0, (BATCH, SEQ, D), 1.0),
        "c": nrm(1, (BATCH, D), 1.0),
        "positions": positions,
        "ada_w": nrm(3, (L, D, 6 * D), 0.5 * D ** -0.5),
        "ada_b": nrm(4, (L, 6 * D), 0.02),
        "norm1_g": 1.0 + nrm(5, (L, D), 0.05),
        "norm2_g": 1.0 + nrm(6, (L, D), 0.05),
        "w_in": nrm(7, (L, D, IN_W), D ** -0.5),
        "w_out": nrm(8, (L, D, D), D ** -0.5),
        "attn_lambda": nrm(9, (L, 4, ATTN_HEAD_DIM), 0.1),
        "attn_subln_g": 1.0 + nrm(10, (L, 2 * ATTN_HEAD_DIM), 0.05),
        "rwkv_shift_mu": uni(11, (L, RWKV_IN), 0.0, 1.0),
        "rwkv_w0": uni(12, (L, RWKV_WIDTH), -6.0, -1.0),
        "rwkv_w_up": nrm(13, (L, W_LORA, RWKV_WIDTH), 0.5 * W_LORA ** -0.5),
        "rwkv_a0": nrm(14, (L, RWKV_WIDTH), 0.1),
        "rwkv_a_up": nrm(15, (L, A_LORA, RWKV_WIDTH), A_LORA ** -0.5),
        "rwkv_g_up": nrm(16, (L, G_LORA, RWKV_WIDTH), G_LORA ** -0.5),
        "rwkv_k_k": 0.85 + nrm(17, (L, RWKV_WIDTH), 0.05),
        "rwkv_k_a": 1.0 + nrm(18, (L, RWKV_WIDTH), 0.05),
        "rwkv_r_k": nrm(19, (L, RWKV_HEADS, RWKV_HEAD_SIZE), 0.1),
        "rwkv_lnx_g": 1.0 + nrm(20, (L, RWKV_WIDTH), 0.05),
        "rwkv_lnx_b": nrm(21, (L, RWKV_WIDTH), 0.02),
        "moe_w_group": nrm(22, (L, D, N_EXPERT_GROUPS), D ** -0.5),
        "moe_b_group": nrm(23, (L, N_EXPERT_GROUPS), 0.01),
        "moe_w_router": nrm(24, (L, D, N_EXPERTS), D ** -0.5),
        "moe_b_router": nrm(25, (L, N_EXPERTS), 0.01),
        "moe_w_gate": nrm(26, (L, N_EXPERTS, D, D_EXPERT), D ** -0.5),
        "moe_w_up": nrm(27, (L, N_EXPERTS, D, D_EXPERT), D ** -0.5),
        "moe_w_down": nrm(28, (L, N_EXPERTS, D_EXPERT, D), D_EXPERT ** -0.5),
        "final_g": 1.0 + nrm(29, (D,), 0.05),
    }


def reference(x, c, positions, ada_w, ada_b, norm1_g, norm2_g, w_in, w_out, attn_lambda,
              attn_subln_g, rwkv_shift_mu, rwkv_w0, rwkv_w_up, rwkv_a0, rwkv_a_up, rwkv_g_up,
              rwkv_k_k, rwkv_k_a, rwkv_r_k, rwkv_lnx_g, rwkv_lnx_b, moe_w_group, moe_b_group,
              moe_w_router, moe_b_router, moe_w_gate, moe_w_up, moe_w_down, final_g):
    bsz, seq, _ = x.shape
    AW = ATTN_WIDTH
    inv_freq = ROPE_THETA ** (-jnp.arange(0, ROT_DIM, 2, dtype=jnp.float32) / ROT_DIM)
    ang = positions.astype(jnp.float32)[..., None] * inv_freq
    cos = jnp.cos(ang)[:, :, None, None, :].astype(x.dtype)
    sin = jnp.sin(ang)[:, :, None, None, :].astype(x.dtype)
    c_act = jax.nn.silu(c)
    for l in range(DEPTH):
        mod = (c_act @ ada_w[l] + ada_b[l])[:, None, :]
        sh1, sc1, gt1, sh2, sc2, gt2 = jnp.split(mod, 6, axis=-1)
        h = rms_norm(x, norm1_g[l]) * (1.0 + sc1) + sh1
        proj = h @ w_in[l]
        q = proj[..., :AW].reshape(bsz, seq, ATTN_HEADS, 2, ATTN_HEAD_DIM)
        k = proj[..., AW:2 * AW].reshape(bsz, seq, ATTN_HEADS, 2, ATTN_HEAD_DIM)
        v = proj[..., 2 * AW:3 * AW].reshape(bsz, seq, ATTN_HEADS, 2 * ATTN_HEAD_DIM)
        q = partial_rotary(q, cos, sin)
        k = partial_rotary(k, cos, sin)
        attn_out = diff_attention(q, k, v, attn_lambda[l], attn_subln_g[l], lambda_init_of(l))
        rwkv_out = rwkv7_time_mix(proj[..., 3 * AW:], rwkv_shift_mu[l], rwkv_w0[l], rwkv_w_up[l],
                                  rwkv_a0[l], rwkv_a_up[l], rwkv_g_up[l], rwkv_k_k[l], rwkv_k_a[l],
                                  rwkv_r_k[l], rwkv_lnx_g[l], rwkv_lnx_b[l])
        mixed = jnp.concatenate([attn_out, rwkv_out], axis=-1) @ w_out[l]
        x = x + gt1 * mixed
        h2 = rms_norm(x, norm2_g[l]) * (1.0 + sc2) + sh2
        x = x + gt2 * hier_moe(h2, moe_w_group[l], moe_b_group[l], moe_w_router[l], moe_b_router[l],
                               moe_w_gate[l], moe_w_up[l], moe_w_down[l])
    return rms_norm(x, final_g)
```

```python
import math
from contextlib import ExitStack

import numpy as np
import concourse.bass as bass
import concourse.mybir as mybir
from concourse.bass_utils import run_bass_kernel_spmd

F32 = mybir.dt.float32
BF16 = mybir.dt.bfloat16
I32 = mybir.dt.int32
ALU = mybir.AluOpType
AF = mybir.ActivationFunctionType
AX = mybir.AxisListType

D = 1024
NCORES = 8
SEQ = 4096
DEPTH = 4
INW = 3232
NE = 32
DE = 256
LDC = 0.6065306597126334
ROPE_THETA = 500000.0

ENGS = ("pe", "act", "dve", "pool", "sp")
NDSEM = 12
SEM_ROLL = 30000


class Prog:
    def __init__(self, nc, same_engine_sync=True):
        self.nc = nc
        self.stack = None
        self.q = {e: [] for e in ENGS}
        self.cnt = {e: 0 for e in ENGS}
        self.seen = {e: {} for e in ENGS}
        self.last_w = {}
        self.readers = {}
        self.same = same_engine_sync
        self.esem = {}
        self.dsem = {}
        self.dcnt = {}
        self.dnext = {e: 0 for e in ENGS}
        self.nroll = 0
        self.n_inst = 0

    def setup(self, stack):
        self.root = stack
        self.stack = stack
        nc = self.nc
        for e in ENGS:
            self.esem[e] = stack.enter_context(nc.semaphore("es_" + e))
        for e in ("sp", "act", "pool"):
            self.dsem[e] = [stack.enter_context(nc.semaphore("ds_%s_%d" % (e, j))) for j in range(NDSEM)]
            self.dcnt[e] = [0] * NDSEM

    def sb(self, name, shape, dt=F32):
        self.uid = getattr(self, "uid", 0) + 1
        return self.stack.enter_context(self.nc.sbuf_tensor("%s_%d" % (name, self.uid), list(shape), dt))

    def ps(self, name, shape, dt=F32):
        return self.stack.enter_context(self.nc.psum_tensor(name, list(shape), dt))

    @staticmethod
    def key(x):
        if isinstance(x, (str, tuple)):
            return x
        t = getattr(x, "tensor", x)
        return t.name

    def _deps(self, eng, reads, writes, tokname=None):
        deps = {}
        tokname = tokname or eng

        def add(tok):
            if tok is None:
                return
            s, v, e = tok
            if e == tokname and (eng == "pe" or not self.same):
                return
            if deps.get(id(s), (None, 0))[1] < v:
                deps[id(s)] = (s, v)

        rk = [self.key(r) for r in reads]
        wk = [self.key(w) for w in writes]
        for k in rk:
            add(self.last_w.get(k))
        for k in wk:
            add(self.last_w.get(k))
            for t in self.readers.get(k, ()):
                add(t)
        waits = []
        for sid, (s, v) in deps.items():
            if self.seen[eng].get(sid, 0) < v:
                self.seen[eng][sid] = v
                waits.append((s, v))
        return rk, wk, waits

    def _commit(self, rk, wk, tok):
        for k in wk:
            self.last_w[k] = tok
            self.readers[k] = []
        for k in rk:
            if k not in wk:
                lst = self.readers.setdefault(k, [])
                lst[:] = [t for t in lst if t[0] is not tok[0]]
                lst.append(tok)

    def op(self, eng, fn, reads=(), writes=(), cls=None):
        tokname = eng if cls is None else eng + cls
        rk, wk, waits = self._deps(eng, reads, writes, tokname)
        if self.cnt[eng] >= SEM_ROLL:
            self.nroll += 1
            self.esem[eng] = self.root.enter_context(self.nc.semaphore("es_%s_%d" % (eng, self.nroll)))
            self.cnt[eng] = 0
        self.cnt[eng] += 1
        tok = (self.esem[eng], self.cnt[eng], tokname)
        self.q[eng].append((waits, fn, self.esem[eng], 1))
        self._commit(rk, wk, tok)
        self.n_inst += 1
        return tok

    def dma(self, eng, out, in_, reads=None, writes=None, **kw):
        reads = [in_] if reads is None else reads
        writes = [out] if writes is None else writes
        rk, wk, waits = self._deps(eng, reads, writes)
        j = self.dnext[eng]
        self.dnext[eng] = (j + 1) % NDSEM
        s = self.dsem[eng][j]
        prev = self.dcnt[eng][j]
        if prev > 0 and self.seen[eng].get(id(s), 0) < prev:
            self.seen[eng][id(s)] = prev
            waits.append((s, prev))
        self.dcnt[eng][j] = prev + 16
        tok = (s, prev + 16, "dma_" + eng)
        self.q[eng].append((waits, lambda e: e.dma_start(out=out, in_=in_, **kw), s, 16))
        self._commit(rk, wk, tok)
        self.n_inst += 1
        return tok

    def barrier(self):
        toks = [(self.esem[e], self.cnt[e]) for e in ENGS if self.cnt[e] > 0]
        for e in ("sp", "act", "pool"):
            for j in range(NDSEM):
                if self.dcnt[e][j] > 0:
                    toks.append((self.dsem[e][j], self.dcnt[e][j]))
        for e in ENGS:
            waits = []
            for s, v in toks:
                if s is self.esem[e]:
                    continue
                if self.seen[e].get(id(s), 0) < v:
                    self.seen[e][id(s)] = v
                    waits.append((s, v))
            if waits:
                self.q[e].append((waits, None, None, 0))

    def finish_wait(self, eng, keys):
        rk, wk, waits = self._deps(eng, keys, [])
        self.q[eng].append((waits, None, None, 0))

    def emit(self):
        nc = self.nc
        names = {"pe": "tensor", "act": "scalar", "dve": "vector", "pool": "gpsimd", "sp": "sync"}
        with nc.Block() as block:
            for e in ENGS:
                lst = self.q[e]
                if not lst:
                    continue

                def body(engobj, lst=lst):
                    for waits, fn, sem, inc in lst:
                        for s, v in waits:
                            engobj.wait_ge(s, v)
                        if fn is not None:
                            fn(engobj).then_inc(sem, inc)

                getattr(block, names[e])(body)

    def mm(self, out, lhsT, rhs, start=True, stop=True, r=None, w=None):
        kp = lhsT.partition_size()
        cls = None if kp > 64 else "_%d_%d" % (lhsT.base_partition(), 32 if kp <= 32 else 64)
        return self.op("pe", lambda e: e.matmul(out, lhsT=lhsT, rhs=rhs, start=start, stop=stop),
                       r if r is not None else [lhsT, rhs], w if w is not None else [out], cls=cls)

    def tr(self, out, in_, ident, r=None, w=None):
        kp = in_.partition_size()
        cls = None if kp > 64 else "_%d_%d" % (in_.base_partition(), 32 if kp <= 32 else 64)
        return self.op("pe", lambda e: e.transpose(out, in_, ident),
                       r if r is not None else [in_, ident], w if w is not None else [out], cls=cls)

    def act(self, out, in_, func, bias=None, scale=None, accum=None, r=None, w=None, eng="act"):
        kw = {}
        rr = [in_]
        if bias is not None:
            kw["bias"] = bias
            if not isinstance(bias, (int, float)):
                rr.append(bias)
        if scale is not None:
            kw["scale"] = scale
            if not isinstance(scale, (int, float)):
                rr.append(scale)
        ww = [out]
        if accum is not None:
            kw["accum_out"] = accum
            ww.append(accum)
        return self.op("act", lambda e: e.activation(out=out, in_=in_, func=func, **kw),
                       r if r is not None else rr, w if w is not None else ww)

    def tt(self, eng, out, in0, in1, op, r=None, w=None):
        return self.op(eng, lambda e: e.tensor_tensor(out=out, in0=in0, in1=in1, op=op),
                       r if r is not None else [in0, in1], w if w is not None else [out])

    def ts(self, eng, out, in0, s1, op0, s2=None, op1=None, r=None, w=None):
        rr = [in0]
        for s in (s1, s2):
            if s is not None and not isinstance(s, (int, float)):
                rr.append(s)
        if op1 is None:
            fn = lambda e: e.tensor_scalar(out=out, in0=in0, scalar1=s1, scalar2=None, op0=op0)
        else:
            fn = lambda e: e.tensor_scalar(out=out, in0=in0, scalar1=s1, scalar2=s2, op0=op0, op1=op1)
        return self.op(eng, fn, r if r is not None else rr, w if w is not None else [out])

    def stt(self, eng, out, in0, scalar, in1, op0, op1, r=None, w=None):
        rr = [in0, in1]
        if not isinstance(scalar, (int, float)):
            rr.append(scalar)
        return self.op(eng, lambda e: e.scalar_tensor_tensor(out=out, in0=in0, scalar=scalar, in1=in1, op0=op0, op1=op1),
                       r if r is not None else rr, w if w is not None else [out])

    def cp(self, eng, out, in_, r=None, w=None):
        if eng == "act":
            return self.act(out, in_, AF.Copy, r=r, w=w)
        return self.op(eng, lambda e: e.tensor_copy(out=out, in_=in_),
                       r if r is not None else [in_], w if w is not None else [out])

    def memset(self, eng, out, val, w=None):
        return self.op(eng, lambda e: e.memset(out, val), [], w if w is not None else [out])

    def recip(self, out, in_, r=None, w=None):
        return self.op("dve", lambda e: e.reciprocal(out=out, in_=in_),
                       r if r is not None else [in_], w if w is not None else [out])

    def red(self, out, in_, op, r=None, w=None):
        return self.op("dve", lambda e: e.tensor_reduce(out=out, in_=in_, axis=AX.X, op=op),
                       r if r is not None else [in_], w if w is not None else [out])


def host_consts():
    c = {}
    c["idn"] = np.eye(128, dtype=np.float32)
    rp = np.zeros((128, 128), np.float32)
    for f in range(128):
        d = f % 64
        if d < 8:
            rp[f + 8, f] = 1.0
        elif d < 16:
            rp[f - 8, f] = 1.0
    c["rperm"] = rp
    invf = (ROPE_THETA ** (-np.arange(0, 16, 2, dtype=np.float32) / np.float32(16))).astype(np.float32)
    col = np.zeros((128, 4), np.float32)
    for f in range(128):
        d = f % 64
        if d < 16:
            col[f, 0] = invf[d % 8]
            col[f, 1] = -1.0 if d < 8 else 1.0
    c["ropecol"] = col
    bd = np.zeros((128, 128), np.float32)
    bd[:64, :64] = 1.0
    bd[64:, 64:] = 1.0
    c["bd64"] = bd
    c["ones"] = np.ones((128, 128), np.float32)
    s = np.arange(64)[:, None]
    t = np.arange(64)[None, :]
    m = np.zeros((64, 192), np.float32)
    m[:, 0:64] = (s < t)
    m[:, 64:128] = (s <= t)
    m[:, 128:192] = (t < s)
    c["masks"] = m
    rm = np.ones((128, 512), np.float32)
    rm[:, ::64] = 0.0
    c["rmask"] = rm
    c["idn64x8"] = np.tile(np.eye(64, dtype=np.float32), (1, 8))
    return c


CONST_SHAPES = {"idn": [128, 128], "rperm": [128, 128], "ropecol": [128, 4], "bd64": [128, 128],
                "ones": [128, 128], "masks": [64, 192], "rmask": [128, 512], "idn64x8": [64, 512]}


def lambda_init_of(layer):
    return 0.8 - 0.6 * math.exp(-0.3 * layer)


def build_program(S=SEQ, L=DEPTH, dbg=False, same_sync=True, do_a=True, do_b=True, do_c=True, GB=256, bstop=99):
    assert S % 512 == 0
    NG = S // 512
    NT = S // 128
    SG = min(2048, S)
    NSG = S // SG
    NTS = SG // 128
    NGB = S // GB
    NCB = GB // 64
    nc = bass.Bass("TRN2", target_bir_lowering=False)
    P = Prog(nc, same_engine_sync=same_sync)

    def din(name, shape, dt=F32):
        return nc.dram_tensor(name, list(shape), dt, kind="ExternalInput").ap()

    def dscr(name, shape, dt=F32):
        return nc.dram_tensor(name, list(shape), dt).ap()

    x_in = din("x", [S, D])
    pos_in = din("pos", [1, S], I32)
    c_in = din("c_t", [128, 8])
    ada_w = din("ada_w", [L, D, 6 * D])
    ada_b = din("ada_b", [L, 1, 6 * D])
    g1c = din("g1c", [L, 128, 8])
    g2c = din("g2c", [L, 128, 8])
    w_in = din("w_in", [L, D, INW])
    w_out = din("w_out", [L, D, D])
    lamv = din("lamv", [L, 1, 256])
    sublng = din("sublng", [L, 128, 1])
    mu_c = din("mu_c", [L, 128, 14])
    rw_cols = din("rw_cols", [L, 128, 28])
    lw_in = din("lw", [L, 64, 512])
    gu_in = din("gu", [L, 96, 512])
    wr_in = din("wr", [L, D, 36])
    br_in = din("br", [L, 1, 36])
    wg_in = din("wg", [L, NE, D, DE])
    wu_in = din("wu", [L, NE, D, DE])
    wd_in = din("wd", [L, NE, DE, D])
    fg_in = din("fg", [1, D])
    cst = {k: din("k_" + k, v) for k, v in CONST_SHAPES.items()}
    y_out = nc.dram_tensor("y", [S, D], F32, kind="ExternalOutput").ap()

    xcur = dscr("xcur", [S, D])
    x1_d = dscr("x1_d", [S, D])
    hT_d = dscr("hT_d", [128, 8, S], BF16)
    ao_d = dscr("ao_d", [128, 4, S], BF16)
    tabC_d = dscr("tabC_d", [128, S])
    tabS_d = dscr("tabS_d", [128, S])
    dbg_outs = {}
    if dbg:
        dbg_outs["d_ao"] = nc.dram_tensor("d_ao", [128, 4, S], BF16, kind="ExternalOutput").ap()
        dbg_outs["d_x1"] = nc.dram_tensor("d_x1", [S, D], F32, kind="ExternalOutput").ap()
        dbg_outs["d_x2"] = nc.dram_tensor("d_x2", [S, D], F32, kind="ExternalOutput").ap()

    root = ExitStack()
    with root:
        P.setup(root)
        DB = [P.ps("db%d" % i, [128, 1024]) for i in range(4)]

        def bank(i):
            return DB[i // 2][:, (i % 2) * 512:(i % 2) * 512 + 512]

        def bk(i):
            return ("bank", i)

        idn = P.sb("idn", [128, 128])
        ones = P.sb("ones", [128, 128])
        ones_bf = P.sb("ones_bf", [128, 128], BF16)
        rperm = P.sb("rperm", [128, 128])
        bd64 = P.sb("bd64", [128, 128])
        masks = P.sb("masks", [64, 192])
        rmask = P.sb("rmask", [128, 512])
        idn8 = P.sb("idn8", [64, 512])
        ropecol = P.sb("ropecol", [128, 4])
        onesm = P.sb("onesm", [128, 128])
        cact = P.sb("cact", [128, 8])
        fgbc = P.sb("fgbc", [128, D])
        for t, k in ((idn, "idn"), (ones, "ones"), (rperm, "rperm"), (bd64, "bd64"), (masks, "masks"),
                     (rmask, "rmask"), (idn8, "idn64x8"), (ropecol, "ropecol")):
            P.dma("sp", t[:], cst[k])
        P.cp("dve", ones_bf[:], ones[:])
        P.ts("dve", onesm[:], ones[:], 1.0 / 128.0, ALU.mult)
        P.dma("sp", cact[:], c_in)
        P.act(cact[:], cact[:], AF.Silu)
        P.dma("sp", fgbc[:], fg_in.partition_broadcast(128))
        A1 = P.sb("A1", [128, 8]); B1 = P.sb("B1", [128, 8])
        A2 = P.sb("A2", [128, 8]); B2 = P.sb("B2", [128, 8])
        G1bc = P.sb("G1bc", [128, D]); G2bc = P.sb("G2bc", [128, D])
        lam = P.sb("lam", [1, 4])
        sgcol = P.sb("sgcol", [128, 1])

        def b3(ap, a, b):
            return ap.unsqueeze(2).to_broadcast([ap.shape[0], a, b])

        def v3(ap, a):
            return ap.rearrange("p (a b) -> p a b", a=a)

        def rms_rstd(xtt, junk, stt_):
            P.memset("pool", stt_[:, 0:1], 0.0, w=[stt_])
            P.act(junk[:], xtt[:], AF.Square, accum=stt_[:, 0:1], r=[xtt, stt_], w=[junk, stt_])
            P.ts("dve", stt_[:, 1:2], stt_[:, 0:1], 1.0 / D, ALU.mult, 1e-6, ALU.add, r=[stt_], w=[stt_])
            P.act(stt_[:, 2:3], stt_[:, 1:2], AF.Sqrt, r=[stt_], w=[stt_])
            P.recip(stt_[:, 3:4], stt_[:, 2:3], r=[stt_], w=[stt_])

        def norm_transpose(xtt, xhh, stt_, Acol, Bcol, htmp, out3, out_eng="pool"):
            rms_rstd(xtt, xhh, stt_)
            P.ts("pool", xhh[:], xtt[:], stt_[:, 3:4], ALU.mult, r=[xtt, stt_], w=[xhh])
            for k in range(8):
                P.tr(DB[0][:, k * 128:(k + 1) * 128], xhh[:, k * 128:(k + 1) * 128], idn[:], w=[bk(k // 4)])
            pv = v3(DB[0][:], 8)
            P.tt("dve", htmp[:], pv, b3(Acol[:], 8, 128), ALU.mult, r=[bk(0), bk(1), Acol], w=[htmp])
            P.tt(out_eng, out3, htmp[:], b3(Bcol[:], 8, 128), ALU.add, r=[htmp, Bcol], w=[out3])

        with ExitStack() as sc:
            P.stack = sc
            posi = P.sb("posi", [128, S], I32)
            ang = P.sb("ang", [128, S])
            kf_ = P.sb("kf_", [128, S])
            ki_ = P.sb("ki_", [128, S], I32)
            kg_ = P.sb("kg_", [128, S])
            P.dma("sp", posi[:], pos_in.partition_broadcast(128))
            P.cp("dve", ang[:], posi[:])
            P.ts("pool", ang[:], ang[:], ropecol[:, 0:1], ALU.mult)
            for which, shift, dst in (("sin", 0.5, tabS_d), ("cos", 0.75, tabC_d)):
                P.ts("dve", kf_[:], ang[:], 1.0 / (2.0 * math.pi), ALU.mult, shift, ALU.add)
                P.cp("dve", ki_[:], kf_[:])
                P.cp("pool", kg_[:], ki_[:])
                P.tt("dve", kf_[:], kf_[:], kg_[:], ALU.subtract)
                P.ts("pool", kg_[:], kf_[:], 0.0, ALU.is_lt)
                P.tt("dve", kf_[:], kf_[:], kg_[:], ALU.add)
                P.ts("dve", kf_[:], kf_[:], 2.0 * math.pi, ALU.mult, -math.pi, ALU.add)
                P.ts("dve", kf_[:], kf_[:], 3.14159, ALU.min, -3.14159, ALU.max)
                P.act(kf_[:], kf_[:], AF.Sin)
                if which == "sin":
                    P.ts("dve", kf_[:], kf_[:], ropecol[:, 1:2], ALU.mult)
                P.dma("sp", dst, kf_[:])
        P.barrier()

        def modulation(l):
            linit = lambda_init_of(l)
            awb = [P.sb("awb%d" % i, [128, 8, 512]) for i in range(2)]
            brow = P.sb("brow", [1, 6 * D])
            mrow = P.sb("mrow", [1, 512])
            colt = P.sb("colt", [128, 48])
            gc1 = P.sb("gc1", [128, 8]); gc2 = P.sb("gc2", [128, 8])
            lrow = P.sb("lrow", [1, 256]); ltmp = P.sb("ltmp", [1, 128]); lsum = P.sb("lsum", [1, 2])
            sgl = P.sb("sgl", [128, 1])
            P.dma("sp", brow[:], ada_b[l])
            P.dma("sp", gc1[:], g1c[l]); P.dma("sp", gc2[:], g2c[l])
            awv = ada_w[l].rearrange("(k p) n -> p k n", p=128)
            for cc in range(12):
                wb = awb[cc % 2]
                P.dma("sp" if cc % 2 == 0 else "act", wb[:], awv[:, :, cc * 512:(cc + 1) * 512])
                for k in range(8):
                    P.mm(bank(0)[0:1, :], cact[:, k:k + 1], wb[:, k, :], start=(k == 0), stop=(k == 7), w=[bk(0)])
                P.tt("dve", mrow[:], bank(0)[0:1, :], brow[:, cc * 512:(cc + 1) * 512], ALU.add,
                     r=[bk(0), brow], w=[mrow])
                if cc in (4, 5, 10, 11):
                    P.mm(bank(1), ones[0:1, :], mrow[:], w=[bk(1)])
                    dstg = G1bc if cc < 6 else G2bc
                    off = (cc % 2) * 512
                    P.cp("act", dstg[:, off:off + 512], bank(1), r=[bk(1)], w=[dstg])
                else:
                    for j in range(4):
                        P.mm(bank(1)[:, j:j + 1], mrow[0:1, j * 128:(j + 1) * 128], ones[0:1, 0:1], w=[bk(1)])
                    P.cp("act", colt[:, cc * 4:cc * 4 + 4], bank(1)[:, 0:4], r=[bk(1)], w=[colt])
            P.stt("dve", A1[:], colt[:, 8:16], 1.0, gc1[:], ALU.add, ALU.mult)
            P.cp("dve", B1[:], colt[:, 0:8])
            P.stt("dve", A2[:], colt[:, 32:40], 1.0, gc2[:], ALU.add, ALU.mult)
            P.cp("dve", B2[:], colt[:, 24:32])
            P.dma("sp", lrow[:], lamv[l])
            lv = lrow[:].rearrange("o (a b d) -> o a b d", a=2, b=2)
            P.tt("dve", v3(ltmp[:], 2), lv[:, :, 0, :], lv[:, :, 1, :], ALU.mult, r=[lrow], w=[ltmp])
            P.red(lsum[:], v3(ltmp[:], 2), ALU.add, r=[ltmp], w=[lsum])
            P.act(lsum[:], lsum[:], AF.Exp)
            P.tt("dve", lam[:, 0:1], lsum[:, 0:1], lsum[:, 1:2], ALU.subtract, r=[lsum], w=[lam])
            P.ts("dve", lam[:, 0:1], lam[:, 0:1], float(linit), ALU.add)
            P.dma("sp", sgl[:], sublng[l])
            P.ts("dve", sgcol[:], sgl[:], float(1.0 - linit), ALU.mult)

        def pass_a(l):
            xsrc = x_in if l == 0 else xcur
            wqkv = P.sb("wqkv", [128, 8, 1536], BF16)
            kc = P.sb("kc", [128, 4, S], BF16)
            vc = P.sb("vc", [128, NT, 512], BF16)
            xt = [P.sb("a_xt%d" % i, [128, D]) for i in range(2)]
            xh = P.sb("a_xh", [128, D])
            st = [P.sb("a_st%d" % i, [128, 4]) for i in range(2)]
            htmp = P.sb("a_htmp", [128, 8, 128])
            hT = [P.sb("a_hT%d" % i, [128, 8, 512], BF16) for i in range(2)]
            tC = [P.sb("a_tC%d" % i, [128, 512]) for i in range(2)]
            tS = [P.sb("a_tS%d" % i, [128, 512]) for i in range(2)]
            qf = [P.sb("a_qf%d" % i, [128, 512]) for i in range(2)]
            t1 = [P.sb("a_t1%d" % i, [128, 512]) for i in range(2)]
            t2 = [P.sb("a_t2%d" % i, [128, 512]) for i in range(2)]
            qr = [P.sb("a_qr%d" % i, [128, 4, 512], BF16) for i in range(2)]
            PT = [P.sb("a_PT%d" % i, [128, 512], BF16) for i in range(3)]
            rs = P.sb("a_rs", [1, 512])
            bcs = P.sb("a_bcs", [128, 512])
            om = [P.sb("a_om%d" % i, [128, 512]) for i in range(2)]
            osq = P.sb("a_osq", [128, 512])
            rstd = P.sb("a_rstd", [128, 512])
            aoT = [P.sb("a_aoT%d" % i, [128, 4, 512], BF16) for i in range(2)]
            wv = w_in[l].rearrange("(k p) n -> p k n", p=128)
            P.dma("pool", wqkv[:], wv[:, :, 0:1536])
            npt = 0
            for g in range(NG):
                hTg = hT[g % 2]
                for t in range(4):
                    ti = g * 4 + t
                    xtt = xt[ti % 2]
                    P.dma("sp", xtt[:], xsrc[ti * 128:(ti + 1) * 128, :], reads=[("xcur", ti)])
                    norm_transpose(xtt, xh, st[ti % 2], A1, B1, htmp, hTg[:, :, t * 128:(t + 1) * 128])
                P.dma("sp", hT_d[:, :, g * 512:(g + 1) * 512], hTg[:], writes=[("hT_d", g)])
                tCg = tC[g % 2]; tSg = tS[g % 2]
                P.dma("act", tCg[:], tabC_d[:, g * 512:(g + 1) * 512])
                P.dma("act", tSg[:], tabS_d[:, g * 512:(g + 1) * 512])
                qrg = qr[g % 2]
                for c in range(8):
                    pb = 2 + (c % 2)
                    sb2 = 4 + (c % 2)
                    for k in range(8):
                        P.mm(bank(pb), wqkv[:, k, c * 128:(c + 1) * 128], hTg[:, k, :], start=(k == 0),
                             stop=(k == 7), w=[bk(pb)])
                    qff = qf[c % 2]; t1c = t1[c % 2]; t2c = t2[c % 2]
                    P.cp("act", qff[:], bank(pb), r=[bk(pb)], w=[qff])
                    P.mm(bank(sb2), rperm[:], qff[:], w=[bk(sb2)])
                    P.tt("pool", t1c[:], qff[:], tCg[:], ALU.mult)
                    P.tt("dve", t2c[:], bank(sb2), tSg[:], ALU.mult, r=[bk(sb2), tSg], w=[t2c])
                    if c < 4:
                        P.tt("pool", qrg[:, c, :], t1c[:], t2c[:], ALU.add, w=[qrg])
                    else:
                        P.tt("pool", kc[:, c - 4, g * 512:(g + 1) * 512], t1c[:], t2c[:], ALU.add, w=[("kc", g)])
                for t in range(4):
                    pb = 2 + (t % 2)
                    for k in range(8):
                        P.mm(bank(pb), hTg[:, k, t * 128:(t + 1) * 128], wqkv[:, k, 1024:1536], start=(k == 0),
                             stop=(k == 7), w=[bk(pb)])
                    P.cp("act", vc[:, g * 4 + t, :], bank(pb), r=[bk(pb)], w=[("vc", g)])
                aog = aoT[g % 2]
                nkt = 4 * g + 4
                for h in range(4):
                    for m in range(2):
                        prt = slice(64 * m, 64 * m + 64)
                        for j in range(nkt):
                            jl = j - 4 * g
                            qs = 128 * jl if jl > 0 else 0
                            sb_ = 4 + (npt % 2)
                            PTt = PT[npt % 3]
                            npt += 1
                            P.mm(bank(sb_)[:, qs:512], kc[prt, h, j * 128:(j + 1) * 128], qrg[prt, h, qs:512],
                                 r=[("kc", j // 4), qrg], w=[bk(sb_)])
                            P.act(PTt[:, qs:512], bank(sb_)[:, qs:512], AF.Exp, scale=0.125, r=[bk(sb_)], w=[PTt])
                            if jl >= 0:
                                P.memset("pool", PTt[64:128, qs:qs + 64], 0.0, w=[PTt])
                            P.mm(bank(6)[:, qs:512], vc[:, j, h * 128:(h + 1) * 128], PTt[:, qs:512],
                                 start=(j == 0), stop=(j == nkt - 1), r=[("vc", j // 4), PTt], w=[bk(6)])
                            P.mm(bank(7)[0:1, qs:512], ones_bf[:, 0:1], PTt[:, qs:512],
                                 start=(j == 0), stop=(j == nkt - 1), r=[PTt], w=[bk(7)])
                        P.recip(rs[:], bank(7)[0:1, :], r=[bk(7)], w=[rs])
                        if m == 1:
                            P.ts("dve", rs[:], rs[:], lam[0:1, 0:1], ALU.mult)
                        P.mm(bank(0), ones[0:1, :], rs[:], w=[bk(0)])
                        P.cp("act", bcs[:], bank(0), r=[bk(0)], w=[bcs])
                        P.tt("dve", om[m][:], bank(6), bcs[:], ALU.mult, r=[bk(6), bcs], w=[om[m]])
                    P.tt("pool", om[0][:], om[0][:], om[1][:], ALU.subtract)
                    P.tt("pool", osq[:], om[0][:], om[0][:], ALU.mult)
                    P.mm(bank(1), onesm[:], osq[:], w=[bk(1)])
                    P.ts("dve", rstd[:], bank(1), 1e-5, ALU.add, r=[bk(1)], w=[rstd])
                    P.act(rstd[:], rstd[:], AF.Sqrt)
                    P.recip(rstd[:], rstd[:])
                    P.tt("dve", om[0][:], om[0][:], rstd[:], ALU.mult)
                    P.ts("pool", aog[:, h, :], om[0][:], sgcol[:, 0:1], ALU.mult, r=[om[0], sgcol], w=[aog])
                P.dma("sp", ao_d[:, :, g * 512:(g + 1) * 512], aog[:], writes=[("ao_d", g)])

        def pass_b(l):
            xsrc = x_in if l == 0 else xcur
            wrw = P.sb("wrw", [128, 8, 1696], BF16)
            wout = P.sb("wout", [128, 8, D], BF16)
            LW = P.sb("LW", [64, 512]); GU = P.sb("GU", [96, 512])
            muc = P.sb("muc", [128, 14]); rwc = P.sb("rwc", [128, 28]); omka = P.sb("omka", [128, 4])
            ST = P.sb("ST", [128, 4, 64])
            bnd = P.sb("bnd", [128, 14])
            hTb = [P.sb("b_hT%d" % i, [128, 8, GB], BF16) for i in range(2)]
            pcb = [P.sb("b_pc%d" % i, [128, GB + 1]) for i in range(2)]
            lt = P.sb("b_lt", [128, GB])
            PMr = P.sb("PMr", [128, GB]); PMk = P.sb("PMk", [128, GB]); PMv = P.sb("PMv", [128, 4, GB])
            PL1 = P.sb("PL1", [64, GB]); PL2 = P.sb("PL2", [96, GB])
            T = [P.sb("b_T%d" % i, [128, GB]) for i in range(8)]
            gT = [P.sb("b_gT%d" % i, [128, GB]) for i in range(4)]
            bonusT = [P.sb("b_bo%d" % i, [128, GB]) for i in range(4)]
            ARt = P.sb("ARt", [128, 4, NCB, 2, 64])
            BKt = P.sb("BKt", [128, 4, NCB, 2, 64])
            PC = P.sb("PC", [128, 4, NCB])
            Vtok = [P.sb("Vtok%d" % i, [64, 512]) for i in range(2)]
            Btok = [P.sb("Btok%d" % i, [64, 512]) for i in range(2)]
            Ktok = [P.sb("Ktok%d" % i, [64, 512]) for i in range(2)]
            M1 = P.sb("M1", [64, 8, 128]); M2 = P.sb("M2", [64, 8, 128])
            X = P.sb("Xm", [64, 8, 64]); YT = P.sb("YT", [64, 8, 128])
            X0s = P.sb("X0s", [64, 512]); Us = P.sb("Us", [64, 512])
            Ys = P.sb("Ys", [64, 512]); Ysq = P.sb("Ysq", [64, 512]); gs = P.sb("gs", [64, 16])
            yT = P.sb("yT", [128, 4, GB])
            fo = P.sb("fo", [128, GB])
            catT = [P.sb("catT%d" % i, [128, 8, GB], BF16) for i in range(2)]
            xr = [P.sb("b_xr%d" % i, [128, D]) for i in range(2)]
            mix = [P.sb("b_mix%d" % i, [128, D]) for i in range(2)]
            wv = w_in[l].rearrange("(k p) n -> p k n", p=128)
            P.dma("pool", wrw[:], wv[:, :, 1536:INW])
            P.dma("pool", wout[:], w_out[l].rearrange("(k p) n -> p k n", p=128))
            P.dma("sp", LW[:], lw_in[l]); P.dma("sp", GU[:], gu_in[l])
            P.dma("sp", muc[:], mu_c[l]); P.dma("sp", rwc[:], rw_cols[l])
            P.ts("dve", omka[:], rwc[:, 12:16], -1.0, ALU.mult, 1.0, ALU.add)
            if bstop == 421:
                P.memset("dve", ST[:].rearrange("p a b -> p (a b)"), 0.0, w=[ST])
            else:
                P.memset("pool", ST[:], 0.0)
            P.memset("pool", bnd[:], 0.0)
            rmk = rmask[:, 0:GB]

            def proj_chunk(hTg, c, dst, nchunk):
                if c < 12:
                    cols = slice(c * 128, (c + 1) * 128); M = 128
                elif c == 12:
                    cols = slice(1536, 1600); M = 64
                else:
                    cols = slice(1600, 1696); M = 96
                pb = nchunk % 2
                pc = pcb[nchunk % 2]
                for k in range(8):
                    P.mm(bank(pb)[0:M, 0:GB], wrw[:, k, cols], hTg[:, k, :], start=(k == 0), stop=(k == 7), w=[bk(pb)])
                P.cp("act", pc[0:M, 1:GB + 1], bank(pb)[0:M, 0:GB], r=[bk(pb)], w=[pc])
                P.cp("pool", pc[0:M, 0:1], bnd[0:M, c:c + 1], r=[bnd], w=[pc])
                P.tt("pool", lt[0:M, :], pc[0:M, 0:GB], pc[0:M, 1:GB + 1], ALU.subtract, r=[pc], w=[lt])
                P.stt("dve", dst, lt[0:M, :], muc[0:M, c:c + 1], pc[0:M, 1:GB + 1], ALU.mult, ALU.add,
                      r=[lt, muc, pc], w=[dst])
                P.cp("pool", bnd[0:M, c:c + 1], pc[0:M, GB:GB + 1], r=[pc], w=[bnd])

            if bstop == 0:
                return
            nch = 0
            for g in range(NGB):
                hTg = hTb[g % 2]
                P.dma("sp", hTg[:], hT_d[:, :, g * GB:(g + 1) * GB], reads=[("hT_d", (g * GB) // 512)])
                proj_chunk(hTg, 12, PL1[:], nch); nch += 1
                proj_chunk(hTg, 13, PL2[:], nch); nch += 1
                if bstop == 1:
                    return
                P.act(PL1[0:32, :], PL1[0:32, :], AF.Tanh)
                P.act(PL2[:], PL2[:], AF.Sigmoid)
                for hp in range(4):
                    proj_chunk(hTg, hp, PMr[:], nch); nch += 1
                    proj_chunk(hTg, 4 + hp, PMk[:], nch); nch += 1
                    proj_chunk(hTg, 8 + hp, PMv[:, hp, :], nch); nch += 1
                    cs = slice(hp * 128, (hp + 1) * 128)

                    def col(pi, hp=hp):
                        return rwc[:, pi * 4 + hp:pi * 4 + hp + 1]
                    ta, tb_, tc_, td, te, tf, tg, th = T
                    r_ = PMr[:]; k_ = PMk[:]; v_ = PMv[:, hp, :]
                    P.mm(bank(2)[:, 0:GB], LW[0:32, cs], PL1[0:32, :], w=[bk(2)])
                    P.act(ta[:], bank(2)[:, 0:GB], AF.Sigmoid, bias=col(0), r=[bk(2), rwc], w=[ta])
                    P.op("dve", lambda e, o=tb_, a=ta: e.tensor_tensor_scan(out=o[:], data0=rmk, data1=a[:], initial=0.0,
                                                                          op0=ALU.mult, op1=ALU.add), [rmask, ta], [tb_])
                    P.act(tc_[:], tb_[:], AF.Exp, scale=-LDC)
                    P.act(td[:], tb_[:], AF.Exp, scale=LDC)
                    P.tt("pool", te[:], tb_[:], ta[:], ALU.subtract)
                    P.act(te[:], te[:], AF.Exp, scale=-LDC)
                    P.mm(bank(3)[:, 0:GB], LW[32:64, cs], PL1[32:64, :], w=[bk(3)])
                    P.act(tf[:], bank(3)[:, 0:GB], AF.Sigmoid, bias=col(1), r=[bk(3), rwc], w=[tf])
                    P.mm(bank(2)[:, 0:GB], GU[0:96, cs], PL2[0:96, :], w=[bk(2)])
                    P.cp("act", gT[hp][:], bank(2)[:, 0:GB], r=[bk(2)], w=[gT[hp]])
                    P.ts("pool", tg[:], k_, col(2), ALU.mult, r=[PMk, rwc], w=[tg])
                    P.tt("pool", th[:], tg[:], tg[:], ALU.mult)
                    P.mm(bank(3)[:, 0:GB], bd64[:], th[:], w=[bk(3)])
                    P.act(th[:], bank(3)[:, 0:GB], AF.Sqrt, r=[bk(3)], w=[th])
                    P.ts("dve", th[:], th[:], 1e-12, ALU.max)
                    P.recip(th[:], th[:])
                    P.tt("pool", tg[:], tg[:], th[:], ALU.mult)
                    P.ts("dve", th[:], tf[:], col(3), ALU.mult, omka[:, hp:hp + 1], ALU.add, r=[tf, rwc, omka], w=[th])
                    P.tt("pool", th[:], k_, th[:], ALU.mult, r=[PMk, th], w=[th])
                    P.tt("pool", ta[:], tg[:], tf[:], ALU.mult)
                    P.stt("dve", tb_[:], r_, col(4), th[:], ALU.mult, ALU.mult, r=[PMr, rwc, th], w=[tb_])
                    P.mm(bank(2)[:, 0:GB], bd64[:], tb_[:], w=[bk(2)])
                    P.tt("dve", bonusT[hp][:], bank(2)[:, 0:GB], v_, ALU.mult, r=[bk(2), PMv], w=[bonusT[hp]])
                    P.stt("dve", ARt[:, hp, :, 0, :], v3(tg[:], NCB), -1.0, v3(te[:], NCB), ALU.mult, ALU.mult,
                          r=[tg, te], w=[("ARt", hp)])
                    P.tt("pool", ARt[:, hp, :, 1, :], v3(r_, NCB), v3(tc_[:], NCB), ALU.mult, r=[PMr, tc_], w=[("ARt", hp)])
                    P.tt("pool", BKt[:, hp, :, 0, :], v3(ta[:], NCB), v3(td[:], NCB), ALU.mult, r=[ta, td], w=[("BKt", hp)])
                    P.tt("dve", BKt[:, hp, :, 1, :], v3(th[:], NCB), v3(td[:], NCB), ALU.mult, r=[th, td], w=[("BKt", hp)])
                    P.cp("pool", PC[:, hp, :], v3(tc_[:], NCB)[:, :, 63], r=[tc_], w=[PC])

                if bstop == 2:
                    return
                ARk = [("ARt", hp) for hp in range(4)]
                BKk = [("BKt", hp) for hp in range(4)]
                for c in range(NCB):
                    cc = g * NCB + c
                    Vt = Vtok[cc % 2]; Bt = Btok[cc % 2]; Kt = Ktok[cc % 2]
                    for hp in range(4):
                        P.tr(bank(3)[0:64, hp * 128:(hp + 1) * 128], PMv[:, hp, c * 64:(c + 1) * 64], idn[:],
                             r=[PMv, idn], w=[bk(3)])
                        P.tr(bank(0)[0:64, hp * 128:(hp + 1) * 128], BKt[:, hp, c, 0, :], idn[:],
                             r=[("BKt", hp), idn], w=[bk(0)])
                        P.tr(bank(1)[0:64, hp * 128:(hp + 1) * 128], BKt[:, hp, c, 1, :], idn[:],
                             r=[("BKt", hp), idn], w=[bk(1)])
                    P.cp("act", Vt[:], bank(3)[0:64, :], r=[bk(3)], w=[Vt])
                    P.cp("dve", Bt[:], bank(0)[0:64, :], r=[bk(0)], w=[Bt])
                    P.cp("act", Kt[:], bank(1)[0:64, :], r=[bk(1)], w=[Kt])
                    if bstop == 3:
                        return
                    for h in range(8):
                        hp, j = divmod(h, 2)
                        prt = slice(64 * j, 64 * j + 64)
                        arr = ARt[prt, hp, c, :, :].rearrange("p a t -> p (a t)")
                        P.mm(DB[2][0:64, h * 128:(h + 1) * 128], BKt[prt, hp, c, 0, :], arr,
                             r=[("BKt", hp), ("ARt", hp)], w=[bk(4 + h // 4)])
                        P.mm(DB[3][0:64, h * 128:(h + 1) * 128], BKt[prt, hp, c, 1, :], arr,
                             r=[("BKt", hp), ("ARt", hp)], w=[bk(6 + h // 4)])
                        P.mm(bank(2)[0:64, h * 64:(h + 1) * 64], ARt[prt, hp, c, 0, :], BKt[prt, hp, c, 0, :],
                             r=[("BKt", hp), ("ARt", hp)], w=[bk(2)])
                    mk2 = masks[:, 0:128].unsqueeze(1).to_broadcast([64, 8, 128])
                    mk3 = masks[:, 128:192].unsqueeze(1).to_broadcast([64, 8, 64])
                    P.tt("dve", M1[:], v3(DB[2][0:64, :], 8), mk2, ALU.mult, r=[bk(4), bk(5), masks], w=[M1])
                    P.tt("dve", M2[:], v3(DB[3][0:64, :], 8), mk2, ALU.mult, r=[bk(6), bk(7), masks], w=[M2])
                    P.tt("dve", X[:], v3(bank(2)[0:64, :], 8), mk3, ALU.mult, r=[bk(2), masks], w=[X])
                    P.cp("pool", YT[:, :, 0:64], M1[:, :, 0:64], r=[M1], w=[YT])
                    P.cp("pool", YT[:, :, 64:128], v3(idn8[:], 8), r=[idn8], w=[YT])
                    for lvl in range(6 if bstop != 414 else 0):
                        last = lvl == 5
                        for h in range(8):
                            if not last:
                                P.mm(DB[2][0:64, h * 128:(h + 1) * 128], X[:, h, :], YT[:, h, :], w=[bk(4 + h // 4)])
                                P.mm(bank(2)[0:64, h * 64:(h + 1) * 64], YT[:, h, 0:64], X[:, h, :], w=[bk(2)])
                            else:
                                P.mm(DB[2][0:64, h * 128 + 64:(h + 1) * 128], X[:, h, :], YT[:, h, 64:128],
                                     w=[bk(4 + h // 4)])
                        PYv = v3(DB[2][0:64, :], 8)
                        if not last:
                            P.cp("act", YT[:, :, 0:64], PYv[:, :, 0:64], r=[bk(4), bk(5)], w=[YT])
                        P.tt("dve", YT[:, :, 64:128], YT[:, :, 64:128], PYv[:, :, 64:128], ALU.add,
                             r=[YT, bk(4), bk(5)], w=[YT])
                        if not last:
                            P.cp("act", X[:], v3(bank(2)[0:64, :], 8), r=[bk(2)], w=[X])
                    if bstop == 4:
                        return
                    for h in range(8 if bstop not in (416, 417, 418, 419) else 0):
                        hp, j = divmod(h, 2)
                        prt = slice(64 * j, 64 * j + 64)
                        hs = slice(h * 64, (h + 1) * 64)
                        xb = 6 if bstop == 413 else 3
                        if bstop != 412:
                            if bstop == 420:
                                P.mm(bank(xb)[0:64, hs], ARt[prt, hp, c, 0, :], BKt[prt, hp, c, 1, :], start=True, stop=True,
                                     r=[("ARt", hp), ("BKt", hp)], w=[bk(xb)])
                            else:
                                P.mm(bank(xb)[0:64, hs], ARt[prt, hp, c, 0, :], ST[prt, hp, :], start=True, stop=(bstop in (411, 421)),
                                     r=[("ARt", hp), ST], w=[bk(xb)])
                        if bstop not in (411, 420, 421):
                            P.mm(bank(xb)[0:64, hs], M2[:, h, 0:64], Vt[:, hs], start=(bstop == 412), stop=True, w=[bk(xb)])
                    if bstop == 417:
                        P.cp("dve", X0s[:], bank(3)[0:64, :], r=[bk(3)], w=[X0s])
                    elif bstop == 418:
                        P.cp("act", Us[:], bank(3)[0:64, :], r=[bk(3)], w=[Us])
                    elif bstop == 419:
                        P.memset("pool", X0s[:], 0.0)
                    elif bstop != 415:
                        P.cp("act", X0s[:], bank(6 if bstop == 413 else 3)[0:64, :], r=[bk(6 if bstop == 413 else 3)], w=[X0s])
                    if bstop in (41, 411, 412, 413, 414, 415, 416, 417, 418, 419, 420, 421):
                        return
                    for h in range(8):
                        hs = slice(h * 64, (h + 1) * 64)
                        P.mm(bank(0)[0:64, hs], YT[:, h, 64:128], X0s[:, hs], w=[bk(0)])
                    P.cp("dve", Us[:], bank(0)[0:64, :], r=[bk(0)], w=[Us])
                    if bstop == 42:
                        return
                    for h in range(8):
                        hp, j = divmod(h, 2)
                        prt = slice(64 * j, 64 * j + 64)
                        hs = slice(h * 64, (h + 1) * 64)
                        P.mm(bank(1)[0:64, hs], ARt[prt, hp, c, 1, :], ST[prt, hp, :], start=True, stop=False,
                             r=[("ARt", hp), ST], w=[bk(1)])
                        P.mm(bank(1)[0:64, hs], M1[:, h, 64:128], Us[:, hs], start=False, stop=False, w=[bk(1)])
                        P.mm(bank(1)[0:64, hs], M2[:, h, 64:128], Vt[:, hs], start=False, stop=True, w=[bk(1)])
                        P.mm(bank(3)[:, hs], Bt[:, hp * 128:(hp + 1) * 128], Us[:, hs], start=True, stop=False, w=[bk(3)])
                        P.mm(bank(3)[:, hs], Kt[:, hp * 128:(hp + 1) * 128], Vt[:, hs], start=False, stop=True, w=[bk(3)])
                    if bstop == 43:
                        return
                    for j in range(2):
                        prt = slice(64 * j, 64 * j + 64)
                        psv = bank(3)[prt, :].rearrange("p (hp jj v) -> p hp jj v", hp=4, jj=2)[:, :, j, :]
                        P.tt("dve", ST[prt, :, :], ST[prt, :, :], psv, ALU.add, r=[ST, bk(3)], w=[ST])
                        P.tt("pool", ST[prt, :, :], ST[prt, :, :], PC[prt, :, c:c + 1].to_broadcast([64, 4, 64]),
                             ALU.mult, r=[ST, PC], w=[ST])
                    if bstop == 5:
                        return
                    P.cp("act", Ys[:], bank(1)[0:64, :], r=[bk(1)], w=[Ys])
                    Yv = v3(Ys[:], 8)
                    P.red(gs[:, 0:8], Yv, ALU.add, r=[Ys], w=[gs])
                    P.ts("dve", gs[:, 0:8], gs[:, 0:8], 1.0 / 64.0, ALU.mult, r=[gs], w=[gs])
                    P.tt("dve", Yv, Yv, b3(gs[:, 0:8], 8, 64), ALU.subtract, r=[Ys, gs], w=[Ys])
                    P.tt("pool", Ysq[:], Ys[:], Ys[:], ALU.mult)
                    P.red(gs[:, 8:16], v3(Ysq[:], 8), ALU.add, r=[Ysq], w=[gs])
                    P.ts("dve", gs[:, 8:16], gs[:, 8:16], 1.0 / 64.0, ALU.mult, 64e-5, ALU.add, r=[gs], w=[gs])
                    P.act(gs[:, 8:16], gs[:, 8:16], AF.Sqrt, r=[gs], w=[gs])
                    P.recip(gs[:, 8:16], gs[:, 8:16], r=[gs], w=[gs])
                    P.tt("dve", Yv, Yv, b3(gs[:, 8:16], 8, 64), ALU.mult, r=[Ys, gs], w=[Ys])
                    for hp in range(4):
                        P.tr(bank(2)[:, hp * 64:(hp + 1) * 64], Ys[:, hp * 128:(hp + 1) * 128], idn[0:64, 0:64],
                             r=[Ys, idn], w=[bk(2)])
                    P.cp("act", yT[:, :, c * 64:(c + 1) * 64], v3(bank(2)[:, 0:256], 4), r=[bk(2)], w=[yT])
                if bstop == 6:
                    return
                ct = catT[g % 2]
                for hp in range(4):
                    P.ts("dve", fo[:], yT[:, hp, :], rwc[:, 20 + hp:21 + hp], ALU.mult, rwc[:, 24 + hp:25 + hp], ALU.add,
                         r=[yT, rwc], w=[fo])
                    P.tt("pool", fo[:], fo[:], bonusT[hp][:], ALU.add)
                    P.tt("dve", ct[:, 4 + hp, :], fo[:], gT[hp][:], ALU.mult, r=[fo, gT[hp]], w=[("ct", g % 2, 1)])
                P.dma("act", ct[:, 0:4, :], ao_d[:, :, g * GB:(g + 1) * GB], reads=[("ao_d", (g * GB) // 512)],
                      writes=[("ct", g % 2, 0)])
                for t in range(GB // 128):
                    ti = g * (GB // 128) + t
                    xrr = xr[ti % 2]; mx = mix[ti % 2]
                    for half in range(2):
                        for k in range(8):
                            P.mm(DB[0][:, half * 512:(half + 1) * 512], ct[:, k, t * 128:(t + 1) * 128],
                                 wout[:, k, half * 512:(half + 1) * 512], start=(k == 0), stop=(k == 7),
                                 r=[("ct", g % 2, 0), ("ct", g % 2, 1), wout], w=[bk(half)])
                    P.dma("sp", xrr[:], xsrc[ti * 128:(ti + 1) * 128, :], reads=[("xcur", ti)])
                    P.tt("dve", mx[:], DB[0][:], G1bc[:], ALU.mult, r=[bk(0), bk(1), G1bc], w=[mx])
                    P.tt("pool", mx[:], mx[:], xrr[:], ALU.add)
                    P.dma("sp", x1_d[ti * 128:(ti + 1) * 128, :], mx[:], writes=[("x1", ti)])

        def pass_c(l):
            h2T = P.sb("h2T", [128, 8, SG], BF16)
            yacc = P.sb("yacc", [128, NTS, D])
            Gm = P.sb("Gm", [128, NTS, NE])
            wr = P.sb("wr", [128, 8, 36]); brbc = P.sb("brbc", [128, 36])
            wg = [P.sb("wg%d" % i, [128, 8, DE], BF16) for i in range(2)]
            wu = [P.sb("wu%d" % i, [128, 8, DE], BF16) for i in range(2)]
            wd = [P.sb("wd%d" % i, [128, 2, D], BF16) for i in range(2)]
            xt = [P.sb("c_xt%d" % i, [128, D]) for i in range(2)]
            xh = P.sb("c_xh", [128, D])
            st = [P.sb("c_st%d" % i, [128, 4]) for i in range(2)]
            htmp = P.sb("c_htmp", [128, 8, 128])
            h2f = [P.sb("c_h2f%d" % i, [128, 8, 128]) for i in range(2)]
            lg = P.sb("c_lg", [128, 36]); sm = P.sb("c_sm", [128, 16])
            ge = P.sb("c_ge", [128, 4]); gsel = P.sb("c_gsel", [128, 4]); pen = P.sb("c_pen", [128, 4])
            mm_ = P.sb("c_m", [128, 32]); m2 = P.sb("c_m2", [128, 32])
            sel1 = P.sb("c_sel1", [128, 32]); sel2 = P.sb("c_sel2", [128, 32])
            sil = [P.sb("c_sil%d" % i, [128, 512]) for i in range(2)]
            hid = [P.sb("c_hid%d" % i, [128, 2, 512], BF16) for i in range(2)]
            ob = [P.sb("c_ob%d" % i, [128, D]) for i in range(2)]
            P.dma("sp", wr[:], wr_in[l].rearrange("(k p) n -> p k n", p=128))
            P.dma("sp", brbc[:], br_in[l].partition_broadcast(128))
            nhid = 0
            for sg in range(NSG):
                for tl in range(NTS):
                    ti = sg * NTS + tl
                    xtt = xt[ti % 2]; hf = h2f[ti % 2]
                    P.dma("sp", xtt[:], x1_d[ti * 128:(ti + 1) * 128, :], reads=[("x1", ti)])
                    norm_transpose(xtt, xh, st[ti % 2], A2, B2, htmp, hf[:], out_eng="dve")
                    P.cp("act", h2T[:, :, tl * 128:(tl + 1) * 128], hf[:], w=[("h2T", tl // 4)])
                    for k in range(8):
                        P.mm(bank(2)[:, 0:36], hf[:, k, :], wr[:, k, :], start=(k == 0), stop=(k == 7), w=[bk(2)])
                    P.tt("dve", lg[:], bank(2)[:, 0:36], brbc[:], ALU.add, r=[bk(2), brbc], w=[lg])
                    P.red(sm[:, 0:1], lg[:, 0:4], ALU.max, r=[lg], w=[sm])
                    P.ts("dve", sm[:, 1:2], sm[:, 0:1], -1.0, ALU.mult, r=[sm], w=[sm])
                    P.memset("dve", sm[:, 2:3], 0.0, w=[sm])
                    P.act(ge[:], lg[:, 0:4], AF.Exp, bias=sm[:, 1:2], accum=sm[:, 2:3], r=[lg, sm], w=[ge, sm])
                    P.recip(sm[:, 3:4], sm[:, 2:3], r=[sm], w=[sm])
                    P.ts("dve", gsel[:], lg[:, 0:4], sm[:, 0:1], ALU.is_equal, r=[lg, sm], w=[gsel])
                    P.ts("dve", pen[:], gsel[:], 1e30, ALU.mult, -1e30, ALU.add)
                    P.tt("dve", v3(mm_[:], 4), v3(lg[:, 4:36], 4), b3(pen[:], 4, 8), ALU.add, r=[lg, pen], w=[mm_])
                    P.red(sm[:, 4:5], mm_[:], ALU.max, r=[mm_], w=[sm])
                    P.ts("dve", sel1[:], mm_[:], sm[:, 4:5], ALU.is_equal, r=[mm_, sm], w=[sel1])
                    P.stt("dve", m2[:], sel1[:], -1e30, mm_[:], ALU.mult, ALU.add)
                    P.red(sm[:, 5:6], m2[:], ALU.max, r=[m2], w=[sm])
                    P.ts("dve", sel2[:], m2[:], sm[:, 5:6], ALU.is_equal, r=[m2, sm], w=[sel2])
                    P.tt("dve", sm[:, 6:7], sm[:, 5:6], sm[:, 4:5], ALU.subtract, r=[sm], w=[sm])
                    P.act(sm[:, 7:8], sm[:, 6:7], AF.Exp, r=[sm], w=[sm])
                    P.ts("dve", sm[:, 8:9], sm[:, 7:8], 1.0, ALU.add, r=[sm], w=[sm])
                    P.recip(sm[:, 8:9], sm[:, 8:9], r=[sm], w=[sm])
                    P.tt("dve", sm[:, 9:10], sm[:, 8:9], sm[:, 3:4], ALU.mult, r=[sm], w=[sm])
                    P.tt("dve", sm[:, 10:11], sm[:, 3:4], sm[:, 9:10], ALU.subtract, r=[sm], w=[sm])
                    P.ts("dve", Gm[:, tl, :], sel1[:], sm[:, 9:10], ALU.mult, r=[sel1, sm], w=[("Gm", tl)])
                    P.stt("dve", Gm[:, tl, :], sel2[:], sm[:, 10:11], Gm[:, tl, :], ALU.mult, ALU.add,
                          r=[sel2, sm, ("Gm", tl)], w=[("Gm", tl)])
                for e in range(NE):
                    wgb = wg[e % 2]; wub = wu[e % 2]; wdb = wd[e % 2]
                    P.dma("pool", wgb[:], wg_in[l, e].rearrange("(k p) f -> p k f", p=128))
                    P.dma("pool", wub[:], wu_in[l, e].rearrange("(k p) f -> p k f", p=128))
                    P.dma("pool", wdb[:], wd_in[l, e].rearrange("(c p) n -> p c n", p=128))
                    for tb in range(SG // 512):
                        hd = hid[nhid % 2]
                        nhid += 1
                        for fc in range(2):
                            gb = 2 + fc
                            ub = 4 + fc
                            for k in range(8):
                                P.mm(bank(gb), wgb[:, k, fc * 128:(fc + 1) * 128], h2T[:, k, tb * 512:(tb + 1) * 512],
                                     start=(k == 0), stop=(k == 7), r=[wgb, ("h2T", tb)], w=[bk(gb)])
                            for k in range(8):
                                P.mm(bank(ub), wub[:, k, fc * 128:(fc + 1) * 128], h2T[:, k, tb * 512:(tb + 1) * 512],
                                     start=(k == 0), stop=(k == 7), r=[wub, ("h2T", tb)], w=[bk(ub)])
                            P.act(sil[fc][:], bank(gb), AF.Silu, r=[bk(gb)], w=[sil[fc]])
                            P.tt("dve", hd[:, fc, :], sil[fc][:], bank(ub), ALU.mult, r=[sil[fc], bk(ub)], w=[hd])
                        for t in range(4):
                            tl = tb * 4 + t
                            dbi = 0 if t % 2 == 0 else 3
                            for half in range(2):
                                for fc in range(2):
                                    P.mm(DB[dbi][:, half * 512:(half + 1) * 512], hd[:, fc, t * 128:(t + 1) * 128],
                                         wdb[:, fc, half * 512:(half + 1) * 512], start=(fc == 0), stop=(fc == 1),
                                         w=[bk(2 * dbi + half)])
                            if e == 0:
                                P.ts("dve", yacc[:, tl, :], DB[dbi][:], Gm[:, tl, e:e + 1], ALU.mult,
                                     r=[bk(2 * dbi), bk(2 * dbi + 1), ("Gm", tl)], w=[("yacc", tl)])
                            else:
                                P.stt("dve", yacc[:, tl, :], DB[dbi][:], Gm[:, tl, e:e + 1], yacc[:, tl, :], ALU.mult,
                                      ALU.add, r=[bk(2 * dbi), bk(2 * dbi + 1), ("Gm", tl), ("yacc", tl)],
                                      w=[("yacc", tl)])
                for tl in range(NTS):
                    ti = sg * NTS + tl
                    xtt = xt[ti % 2]; o = ob[ti % 2]
                    P.dma("sp", xtt[:], x1_d[ti * 128:(ti + 1) * 128, :], reads=[("x1", ti)])
                    P.tt("dve", o[:], yacc[:, tl, :], G2bc[:], ALU.mult, r=[("yacc", tl), G2bc], w=[o])
                    P.tt("pool", o[:], o[:], xtt[:], ALU.add)
                    if l < L - 1 or dbg:
                        P.dma("sp", xcur[ti * 128:(ti + 1) * 128, :], o[:], writes=[("xcur", ti)])
                    if l == L - 1:
                        stt_ = st[ti % 2]
                        rms_rstd(o, xh, stt_)
                        P.ts("dve", xh[:], o[:], stt_[:, 3:4], ALU.mult, r=[o, stt_], w=[xh])
                        P.tt("pool", o[:], xh[:], fgbc[:], ALU.mult)
                        P.dma("sp", y_out[ti * 128:(ti + 1) * 128, :], o[:], writes=[("y", ti)])

        for l in range(L):
            with ExitStack() as sc:
                P.stack = sc
                modulation(l)
            P.barrier()
            if do_a:
                with ExitStack() as sc:
                    P.stack = sc
                    pass_a(l)
                P.barrier()
            if do_b:
                with ExitStack() as sc:
                    P.stack = sc
                    pass_b(l)
                P.barrier()
            if do_c:
                with ExitStack() as sc:
                    P.stack = sc
                    pass_c(l)
                P.barrier()

        fin = [("y", ti) for ti in range(NT)]
        if dbg:
            if do_a:
                P.dma("sp", dbg_outs["d_ao"], ao_d, reads=[("ao_d", g) for g in range(NG)])
            if do_b:
                P.dma("sp", dbg_outs["d_x1"], x1_d, reads=[("x1", ti) for ti in range(NT)])
            if do_c:
                P.dma("sp", dbg_outs["d_x2"], xcur, reads=[("xcur", ti) for ti in range(NT)])
            fin += list(dbg_outs.values())
        P.finish_wait("sp", fin)
        P.emit()
    return nc, P


def host_layout(inp, b, S=SEQ, L=DEPTH):
    f = np.float32
    m = {}
    m["x"] = np.ascontiguousarray(inp["x"][b, :S], dtype=f)
    m["pos"] = np.ascontiguousarray(inp["positions"][b:b + 1, :S], dtype=np.int32)
    m["c_t"] = np.ascontiguousarray(inp["c"][b].reshape(8, 128).T, dtype=f)
    m["ada_w"] = np.ascontiguousarray(inp["ada_w"][:L], dtype=f)
    m["ada_b"] = np.ascontiguousarray(inp["ada_b"][:L].reshape(L, 1, 6 * D), dtype=f)
    m["g1c"] = np.ascontiguousarray(inp["norm1_g"][:L].reshape(L, 8, 128).transpose(0, 2, 1), dtype=f)
    m["g2c"] = np.ascontiguousarray(inp["norm2_g"][:L].reshape(L, 8, 128).transpose(0, 2, 1), dtype=f)
    m["w_in"] = np.ascontiguousarray(inp["w_in"][:L], dtype=f)
    m["w_out"] = np.ascontiguousarray(inp["w_out"][:L], dtype=f)
    m["lamv"] = np.ascontiguousarray(inp["attn_lambda"][:L].reshape(L, 1, 256), dtype=f)
    m["sublng"] = np.ascontiguousarray(inp["attn_subln_g"][:L].reshape(L, 128, 1), dtype=f)
    mu = inp["rwkv_shift_mu"][:L]
    muc = np.zeros((L, 128, 14), f)
    muc[:, :, 0:12] = mu[:, 0:1536].reshape(L, 12, 128).transpose(0, 2, 1)
    muc[:, 0:64, 12] = mu[:, 1536:1600]
    muc[:, 0:96, 13] = mu[:, 1600:1696]
    m["mu_c"] = muc
    cols = []
    for k in ("rwkv_w0", "rwkv_a0", "rwkv_k_k", "rwkv_k_a", "rwkv_r_k", "rwkv_lnx_g", "rwkv_lnx_b"):
        cols.append(inp[k][:L].reshape(L, 4, 128).transpose(0, 2, 1))
    m["rw_cols"] = np.ascontiguousarray(np.concatenate(cols, axis=2), dtype=f)
    m["lw"] = np.ascontiguousarray(np.concatenate([inp["rwkv_w_up"][:L], inp["rwkv_a_up"][:L]], axis=1), dtype=f)
    m["gu"] = np.ascontiguousarray(inp["rwkv_g_up"][:L], dtype=f)
    m["wr"] = np.ascontiguousarray(np.concatenate([inp["moe_w_group"][:L], inp["moe_w_router"][:L]], axis=2), dtype=f)
    m["br"] = np.ascontiguousarray(np.concatenate([inp["moe_b_group"][:L], inp["moe_b_router"][:L]], axis=1).reshape(L, 1, 36), dtype=f)
    m["wg"] = np.ascontiguousarray(inp["moe_w_gate"][:L], dtype=f)
    m["wu"] = np.ascontiguousarray(inp["moe_w_up"][:L], dtype=f)
    m["wd"] = np.ascontiguousarray(inp["moe_w_down"][:L], dtype=f)
    m["fg"] = np.ascontiguousarray(inp["final_g"].reshape(1, D), dtype=f)
    for k, v in host_consts().items():
        m["k_" + k] = v
    return m


_CACHE = {}


def kernel(**inputs):
    inputs = {k: np.asarray(v) for k, v in inputs.items()}
    if "prog" not in _CACHE:
        _CACHE["prog"] = build_program()[0]
    nc = _CACHE["prog"]
    in_maps = [host_layout(inputs, b) for b in range(NCORES)]
    res = run_bass_kernel_spmd(nc, in_maps, core_ids=list(range(NCORES)))
    out = np.stack([np.asarray(res.results[b]["y"], dtype=np.float32) for b in range(NCORES)], axis=0)
    return out
```

```python
import math
from contextlib import ExitStack

import numpy as np
import concourse.bass as bass
import concourse.mybir as mybir
from concourse.bass_utils import run_bass_kernel_spmd

F32 = mybir.dt.float32
BF16 = mybir.dt.bfloat16
I32 = mybir.dt.int32
ALU = mybir.AluOpType
AF = mybir.ActivationFunctionType
AX = mybir.AxisListType

D = 1024
NCORES = 8
SEQ = 4096
DEPTH = 4
INW = 3232
NE = 32
DE = 256
LDC = 0.6065306597126334
ROPE_THETA = 500000.0

ENGS = ("pe", "act", "dve", "pool", "sp")
NDSEM = 12
SEM_ROLL = 30000


class Prog:
    def __init__(self, nc, same_engine_sync=True):
        self.nc = nc
        self.stack = None
        self.q = {e: [] for e in ENGS}
        self.cnt = {e: 0 for e in ENGS}
        self.seen = {e: {} for e in ENGS}
        self.last_w = {}
        self.readers = {}
        self.same = same_engine_sync
        self.esem = {}
        self.dsem = {}
        self.dcnt = {}
        self.dnext = {e: 0 for e in ENGS}
        self.nroll = 0
        self.n_inst = 0

    def setup(self, stack):
        self.root = stack
        self.stack = stack
        nc = self.nc
        for e in ENGS:
            self.esem[e] = stack.enter_context(nc.semaphore("es_" + e))
        for e in ("sp", "act", "pool"):
            self.dsem[e] = [stack.enter_context(nc.semaphore("ds_%s_%d" % (e, j))) for j in range(NDSEM)]
            self.dcnt[e] = [0] * NDSEM

    def sb(self, name, shape, dt=F32):
        self.uid = getattr(self, "uid", 0) + 1
        return self.stack.enter_context(self.nc.sbuf_tensor("%s_%d" % (name, self.uid), list(shape), dt))

    def ps(self, name, shape, dt=F32):
        return self.stack.enter_context(self.nc.psum_tensor(name, list(shape), dt))

    @staticmethod
    def key(x):
        if isinstance(x, (str, tuple)):
            return x
        t = getattr(x, "tensor", x)
        return t.name

    def _deps(self, eng, reads, writes, tokname=None):
        deps = {}
        tokname = tokname or eng

        def add(tok):
            if tok is None:
                return
            s, v, e = tok
            if e == tokname and (eng == "pe" or not self.same):
                return
            if deps.get(id(s), (None, 0))[1] < v:
                deps[id(s)] = (s, v)

        rk = [self.key(r) for r in reads]
        wk = [self.key(w) for w in writes]
        for k in rk:
            if isinstance(k, tuple) and k[0] == "bank" and k not in wk:
                wk.append(k)
        for k in rk:
            add(self.last_w.get(k))
        for k in wk:
            add(self.last_w.get(k))
            for t in self.readers.get(k, ()):
                add(t)
        waits = []
        for sid, (s, v) in deps.items():
            if self.seen[eng].get(sid, 0) < v:
                self.seen[eng][sid] = v
                waits.append((s, v))
        return rk, wk, waits

    def _commit(self, rk, wk, tok):
        for k in wk:
            self.last_w[k] = tok
            self.readers[k] = []
        for k in rk:
            if k not in wk:
                lst = self.readers.setdefault(k, [])
                lst[:] = [t for t in lst if t[0] is not tok[0]]
                lst.append(tok)

    def op(self, eng, fn, reads=(), writes=(), cls=None):
        tokname = eng if cls is None else eng + cls
        rk, wk, waits = self._deps(eng, reads, writes, tokname)
        if self.cnt[eng] >= SEM_ROLL:
            self.nroll += 1
            self.esem[eng] = self.root.enter_context(self.nc.semaphore("es_%s_%d" % (eng, self.nroll)))
            self.cnt[eng] = 0
        self.cnt[eng] += 1
        tok = (self.esem[eng], self.cnt[eng], tokname)
        self.q[eng].append((waits, fn, self.esem[eng], 1))
        self._commit(rk, wk, tok)
        self.n_inst += 1
        return tok

    def dma(self, eng, out, in_, reads=None, writes=None, **kw):
        reads = [in_] if reads is None else reads
        writes = [out] if writes is None else writes
        rk, wk, waits = self._deps(eng, reads, writes)
        j = self.dnext[eng]
        self.dnext[eng] = (j + 1) % NDSEM
        s = self.dsem[eng][j]
        prev = self.dcnt[eng][j]
        if prev > 0 and self.seen[eng].get(id(s), 0) < prev:
            self.seen[eng][id(s)] = prev
            waits.append((s, prev))
        self.dcnt[eng][j] = prev + 16
        tok = (s, prev + 16, "dma_" + eng)
        self.q[eng].append((waits, lambda e: e.dma_start(out=out, in_=in_, **kw), s, 16))
        self._commit(rk, wk, tok)
        self.n_inst += 1
        return tok

    def barrier(self):
        toks = [(self.esem[e], self.cnt[e]) for e in ENGS if self.cnt[e] > 0]
        for e in ("sp", "act", "pool"):
            for j in range(NDSEM):
                if self.dcnt[e][j] > 0:
                    toks.append((self.dsem[e][j], self.dcnt[e][j]))
        for e in ENGS:
            waits = []
            for s, v in toks:
                if s is self.esem[e]:
                    continue
                if self.seen[e].get(id(s), 0) < v:
                    self.seen[e][id(s)] = v
                    waits.append((s, v))
            if waits:
                self.q[e].append((waits, None, None, 0))

    def finish_wait(self, eng, keys):
        rk, wk, waits = self._deps(eng, keys, [])
        self.q[eng].append((waits, None, None, 0))

    def emit(self):
        nc = self.nc
        names = {"pe": "tensor", "act": "scalar", "dve": "vector", "pool": "gpsimd", "sp": "sync"}
        with nc.Block() as block:
            for e in ENGS:
                lst = self.q[e]
                if not lst:
                    continue

                def body(engobj, lst=lst):
                    for waits, fn, sem, inc in lst:
                        for s, v in waits:
                            engobj.wait_ge(s, v)
                        if fn is not None:
                            fn(engobj).then_inc(sem, inc)

                getattr(block, names[e])(body)

    def mm(self, out, lhsT, rhs, start=True, stop=True, r=None, w=None):
        kp = lhsT.partition_size()
        cls = None if kp > 64 else "_%d_%d" % (lhsT.base_partition(), 32 if kp <= 32 else 64)
        return self.op("pe", lambda e: e.matmul(out, lhsT=lhsT, rhs=rhs, start=start, stop=stop),
                       r if r is not None else [lhsT, rhs], w if w is not None else [out], cls=cls)

    def tr(self, out, in_, ident, r=None, w=None):
        kp = in_.partition_size()
        cls = None if kp > 64 else "_%d_%d" % (in_.base_partition(), 32 if kp <= 32 else 64)
        return self.op("pe", lambda e: e.transpose(out, in_, ident),
                       r if r is not None else [in_, ident], w if w is not None else [out], cls=cls)

    def act(self, out, in_, func, bias=None, scale=None, accum=None, r=None, w=None, eng="act"):
        kw = {}
        rr = [in_]
        if bias is not None:
            kw["bias"] = bias
            if not isinstance(bias, (int, float)):
                rr.append(bias)
        if scale is not None:
            kw["scale"] = scale
            if not isinstance(scale, (int, float)):
                rr.append(scale)
        ww = [out]
        if accum is not None:
            kw["accum_out"] = accum
            ww.append(accum)
        return self.op("act", lambda e: e.activation(out=out, in_=in_, func=func, **kw),
                       r if r is not None else rr, w if w is not None else ww)

    def tt(self, eng, out, in0, in1, op, r=None, w=None):
        return self.op(eng, lambda e: e.tensor_tensor(out=out, in0=in0, in1=in1, op=op),
                       r if r is not None else [in0, in1], w if w is not None else [out])

    def ts(self, eng, out, in0, s1, op0, s2=None, op1=None, r=None, w=None):
        rr = [in0]
        for s in (s1, s2):
            if s is not None and not isinstance(s, (int, float)):
                rr.append(s)
        if op1 is None:
            fn = lambda e: e.tensor_scalar(out=out, in0=in0, scalar1=s1, scalar2=None, op0=op0)
        else:
            fn = lambda e: e.tensor_scalar(out=out, in0=in0, scalar1=s1, scalar2=s2, op0=op0, op1=op1)
        return self.op(eng, fn, r if r is not None else rr, w if w is not None else [out])

    def stt(self, eng, out, in0, scalar, in1, op0, op1, r=None, w=None):
        rr = [in0, in1]
        if not isinstance(scalar, (int, float)):
            rr.append(scalar)
        return self.op(eng, lambda e: e.scalar_tensor_tensor(out=out, in0=in0, scalar=scalar, in1=in1, op0=op0, op1=op1),
                       r if r is not None else rr, w if w is not None else [out])

    def cp(self, eng, out, in_, r=None, w=None):
        if eng == "act":
            return self.act(out, in_, AF.Copy, r=r, w=w)
        return self.op(eng, lambda e: e.tensor_copy(out=out, in_=in_),
                       r if r is not None else [in_], w if w is not None else [out])

    def memset(self, eng, out, val, w=None):
        return self.op(eng, lambda e: e.memset(out, val), [], w if w is not None else [out])

    def recip(self, out, in_, r=None, w=None):
        return self.op("dve", lambda e: e.reciprocal(out=out, in_=in_),
                       r if r is not None else [in_], w if w is not None else [out])

    def red(self, out, in_, op, r=None, w=None):
        return self.op("dve", lambda e: e.tensor_reduce(out=out, in_=in_, axis=AX.X, op=op),
                       r if r is not None else [in_], w if w is not None else [out])


def host_consts():
    c = {}
    c["idn"] = np.eye(128, dtype=np.float32)
    rp = np.zeros((128, 128), np.float32)
    for f in range(128):
        d = f % 64
        if d < 8:
            rp[f + 8, f] = 1.0
        elif d < 16:
            rp[f - 8, f] = 1.0
    c["rperm"] = rp
    invf = (ROPE_THETA ** (-np.arange(0, 16, 2, dtype=np.float32) / np.float32(16))).astype(np.float32)
    col = np.zeros((128, 4), np.float32)
    for f in range(128):
        d = f % 64
        if d < 16:
            col[f, 0] = invf[d % 8]
            col[f, 1] = -1.0 if d < 8 else 1.0
    c["ropecol"] = col
    bd = np.zeros((128, 128), np.float32)
    bd[:64, :64] = 1.0
    bd[64:, 64:] = 1.0
    c["bd64"] = bd
    c["ones"] = np.ones((128, 128), np.float32)
    s = np.arange(64)[:, None]
    t = np.arange(64)[None, :]
    m = np.zeros((64, 192), np.float32)
    m[:, 0:64] = (s < t)
    m[:, 64:128] = (s <= t)
    m[:, 128:192] = (t < s)
    c["masks"] = m
    rm = np.ones((128, 512), np.float32)
    rm[:, ::64] = 0.0
    c["rmask"] = rm
    c["idn64x8"] = np.tile(np.eye(64, dtype=np.float32), (1, 8))
    return c


CONST_SHAPES = {"idn": [128, 128], "rperm": [128, 128], "ropecol": [128, 4], "bd64": [128, 128],
                "ones": [128, 128], "masks": [64, 192], "rmask": [128, 512], "idn64x8": [64, 512]}


def lambda_init_of(layer):
    return 0.8 - 0.6 * math.exp(-0.3 * layer)


def build_program(S=SEQ, L=DEPTH, dbg=False, same_sync=True, do_a=True, do_b=True, do_c=True, GB=256, bstop=99):
    assert S % 512 == 0
    NG = S // 512
    NT = S // 128
    SG = min(2048, S)
    NSG = S // SG
    NTS = SG // 128
    NGB = S // GB
    NCB = GB // 64
    nc = bass.Bass("TRN2", target_bir_lowering=False)
    P = Prog(nc, same_engine_sync=same_sync)

    def din(name, shape, dt=F32):
        return nc.dram_tensor(name, list(shape), dt, kind="ExternalInput").ap()

    def dscr(name, shape, dt=F32):
        return nc.dram_tensor(name, list(shape), dt).ap()

    x_in = din("x", [S, D])
    pos_in = din("pos", [1, S], I32)
    c_in = din("c_t", [128, 8])
    ada_w = din("ada_w", [L, D, 6 * D])
    ada_b = din("ada_b", [L, 1, 6 * D])
    g1c = din("g1c", [L, 128, 8])
    g2c = din("g2c", [L, 128, 8])
    w_in = din("w_in", [L, D, INW])
    w_out = din("w_out", [L, D, D])
    lamv = din("lamv", [L, 1, 256])
    sublng = din("sublng", [L, 128, 1])
    mu_c = din("mu_c", [L, 128, 14])
    rw_cols = din("rw_cols", [L, 128, 28])
    lw_in = din("lw", [L, 64, 512])
    gu_in = din("gu", [L, 96, 512])
    wr_in = din("wr", [L, D, 36])
    br_in = din("br", [L, 1, 36])
    wg_in = din("wg", [L, NE, D, DE])
    wu_in = din("wu", [L, NE, D, DE])
    wd_in = din("wd", [L, NE, DE, D])
    fg_in = din("fg", [1, D])
    cst = {k: din("k_" + k, v) for k, v in CONST_SHAPES.items()}
    y_out = nc.dram_tensor("y", [S, D], F32, kind="ExternalOutput").ap()

    xcur = dscr("xcur", [S, D])
    x1_d = dscr("x1_d", [S, D])
    hT_d = dscr("hT_d", [128, 8, S], BF16)
    ao_d = dscr("ao_d", [128, 4, S], BF16)
    tabC_d = dscr("tabC_d", [128, S])
    tabS_d = dscr("tabS_d", [128, S])
    dbg_outs = {}
    if dbg:
        dbg_outs["d_ao"] = nc.dram_tensor("d_ao", [128, 4, S], BF16, kind="ExternalOutput").ap()
        dbg_outs["d_x1"] = nc.dram_tensor("d_x1", [S, D], F32, kind="ExternalOutput").ap()
        dbg_outs["d_x2"] = nc.dram_tensor("d_x2", [S, D], F32, kind="ExternalOutput").ap()

    root = ExitStack()
    with root:
        P.setup(root)
        DB = [P.ps("db%d" % i, [128, 1024]) for i in range(4)]

        def bank(i):
            return DB[i // 2][:, (i % 2) * 512:(i % 2) * 512 + 512]

        def bk(i):
            return ("bank", i)

        idn = P.sb("idn", [128, 128])
        ones = P.sb("ones", [128, 128])
        ones_bf = P.sb("ones_bf", [128, 128], BF16)
        rperm = P.sb("rperm", [128, 128])
        bd64 = P.sb("bd64", [128, 128])
        masks = P.sb("masks", [64, 192])
        rmask = P.sb("rmask", [128, 512])
        idn8 = P.sb("idn8", [64, 512])
        ropecol = P.sb("ropecol", [128, 4])
        onesm = P.sb("onesm", [128, 128])
        cact = P.sb("cact", [128, 8])
        fgbc = P.sb("fgbc", [128, D])
        for t, k in ((idn, "idn"), (ones, "ones"), (rperm, "rperm"), (bd64, "bd64"), (masks, "masks"),
                     (rmask, "rmask"), (idn8, "idn64x8"), (ropecol, "ropecol")):
            P.dma("sp", t[:], cst[k])
        P.cp("dve", ones_bf[:], ones[:])
        P.ts("dve", onesm[:], ones[:], 1.0 / 128.0, ALU.mult)
        P.dma("sp", cact[:], c_in)
        P.act(cact[:], cact[:], AF.Silu)
        P.dma("sp", fgbc[:], fg_in.partition_broadcast(128))
        A1 = P.sb("A1", [128, 8]); B1 = P.sb("B1", [128, 8])
        A2 = P.sb("A2", [128, 8]); B2 = P.sb("B2", [128, 8])
        G1bc = P.sb("G1bc", [128, D]); G2bc = P.sb("G2bc", [128, D])
        lam = P.sb("lam", [1, 4])
        sgcol = P.sb("sgcol", [128, 1])

        def b3(ap, a, b):
            return ap.unsqueeze(2).to_broadcast([ap.shape[0], a, b])

        def v3(ap, a):
            return ap.rearrange("p (a b) -> p a b", a=a)

        def rms_rstd(xtt, junk, stt_):
            P.memset("pool", stt_[:, 0:1], 0.0, w=[stt_])
            P.act(junk[:], xtt[:], AF.Square, accum=stt_[:, 0:1], r=[xtt, stt_], w=[junk, stt_])
            P.ts("dve", stt_[:, 1:2], stt_[:, 0:1], 1.0 / D, ALU.mult, 1e-6, ALU.add, r=[stt_], w=[stt_])
            P.act(stt_[:, 2:3], stt_[:, 1:2], AF.Sqrt, r=[stt_], w=[stt_])
            P.recip(stt_[:, 3:4], stt_[:, 2:3], r=[stt_], w=[stt_])

        def norm_transpose(xtt, xhh, stt_, Acol, Bcol, htmp, out3, out_eng="pool"):
            rms_rstd(xtt, xhh, stt_)
            P.ts("pool", xhh[:], xtt[:], stt_[:, 3:4], ALU.mult, r=[xtt, stt_], w=[xhh])
            for k in range(8):
                P.tr(DB[0][:, k * 128:(k + 1) * 128], xhh[:, k * 128:(k + 1) * 128], idn[:], w=[bk(k // 4)])
            pv = v3(DB[0][:], 8)
            P.tt("dve", htmp[:], pv, b3(Acol[:], 8, 128), ALU.mult, r=[bk(0), bk(1), Acol], w=[htmp])
            P.tt(out_eng, out3, htmp[:], b3(Bcol[:], 8, 128), ALU.add, r=[htmp, Bcol], w=[out3])

        with ExitStack() as sc:
            P.stack = sc
            posi = P.sb("posi", [128, S], I32)
            ang = P.sb("ang", [128, S])
            kf_ = P.sb("kf_", [128, S])
            ki_ = P.sb("ki_", [128, S], I32)
            kg_ = P.sb("kg_", [128, S])
            P.dma("sp", posi[:], pos_in.partition_broadcast(128))
            P.cp("dve", ang[:], posi[:])
            P.ts("pool", ang[:], ang[:], ropecol[:, 0:1], ALU.mult)
            for which, shift, dst in (("sin", 0.5, tabS_d), ("cos", 0.75, tabC_d)):
                P.ts("dve", kf_[:], ang[:], 1.0 / (2.0 * math.pi), ALU.mult, shift, ALU.add)
                P.cp("dve", ki_[:], kf_[:])
                P.cp("pool", kg_[:], ki_[:])
                P.tt("dve", kf_[:], kf_[:], kg_[:], ALU.subtract)
                P.ts("pool", kg_[:], kf_[:], 0.0, ALU.is_lt)
                P.tt("dve", kf_[:], kf_[:], kg_[:], ALU.add)
                P.ts("dve", kf_[:], kf_[:], 2.0 * math.pi, ALU.mult, -math.pi, ALU.add)
                P.ts("dve", kf_[:], kf_[:], 3.14159, ALU.min, -3.14159, ALU.max)
                P.act(kf_[:], kf_[:], AF.Sin)
                if which == "sin":
                    P.ts("dve", kf_[:], kf_[:], ropecol[:, 1:2], ALU.mult)
                P.dma("sp", dst, kf_[:])
        P.barrier()

        def modulation(l):
            linit = lambda_init_of(l)
            awb = [P.sb("awb%d" % i, [128, 8, 512]) for i in range(2)]
            brow = P.sb("brow", [1, 6 * D])
            mrow = P.sb("mrow", [1, 512])
            colt = P.sb("colt", [128, 48])
            gc1 = P.sb("gc1", [128, 8]); gc2 = P.sb("gc2", [128, 8])
            lrow = P.sb("lrow", [1, 256]); ltmp = P.sb("ltmp", [1, 128]); lsum = P.sb("lsum", [1, 2])
            sgl = P.sb("sgl", [128, 1])
            P.dma("sp", brow[:], ada_b[l])
            P.dma("sp", gc1[:], g1c[l]); P.dma("sp", gc2[:], g2c[l])
            awv = ada_w[l].rearrange("(k p) n -> p k n", p=128)
            for cc in range(12):
                wb = awb[cc % 2]
                P.dma("sp" if cc % 2 == 0 else "act", wb[:], awv[:, :, cc * 512:(cc + 1) * 512])
                for k in range(8):
                    P.mm(bank(0)[0:1, :], cact[:, k:k + 1], wb[:, k, :], start=(k == 0), stop=(k == 7), w=[bk(0)])
                P.tt("dve", mrow[:], bank(0)[0:1, :], brow[:, cc * 512:(cc + 1) * 512], ALU.add,
                     r=[bk(0), brow], w=[mrow])
                if cc in (4, 5, 10, 11):
                    P.mm(bank(1), ones[0:1, :], mrow[:], w=[bk(1)])
                    dstg = G1bc if cc < 6 else G2bc
                    off = (cc % 2) * 512
                    P.cp("act", dstg[:, off:off + 512], bank(1), r=[bk(1)], w=[dstg])
                else:
                    for j in range(4):
                        P.mm(bank(1)[:, j:j + 1], mrow[0:1, j * 128:(j + 1) * 128], ones[0:1, 0:1], w=[bk(1)])
                    P.cp("act", colt[:, cc * 4:cc * 4 + 4], bank(1)[:, 0:4], r=[bk(1)], w=[colt])
            P.stt("dve", A1[:], colt[:, 8:16], 1.0, gc1[:], ALU.add, ALU.mult)
            P.cp("dve", B1[:], colt[:, 0:8])
            P.stt("dve", A2[:], colt[:, 32:40], 1.0, gc2[:], ALU.add, ALU.mult)
            P.cp("dve", B2[:], colt[:, 24:32])
            P.dma("sp", lrow[:], lamv[l])
            lv = lrow[:].rearrange("o (a b d) -> o a b d", a=2, b=2)
            P.tt("dve", v3(ltmp[:], 2), lv[:, :, 0, :], lv[:, :, 1, :], ALU.mult, r=[lrow], w=[ltmp])
            P.red(lsum[:], v3(ltmp[:], 2), ALU.add, r=[ltmp], w=[lsum])
            P.act(lsum[:], lsum[:], AF.Exp)
            P.tt("dve", lam[:, 0:1], lsum[:, 0:1], lsum[:, 1:2], ALU.subtract, r=[lsum], w=[lam])
            P.ts("dve", lam[:, 0:1], lam[:, 0:1], float(linit), ALU.add)
            P.dma("sp", sgl[:], sublng[l])
            P.ts("dve", sgcol[:], sgl[:], float(1.0 - linit), ALU.mult)

        def pass_a(l):
            xsrc = x_in if l == 0 else xcur
            wqkv = P.sb("wqkv", [128, 8, 1536], BF16)
            kc = P.sb("kc", [128, 4, S], BF16)
            vc = P.sb("vc", [128, NT, 512], BF16)
            xt = [P.sb("a_xt%d" % i, [128, D]) for i in range(2)]
            xh = P.sb("a_xh", [128, D])
            st = [P.sb("a_st%d" % i, [128, 4]) for i in range(2)]
            htmp = P.sb("a_htmp", [128, 8, 128])
            hT = [P.sb("a_hT%d" % i, [128, 8, 512], BF16) for i in range(2)]
            tC = [P.sb("a_tC%d" % i, [128, 512]) for i in range(2)]
            tS = [P.sb("a_tS%d" % i, [128, 512]) for i in range(2)]
            qf = [P.sb("a_qf%d" % i, [128, 512]) for i in range(2)]
            t1 = [P.sb("a_t1%d" % i, [128, 512]) for i in range(2)]
            t2 = [P.sb("a_t2%d" % i, [128, 512]) for i in range(2)]
            qr = [P.sb("a_qr%d" % i, [128, 4, 512], BF16) for i in range(2)]
            PT = [P.sb("a_PT%d" % i, [128, 512], BF16) for i in range(4)]
            rs = P.sb("a_rs", [1, 512])
            bcs = P.sb("a_bcs", [128, 512])
            om = [P.sb("a_om%d" % i, [128, 512]) for i in range(2)]
            osq = P.sb("a_osq", [128, 512])
            rstd = P.sb("a_rstd", [128, 512])
            aoT = [P.sb("a_aoT%d" % i, [128, 4, 512], BF16) for i in range(2)]
            wv = w_in[l].rearrange("(k p) n -> p k n", p=128)
            P.dma("pool", wqkv[:], wv[:, :, 0:1536])
            for g in range(NG):
                hTg = hT[g % 2]
                for t in range(4):
                    ti = g * 4 + t
                    xtt = xt[ti % 2]
                    P.dma("sp", xtt[:], xsrc[ti * 128:(ti + 1) * 128, :], reads=[("xcur", ti)])
                    norm_transpose(xtt, xh, st[ti % 2], A1, B1, htmp, hTg[:, :, t * 128:(t + 1) * 128])
                P.dma("sp", hT_d[:, :, g * 512:(g + 1) * 512], hTg[:], writes=[("hT_d", g)])
                tCg = tC[g % 2]; tSg = tS[g % 2]
                P.dma("act", tCg[:], tabC_d[:, g * 512:(g + 1) * 512])
                P.dma("act", tSg[:], tabS_d[:, g * 512:(g + 1) * 512])
                qrg = qr[g % 2]
                for c in range(8):
                    pb = 2 + (c % 2)
                    sb2 = 4 + (c % 2)
                    for k in range(8):
                        P.mm(bank(pb), wqkv[:, k, c * 128:(c + 1) * 128], hTg[:, k, :], start=(k == 0),
                             stop=(k == 7), w=[bk(pb)])
                    qff = qf[c % 2]; t1c = t1[c % 2]; t2c = t2[c % 2]
                    P.cp("act", qff[:], bank(pb), r=[bk(pb)], w=[qff])
                    P.mm(bank(sb2), rperm[:], qff[:], w=[bk(sb2)])
                    P.tt("pool", t1c[:], qff[:], tCg[:], ALU.mult)
                    P.tt("dve", t2c[:], bank(sb2), tSg[:], ALU.mult, r=[bk(sb2), tSg], w=[t2c])
                    if c < 4:
                        P.tt("pool", qrg[:, c, :], t1c[:], t2c[:], ALU.add, w=[qrg])
                    else:
                        P.tt("pool", kc[:, c - 4, g * 512:(g + 1) * 512], t1c[:], t2c[:], ALU.add, w=[("kc", g)])
                for t in range(4):
                    pb = 2 + (t % 2)
                    for k in range(8):
                        P.mm(bank(pb), hTg[:, k, t * 128:(t + 1) * 128], wqkv[:, k, 1024:1536], start=(k == 0),
                             stop=(k == 7), w=[bk(pb)])
                    P.cp("act", vc[:, g * 4 + t, :], bank(pb), r=[bk(pb)], w=[("vc", g)])
                aog = aoT[g % 2]
                nkt = 4 * g + 4
                steps = [(h, m, j) for h in range(4) for m in range(2) for j in range(nkt)]
                SBK = [4, 5, 2, 3]

                def qk(i):
                    h, m, j = steps[i]
                    prt = slice(64 * m, 64 * m + 64)
                    jl = j - 4 * g
                    qs = 128 * jl if jl > 0 else 0
                    sb_ = SBK[i % 4]
                    PTt = PT[i % 4]
                    P.mm(bank(sb_)[:, qs:512], kc[prt, h, j * 128:(j + 1) * 128], qrg[prt, h, qs:512],
                         r=[("kc", j // 4), qrg], w=[bk(sb_)])
                    P.act(PTt[:, qs:512], bank(sb_)[:, qs:512], AF.Exp, scale=0.125, r=[bk(sb_)], w=[PTt])
                    if jl >= 0:
                        P.memset("pool", PTt[64:128, qs:qs + 64], 0.0, w=[PTt])

                def pv(i):
                    h, m, j = steps[i]
                    jl = j - 4 * g
                    qs = 128 * jl if jl > 0 else 0
                    PTt = PT[i % 4]
                    P.mm(bank(6)[:, qs:512], vc[:, j, h * 128:(h + 1) * 128], PTt[:, qs:512],
                         start=(j == 0), stop=(j == nkt - 1), r=[("vc", j // 4), PTt], w=[bk(6)])
                    P.mm(bank(7)[0:1, qs:512], ones_bf[:, 0:1], PTt[:, qs:512],
                         start=(j == 0), stop=(j == nkt - 1), r=[PTt], w=[bk(7)])
                    if j != nkt - 1:
                        return
                    P.recip(rs[:], bank(7)[0:1, :], r=[bk(7)], w=[rs])
                    if m == 1:
                        P.ts("dve", rs[:], rs[:], lam[0:1, 0:1], ALU.mult)
                    P.mm(bank(0), ones[0:1, :], rs[:], w=[bk(0)])
                    P.cp("act", bcs[:], bank(0), r=[bk(0)], w=[bcs])
                    P.tt("dve", om[m][:], bank(6), bcs[:], ALU.mult, r=[bk(6), bcs], w=[om[m]])
                    if m == 0:
                        return
                    P.tt("pool", om[0][:], om[0][:], om[1][:], ALU.subtract)
                    P.tt("pool", osq[:], om[0][:], om[0][:], ALU.mult)
                    P.mm(bank(1), onesm[:], osq[:], w=[bk(1)])
                    P.ts("dve", rstd[:], bank(1), 1e-5, ALU.add, r=[bk(1)], w=[rstd])
                    P.act(rstd[:], rstd[:], AF.Sqrt)
                    P.recip(rstd[:], rstd[:])
                    P.tt("dve", om[0][:], om[0][:], rstd[:], ALU.mult)
                    P.ts("pool", aog[:, h, :], om[0][:], sgcol[:, 0:1], ALU.mult, r=[om[0], sgcol], w=[aog])

                LOOK = 2
                for i in range(len(steps) + LOOK):
                    if i < len(steps):
                        qk(i)
                    if i >= LOOK:
                        pv(i - LOOK)
                P.dma("sp", ao_d[:, :, g * 512:(g + 1) * 512], aog[:], writes=[("ao_d", g)])

        def pass_b(l):
            xsrc = x_in if l == 0 else xcur
            wrw = P.sb("wrw", [128, 8, 1696], BF16)
            wout = P.sb("wout", [128, 8, D], BF16)
            LW = P.sb("LW", [64, 512]); GU = P.sb("GU", [96, 512])
            muc = P.sb("muc", [128, 14]); rwc = P.sb("rwc", [128, 28]); omka = P.sb("omka", [128, 4])
            ST = P.sb("ST", [128, 4, 64])
            bnd = P.sb("bnd", [128, 14])
            hTb = [P.sb("b_hT%d" % i, [128, 8, GB], BF16) for i in range(2)]
            pcb = [P.sb("b_pc%d" % i, [128, GB + 1]) for i in range(2)]
            lt = P.sb("b_lt", [128, GB])
            PMr = P.sb("PMr", [128, GB]); PMk = P.sb("PMk", [128, GB]); PMv = P.sb("PMv", [128, 4, GB])
            PL1 = P.sb("PL1", [64, GB]); PL2 = P.sb("PL2", [96, GB])
            T = [P.sb("b_T%d" % i, [128, GB]) for i in range(8)]
            gT = [P.sb("b_gT%d" % i, [128, GB]) for i in range(4)]
            bonusT = [P.sb("b_bo%d" % i, [128, GB]) for i in range(4)]
            ARt = P.sb("ARt", [128, 4, NCB, 2, 64], BF16)
            BKt = P.sb("BKt", [128, 4, NCB, 2, 64])
            BKb = P.sb("BKb", [128, 4, NCB, 2, 64], BF16)
            STb = P.sb("STb", [128, 4, 64], BF16)
            TT32 = P.sb("TT32", [64, 8, 64])
            PC = P.sb("PC", [128, 4, NCB])
            Vtok = [P.sb("Vtok%d" % i, [64, 512], BF16) for i in range(2)]
            Btok = [P.sb("Btok%d" % i, [64, 512], BF16) for i in range(2)]
            Ktok = [P.sb("Ktok%d" % i, [64, 512], BF16) for i in range(2)]
            M1 = P.sb("M1", [64, 8, 128], BF16); M2 = P.sb("M2", [64, 8, 128], BF16)
            X = P.sb("Xm", [64, 8, 64], BF16); YT = P.sb("YT", [64, 8, 128], BF16)
            X0s = P.sb("X0s", [64, 512], BF16); Us = P.sb("Us", [64, 512], BF16)
            Ys = P.sb("Ys", [64, 512]); Ysq = P.sb("Ysq", [64, 512]); gs = P.sb("gs", [64, 16])
            yT = P.sb("yT", [128, 4, GB])
            fo = P.sb("fo", [128, GB])
            catT = [P.sb("catT%d" % i, [128, 8, GB], BF16) for i in range(2)]
            xr = [P.sb("b_xr%d" % i, [128, D]) for i in range(2)]
            mix = [P.sb("b_mix%d" % i, [128, D]) for i in range(2)]
            wv = w_in[l].rearrange("(k p) n -> p k n", p=128)
            P.dma("pool", wrw[:], wv[:, :, 1536:INW])
            P.dma("pool", wout[:], w_out[l].rearrange("(k p) n -> p k n", p=128))
            P.dma("sp", LW[:], lw_in[l]); P.dma("sp", GU[:], gu_in[l])
            P.dma("sp", muc[:], mu_c[l]); P.dma("sp", rwc[:], rw_cols[l])
            P.ts("dve", omka[:], rwc[:, 12:16], -1.0, ALU.mult, 1.0, ALU.add)
            P.memset("pool", ST[:], 0.0)
            P.memset("pool", STb[:], 0.0)
            P.memset("pool", bnd[:], 0.0)
            rmk = rmask[:, 0:GB]

            def proj_chunk(hTg, c, dst, nchunk):
                if c < 12:
                    cols = slice(c * 128, (c + 1) * 128); M = 128
                elif c == 12:
                    cols = slice(1536, 1600); M = 64
                else:
                    cols = slice(1600, 1696); M = 96
                pb = nchunk % 2
                pc = pcb[nchunk % 2]
                for k in range(8):
                    P.mm(bank(pb)[0:M, 0:GB], wrw[:, k, cols], hTg[:, k, :], start=(k == 0), stop=(k == 7), w=[bk(pb)])
                P.cp("act", pc[0:M, 1:GB + 1], bank(pb)[0:M, 0:GB], r=[bk(pb)], w=[pc])
                P.cp("pool", pc[0:M, 0:1], bnd[0:M, c:c + 1], r=[bnd], w=[pc])
                P.tt("pool", lt[0:M, :], pc[0:M, 0:GB], pc[0:M, 1:GB + 1], ALU.subtract, r=[pc], w=[lt])
                P.stt("dve", dst, lt[0:M, :], muc[0:M, c:c + 1], pc[0:M, 1:GB + 1], ALU.mult, ALU.add,
                      r=[lt, muc, pc], w=[dst])
                P.cp("pool", bnd[0:M, c:c + 1], pc[0:M, GB:GB + 1], r=[pc], w=[bnd])

            if bstop == 0:
                return
            nch = 0
            for g in range(NGB):
                hTg = hTb[g % 2]
                P.dma("sp", hTg[:], hT_d[:, :, g * GB:(g + 1) * GB], reads=[("hT_d", (g * GB) // 512)])
                proj_chunk(hTg, 12, PL1[:], nch); nch += 1
                proj_chunk(hTg, 13, PL2[:], nch); nch += 1
                if bstop == 1:
                    return
                P.act(PL1[0:32, :], PL1[0:32, :], AF.Tanh)
                P.act(PL2[:], PL2[:], AF.Sigmoid)
                for hp in range(4):
                    proj_chunk(hTg, hp, PMr[:], nch); nch += 1
                    proj_chunk(hTg, 4 + hp, PMk[:], nch); nch += 1
                    proj_chunk(hTg, 8 + hp, PMv[:, hp, :], nch); nch += 1
                    cs = slice(hp * 128, (hp + 1) * 128)

                    def col(pi, hp=hp):
                        return rwc[:, pi * 4 + hp:pi * 4 + hp + 1]
                    ta, tb_, tc_, td, te, tf, tg, th = T
                    r_ = PMr[:]; k_ = PMk[:]; v_ = PMv[:, hp, :]
                    P.mm(bank(2)[:, 0:GB], LW[0:32, cs], PL1[0:32, :], w=[bk(2)])
                    P.act(ta[:], bank(2)[:, 0:GB], AF.Sigmoid, bias=col(0), r=[bk(2), rwc], w=[ta])
                    P.op("dve", lambda e, o=tb_, a=ta: e.tensor_tensor_scan(out=o[:], data0=rmk, data1=a[:], initial=0.0,
                                                                          op0=ALU.mult, op1=ALU.add), [rmask, ta], [tb_])
                    P.act(tc_[:], tb_[:], AF.Exp, scale=-LDC)
                    P.act(td[:], tb_[:], AF.Exp, scale=LDC)
                    P.tt("pool", te[:], tb_[:], ta[:], ALU.subtract)
                    P.act(te[:], te[:], AF.Exp, scale=-LDC)
                    P.mm(bank(3)[:, 0:GB], LW[32:64, cs], PL1[32:64, :], w=[bk(3)])
                    P.act(tf[:], bank(3)[:, 0:GB], AF.Sigmoid, bias=col(1), r=[bk(3), rwc], w=[tf])
                    P.mm(bank(2)[:, 0:GB], GU[0:96, cs], PL2[0:96, :], w=[bk(2)])
                    P.cp("act", gT[hp][:], bank(2)[:, 0:GB], r=[bk(2)], w=[gT[hp]])
                    P.ts("pool", tg[:], k_, col(2), ALU.mult, r=[PMk, rwc], w=[tg])
                    P.tt("pool", th[:], tg[:], tg[:], ALU.mult)
                    P.mm(bank(3)[:, 0:GB], bd64[:], th[:], w=[bk(3)])
                    P.act(th[:], bank(3)[:, 0:GB], AF.Sqrt, r=[bk(3)], w=[th])
                    P.ts("dve", th[:], th[:], 1e-12, ALU.max)
                    P.recip(th[:], th[:])
                    P.tt("pool", tg[:], tg[:], th[:], ALU.mult)
                    P.ts("dve", th[:], tf[:], col(3), ALU.mult, omka[:, hp:hp + 1], ALU.add, r=[tf, rwc, omka], w=[th])
                    P.tt("pool", th[:], k_, th[:], ALU.mult, r=[PMk, th], w=[th])
                    P.tt("pool", ta[:], tg[:], tf[:], ALU.mult)
                    P.stt("dve", tb_[:], r_, col(4), th[:], ALU.mult, ALU.mult, r=[PMr, rwc, th], w=[tb_])
                    P.mm(bank(2)[:, 0:GB], bd64[:], tb_[:], w=[bk(2)])
                    P.tt("dve", bonusT[hp][:], bank(2)[:, 0:GB], v_, ALU.mult, r=[bk(2), PMv], w=[bonusT[hp]])
                    P.stt("dve", ARt[:, hp, :, 0, :], v3(tg[:], NCB), -1.0, v3(te[:], NCB), ALU.mult, ALU.mult,
                          r=[tg, te], w=[("ARt", hp)])
                    P.tt("pool", ARt[:, hp, :, 1, :], v3(r_, NCB), v3(tc_[:], NCB), ALU.mult, r=[PMr, tc_], w=[("ARt", hp)])
                    P.tt("pool", BKt[:, hp, :, 0, :], v3(ta[:], NCB), v3(td[:], NCB), ALU.mult, r=[ta, td], w=[("BKt", hp)])
                    P.tt("dve", BKt[:, hp, :, 1, :], v3(th[:], NCB), v3(td[:], NCB), ALU.mult, r=[th, td], w=[("BKt", hp)])
                    P.cp("pool", PC[:, hp, :], v3(tc_[:], NCB)[:, :, 63], r=[tc_], w=[PC])
                    P.cp("pool", BKb[:, hp, :, :, :], BKt[:, hp, :, :, :], r=[("BKt", hp)], w=[("BKb", hp)])

                if bstop == 2:
                    return
                ARk = [("ARt", hp) for hp in range(4)]
                BKk = [("BKt", hp) for hp in range(4)]
                for c in range(NCB):
                    cc = g * NCB + c
                    Vt = Vtok[cc % 2]; Bt = Btok[cc % 2]; Kt = Ktok[cc % 2]
                    for hp in range(4):
                        P.tr(bank(3)[0:64, hp * 128:(hp + 1) * 128], PMv[:, hp, c * 64:(c + 1) * 64], idn[:],
                             r=[PMv, idn], w=[bk(3)])
                        P.tr(bank(0)[0:64, hp * 128:(hp + 1) * 128], BKt[:, hp, c, 0, :], idn[:],
                             r=[("BKt", hp), idn], w=[bk(0)])
                        P.tr(bank(1)[0:64, hp * 128:(hp + 1) * 128], BKt[:, hp, c, 1, :], idn[:],
                             r=[("BKt", hp), idn], w=[bk(1)])
                    P.cp("act", Vt[:], bank(3)[0:64, :], r=[bk(3)], w=[Vt])
                    P.cp("dve", Bt[:], bank(0)[0:64, :], r=[bk(0)], w=[Bt])
                    P.cp("act", Kt[:], bank(1)[0:64, :], r=[bk(1)], w=[Kt])
                    if bstop == 3:
                        return
                    for h in range(8):
                        hp, j = divmod(h, 2)
                        prt = slice(64 * j, 64 * j + 64)
                        arr = ARt[prt, hp, c, :, :].rearrange("p a t -> p (a t)")
                        P.mm(DB[2][0:64, h * 128:(h + 1) * 128], BKb[prt, hp, c, 0, :], arr,
                             r=[("BKb", hp), ("ARt", hp)], w=[bk(4 + h // 4)])
                        P.mm(DB[3][0:64, h * 128:(h + 1) * 128], BKb[prt, hp, c, 1, :], arr,
                             r=[("BKb", hp), ("ARt", hp)], w=[bk(6 + h // 4)])
                        P.mm(bank(2)[0:64, h * 64:(h + 1) * 64], ARt[prt, hp, c, 0, :], BKb[prt, hp, c, 0, :],
                             r=[("BKb", hp), ("ARt", hp)], w=[bk(2)])
                    mk2 = masks[:, 0:128].unsqueeze(1).to_broadcast([64, 8, 128])
                    mk3 = masks[:, 128:192].unsqueeze(1).to_broadcast([64, 8, 64])
                    P.tt("dve", M1[:], v3(DB[2][0:64, :], 8), mk2, ALU.mult, r=[bk(4), bk(5), masks], w=[M1])
                    P.tt("dve", M2[:], v3(DB[3][0:64, :], 8), mk2, ALU.mult, r=[bk(6), bk(7), masks], w=[M2])
                    P.tt("dve", X[:], v3(bank(2)[0:64, :], 8), mk3, ALU.mult, r=[bk(2), masks], w=[X])
                    if bstop == 35:
                        return
                    P.cp("pool", YT[:, :, 0:64], M1[:, :, 0:64], r=[M1], w=[YT])
                    P.cp("pool", YT[:, :, 64:128], v3(idn8[:], 8), r=[idn8], w=[YT])
                    P.cp("pool", TT32[:], v3(idn8[:], 8), r=[idn8], w=[TT32])
                    for lvl in range(6 if not (360 <= bstop <= 372) else (1 if bstop > 366 else bstop - 360)):
                        last = lvl == 5
                        for h in range(8 if bstop != 372 else 0):
                            if not last:
                                P.mm(DB[2][0:64, h * 128:(h + 1) * 128], X[:, h, :], YT[:, h, :], w=[bk(4 + h // 4)])
                                P.mm(bank(2)[0:64, h * 64:(h + 1) * 64], YT[:, h, 0:64], X[:, h, :], w=[bk(2)])
                            else:
                                P.mm(DB[2][0:64, h * 128 + 64:(h + 1) * 128], X[:, h, :], YT[:, h, 64:128],
                                     w=[bk(4 + h // 4)])
                        PYv = v3(DB[2][0:64, :], 8)
                        if bstop == 371:
                            continue
                        if not last:
                            P.cp("act", YT[:, :, 0:64], PYv[:, :, 0:64], r=[bk(4), bk(5)], w=[YT])
                        P.tt("dve", TT32[:], TT32[:], PYv[:, :, 64:128], ALU.add, r=[TT32, bk(4), bk(5)], w=[TT32])
                        P.cp("pool", YT[:, :, 64:128], TT32[:], r=[TT32], w=[YT])
                        if not last:
                            P.cp("act", X[:], v3(bank(2)[0:64, :], 8), r=[bk(2)], w=[X])
                    if bstop == 4 or (360 <= bstop <= 372):
                        return
                    for h in range(8 if bstop not in (416, 417, 418, 419) else 0):
                        hp, j = divmod(h, 2)
                        prt = slice(64 * j, 64 * j + 64)
                        hs = slice(h * 64, (h + 1) * 64)
                        xb = 6 if bstop == 413 else 3
                        if bstop != 412:
                            P.mm(bank(xb)[0:64, hs], ARt[prt, hp, c, 0, :], STb[prt, hp, :], start=True, stop=(bstop in (411, 421)),
                                 r=[("ARt", hp), STb], w=[bk(xb)])
                        if bstop not in (411, 420, 421):
                            P.mm(bank(xb)[0:64, hs], M2[:, h, 0:64], Vt[:, hs], start=(bstop == 412), stop=True, w=[bk(xb)])
                    if bstop == 417:
                        P.cp("dve", X0s[:], bank(3)[0:64, :], r=[bk(3)], w=[X0s])
                    elif bstop == 418:
                        P.cp("act", Us[:], bank(3)[0:64, :], r=[bk(3)], w=[Us])
                    elif bstop == 419:
                        P.memset("pool", X0s[:], 0.0)
                    elif bstop != 415:
                        P.cp("act", X0s[:], bank(6 if bstop == 413 else 3)[0:64, :], r=[bk(6 if bstop == 413 else 3)], w=[X0s])
                    if bstop in (41, 411, 412, 413, 414, 415, 416, 417, 418, 419, 420, 421):
                        return
                    for h in range(8):
                        hs = slice(h * 64, (h + 1) * 64)
                        P.mm(bank(0)[0:64, hs], YT[:, h, 64:128], X0s[:, hs], w=[bk(0)])
                    P.cp("dve", Us[:], bank(0)[0:64, :], r=[bk(0)], w=[Us])
                    if bstop == 42:
                        return
                    for h in range(8):
                        hp, j = divmod(h, 2)
                        prt = slice(64 * j, 64 * j + 64)
                        hs = slice(h * 64, (h + 1) * 64)
                        P.mm(bank(1)[0:64, hs], ARt[prt, hp, c, 1, :], STb[prt, hp, :], start=True, stop=False,
                             r=[("ARt", hp), STb], w=[bk(1)])
                        P.mm(bank(1)[0:64, hs], M1[:, h, 64:128], Us[:, hs], start=False, stop=False, w=[bk(1)])
                        P.mm(bank(1)[0:64, hs], M2[:, h, 64:128], Vt[:, hs], start=False, stop=True, w=[bk(1)])
                        P.mm(bank(3)[:, hs], Bt[:, hp * 128:(hp + 1) * 128], Us[:, hs], start=True, stop=False, w=[bk(3)])
                        P.mm(bank(3)[:, hs], Kt[:, hp * 128:(hp + 1) * 128], Vt[:, hs], start=False, stop=True, w=[bk(3)])
                    if bstop == 43:
                        return
                    for j in range(2):
                        prt = slice(64 * j, 64 * j + 64)
                        psv = bank(3)[prt, :].rearrange("p (hp jj v) -> p hp jj v", hp=4, jj=2)[:, :, j, :]
                        P.tt("dve", ST[prt, :, :], ST[prt, :, :], psv, ALU.add, r=[ST, bk(3)], w=[ST])
                        P.tt("pool", ST[prt, :, :], ST[prt, :, :], PC[prt, :, c:c + 1].to_broadcast([64, 4, 64]),
                             ALU.mult, r=[ST, PC], w=[ST])
                    P.cp("pool", STb[:], ST[:], r=[ST], w=[STb])
                    if bstop == 5:
                        return
                    P.cp("act", Ys[:], bank(1)[0:64, :], r=[bk(1)], w=[Ys])
                    Yv = v3(Ys[:], 8)
                    P.red(gs[:, 0:8], Yv, ALU.add, r=[Ys], w=[gs])
                    P.ts("dve", gs[:, 0:8], gs[:, 0:8], 1.0 / 64.0, ALU.mult, r=[gs], w=[gs])
                    P.tt("dve", Yv, Yv, b3(gs[:, 0:8], 8, 64), ALU.subtract, r=[Ys, gs], w=[Ys])
                    P.tt("pool", Ysq[:], Ys[:], Ys[:], ALU.mult)
                    P.red(gs[:, 8:16], v3(Ysq[:], 8), ALU.add, r=[Ysq], w=[gs])
                    P.ts("dve", gs[:, 8:16], gs[:, 8:16], 1.0 / 64.0, ALU.mult, 64e-5, ALU.add, r=[gs], w=[gs])
                    P.act(gs[:, 8:16], gs[:, 8:16], AF.Sqrt, r=[gs], w=[gs])
                    P.recip(gs[:, 8:16], gs[:, 8:16], r=[gs], w=[gs])
                    P.tt("dve", Yv, Yv, b3(gs[:, 8:16], 8, 64), ALU.mult, r=[Ys, gs], w=[Ys])
                    for hp in range(4):
                        P.tr(bank(2)[:, hp * 64:(hp + 1) * 64], Ys[:, hp * 128:(hp + 1) * 128], idn[0:64, 0:64],
                             r=[Ys, idn], w=[bk(2)])
                    P.cp("act", yT[:, :, c * 64:(c + 1) * 64], v3(bank(2)[:, 0:256], 4), r=[bk(2)], w=[yT])
                if bstop == 6:
                    return
                ct = catT[g % 2]
                for hp in range(4):
                    P.ts("dve", fo[:], yT[:, hp, :], rwc[:, 20 + hp:21 + hp], ALU.mult, rwc[:, 24 + hp:25 + hp], ALU.add,
                         r=[yT, rwc], w=[fo])
                    P.tt("pool", fo[:], fo[:], bonusT[hp][:], ALU.add)
                    P.tt("dve", ct[:, 4 + hp, :], fo[:], gT[hp][:], ALU.mult, r=[fo, gT[hp]], w=[("ct", g % 2, 1)])
                P.dma("act", ct[:, 0:4, :], ao_d[:, :, g * GB:(g + 1) * GB], reads=[("ao_d", (g * GB) // 512)],
                      writes=[("ct", g % 2, 0)])
                for t in range(GB // 128):
                    ti = g * (GB // 128) + t
                    xrr = xr[ti % 2]; mx = mix[ti % 2]
                    for half in range(2):
                        for k in range(8):
                            P.mm(DB[0][:, half * 512:(half + 1) * 512], ct[:, k, t * 128:(t + 1) * 128],
                                 wout[:, k, half * 512:(half + 1) * 512], start=(k == 0), stop=(k == 7),
                                 r=[("ct", g % 2, 0), ("ct", g % 2, 1), wout], w=[bk(half)])
                    P.dma("sp", xrr[:], xsrc[ti * 128:(ti + 1) * 128, :], reads=[("xcur", ti)])
                    P.tt("dve", mx[:], DB[0][:], G1bc[:], ALU.mult, r=[bk(0), bk(1), G1bc], w=[mx])
                    P.tt("pool", mx[:], mx[:], xrr[:], ALU.add)
                    P.dma("sp", x1_d[ti * 128:(ti + 1) * 128, :], mx[:], writes=[("x1", ti)])

        def pass_c(l):
            h2T = P.sb("h2T", [128, 8, SG], BF16)
            yacc = P.sb("yacc", [128, NTS, D])
            Gm = P.sb("Gm", [128, NTS, NE])
            wr = P.sb("wr", [128, 8, 36]); brbc = P.sb("brbc", [128, 36])
            wg = [P.sb("wg%d" % i, [128, 8, DE], BF16) for i in range(2)]
            wu = [P.sb("wu%d" % i, [128, 8, DE], BF16) for i in range(2)]
            wd = [P.sb("wd%d" % i, [128, 2, D], BF16) for i in range(2)]
            xt = [P.sb("c_xt%d" % i, [128, D]) for i in range(2)]
            xh = P.sb("c_xh", [128, D])
            st = [P.sb("c_st%d" % i, [128, 4]) for i in range(2)]
            htmp = P.sb("c_htmp", [128, 8, 128])
            h2f = [P.sb("c_h2f%d" % i, [128, 8, 128]) for i in range(2)]
            lg = P.sb("c_lg", [128, 36]); sm = P.sb("c_sm", [128, 16])
            ge = P.sb("c_ge", [128, 4]); gsel = P.sb("c_gsel", [128, 4]); pen = P.sb("c_pen", [128, 4])
            mm_ = P.sb("c_m", [128, 32]); m2 = P.sb("c_m2", [128, 32])
            sel1 = P.sb("c_sel1", [128, 32]); sel2 = P.sb("c_sel2", [128, 32])
            sil = [P.sb("c_sil%d" % i, [128, 512]) for i in range(2)]
            hid = [P.sb("c_hid%d" % i, [128, 2, 512], BF16) for i in range(2)]
            ob = [P.sb("c_ob%d" % i, [128, D]) for i in range(2)]
            P.dma("sp", wr[:], wr_in[l].rearrange("(k p) n -> p k n", p=128))
            P.dma("sp", brbc[:], br_in[l].partition_broadcast(128))
            nhid = 0
            for sg in range(NSG):
                for tl in range(NTS):
                    ti = sg * NTS + tl
                    xtt = xt[ti % 2]; hf = h2f[ti % 2]
                    P.dma("sp", xtt[:], x1_d[ti * 128:(ti + 1) * 128, :], reads=[("x1", ti)])
                    norm_transpose(xtt, xh, st[ti % 2], A2, B2, htmp, hf[:], out_eng="dve")
                    P.cp("act", h2T[:, :, tl * 128:(tl + 1) * 128], hf[:], w=[("h2T", tl // 4)])
                    for k in range(8):
                        P.mm(bank(2)[:, 0:36], hf[:, k, :], wr[:, k, :], start=(k == 0), stop=(k == 7), w=[bk(2)])
                    P.tt("dve", lg[:], bank(2)[:, 0:36], brbc[:], ALU.add, r=[bk(2), brbc], w=[lg])
                    P.red(sm[:, 0:1], lg[:, 0:4], ALU.max, r=[lg], w=[sm])
                    P.ts("dve", sm[:, 1:2], sm[:, 0:1], -1.0, ALU.mult, r=[sm], w=[sm])
                    P.memset("dve", sm[:, 2:3], 0.0, w=[sm])
                    P.act(ge[:], lg[:, 0:4], AF.Exp, bias=sm[:, 1:2], accum=sm[:, 2:3], r=[lg, sm], w=[ge, sm])
                    P.recip(sm[:, 3:4], sm[:, 2:3], r=[sm], w=[sm])
                    P.ts("dve", gsel[:], lg[:, 0:4], sm[:, 0:1], ALU.is_equal, r=[lg, sm], w=[gsel])
                    P.ts("dve", pen[:], gsel[:], 1e30, ALU.mult, -1e30, ALU.add)
                    P.tt("dve", v3(mm_[:], 4), v3(lg[:, 4:36], 4), b3(pen[:], 4, 8), ALU.add, r=[lg, pen], w=[mm_])
                    P.red(sm[:, 4:5], mm_[:], ALU.max, r=[mm_], w=[sm])
                    P.ts("dve", sel1[:], mm_[:], sm[:, 4:5], ALU.is_equal, r=[mm_, sm], w=[sel1])
                    P.stt("dve", m2[:], sel1[:], -1e30, mm_[:], ALU.mult, ALU.add)
                    P.red(sm[:, 5:6], m2[:], ALU.max, r=[m2], w=[sm])
                    P.ts("dve", sel2[:], m2[:], sm[:, 5:6], ALU.is_equal, r=[m2, sm], w=[sel2])
                    P.tt("dve", sm[:, 6:7], sm[:, 5:6], sm[:, 4:5], ALU.subtract, r=[sm], w=[sm])
                    P.act(sm[:, 7:8], sm[:, 6:7], AF.Exp, r=[sm], w=[sm])
                    P.ts("dve", sm[:, 8:9], sm[:, 7:8], 1.0, ALU.add, r=[sm], w=[sm])
                    P.recip(sm[:, 8:9], sm[:, 8:9], r=[sm], w=[sm])
                    P.tt("dve", sm[:, 9:10], sm[:, 8:9], sm[:, 3:4], ALU.mult, r=[sm], w=[sm])
                    P.tt("dve", sm[:, 10:11], sm[:, 3:4], sm[:, 9:10], ALU.subtract, r=[sm], w=[sm])
                    P.ts("dve", Gm[:, tl, :], sel1[:], sm[:, 9:10], ALU.mult, r=[sel1, sm], w=[("Gm", tl)])
                    P.stt("dve", Gm[:, tl, :], sel2[:], sm[:, 10:11], Gm[:, tl, :], ALU.mult, ALU.add,
                          r=[sel2, sm, ("Gm", tl)], w=[("Gm", tl)])
                for e in range(NE):
                    wgb = wg[e % 2]; wub = wu[e % 2]; wdb = wd[e % 2]
                    P.dma("pool", wgb[:], wg_in[l, e].rearrange("(k p) f -> p k f", p=128))
                    P.dma("pool", wub[:], wu_in[l, e].rearrange("(k p) f -> p k f", p=128))
                    P.dma("pool", wdb[:], wd_in[l, e].rearrange("(c p) n -> p c n", p=128))
                    for tb in range(SG // 512):
                        hd = hid[nhid % 2]
                        nhid += 1
                        for fc in range(2):
                            gb = 2 + fc
                            ub = 4 + fc
                            for k in range(8):
                                P.mm(bank(gb), wgb[:, k, fc * 128:(fc + 1) * 128], h2T[:, k, tb * 512:(tb + 1) * 512],
                                     start=(k == 0), stop=(k == 7), r=[wgb, ("h2T", tb)], w=[bk(gb)])
                            for k in range(8):
                                P.mm(bank(ub), wub[:, k, fc * 128:(fc + 1) * 128], h2T[:, k, tb * 512:(tb + 1) * 512],
                                     start=(k == 0), stop=(k == 7), r=[wub, ("h2T", tb)], w=[bk(ub)])
                            P.act(sil[fc][:], bank(gb), AF.Silu, r=[bk(gb)], w=[sil[fc]])
                            P.tt("dve", hd[:, fc, :], sil[fc][:], bank(ub), ALU.mult, r=[sil[fc], bk(ub)], w=[hd])
                        for t in range(4):
                            tl = tb * 4 + t
                            dbi = 0 if t % 2 == 0 else 3
                            for half in range(2):
                                for fc in range(2):
                                    P.mm(DB[dbi][:, half * 512:(half + 1) * 512], hd[:, fc, t * 128:(t + 1) * 128],
                                         wdb[:, fc, half * 512:(half + 1) * 512], start=(fc == 0), stop=(fc == 1),
                                         w=[bk(2 * dbi + half)])
                            if e == 0:
                                P.ts("dve", yacc[:, tl, :], DB[dbi][:], Gm[:, tl, e:e + 1], ALU.mult,
                                     r=[bk(2 * dbi), bk(2 * dbi + 1), ("Gm", tl)], w=[("yacc", tl)])
                            else:
                                P.stt("dve", yacc[:, tl, :], DB[dbi][:], Gm[:, tl, e:e + 1], yacc[:, tl, :], ALU.mult,
                                      ALU.add, r=[bk(2 * dbi), bk(2 * dbi + 1), ("Gm", tl), ("yacc", tl)],
                                      w=[("yacc", tl)])
                for tl in range(NTS):
                    ti = sg * NTS + tl
                    xtt = xt[ti % 2]; o = ob[ti % 2]
                    P.dma("sp", xtt[:], x1_d[ti * 128:(ti + 1) * 128, :], reads=[("x1", ti)])
                    P.tt("dve", o[:], yacc[:, tl, :], G2bc[:], ALU.mult, r=[("yacc", tl), G2bc], w=[o])
                    P.tt("pool", o[:], o[:], xtt[:], ALU.add)
                    if l < L - 1 or dbg:
                        P.dma("sp", xcur[ti * 128:(ti + 1) * 128, :], o[:], writes=[("xcur", ti)])
                    if l == L - 1:
                        stt_ = st[ti % 2]
                        rms_rstd(o, xh, stt_)
                        P.ts("dve", xh[:], o[:], stt_[:, 3:4], ALU.mult, r=[o, stt_], w=[xh])
                        P.tt("pool", o[:], xh[:], fgbc[:], ALU.mult)
                        P.dma("sp", y_out[ti * 128:(ti + 1) * 128, :], o[:], writes=[("y", ti)])

        for l in range(L):
            with ExitStack() as sc:
                P.stack = sc
                modulation(l)
            P.barrier()
            if do_a:
                with ExitStack() as sc:
                    P.stack = sc
                    pass_a(l)
                P.barrier()
            if do_b:
                with ExitStack() as sc:
                    P.stack = sc
                    pass_b(l)
                P.barrier()
            if do_c:
                with ExitStack() as sc:
                    P.stack = sc
                    pass_c(l)
                P.barrier()

        fin = [("y", ti) for ti in range(NT)]
        if dbg:
            if do_a:
                P.dma("sp", dbg_outs["d_ao"], ao_d, reads=[("ao_d", g) for g in range(NG)])
            if do_b:
                P.dma("sp", dbg_outs["d_x1"], x1_d, reads=[("x1", ti) for ti in range(NT)])
            if do_c:
                P.dma("sp", dbg_outs["d_x2"], xcur, reads=[("xcur", ti) for ti in range(NT)])
            fin += list(dbg_outs.values())
        P.finish_wait("sp", fin)
        P.emit()
    return nc, P


def host_layout(inp, b, S=SEQ, L=DEPTH):
    f = np.float32
    m = {}
    m["x"] = np.ascontiguousarray(inp["x"][b, :S], dtype=f)
    m["pos"] = np.ascontiguousarray(inp["positions"][b:b + 1, :S], dtype=np.int32)
    m["c_t"] = np.ascontiguousarray(inp["c"][b].reshape(8, 128).T, dtype=f)
    m["ada_w"] = np.ascontiguousarray(inp["ada_w"][:L], dtype=f)
    m["ada_b"] = np.ascontiguousarray(inp["ada_b"][:L].reshape(L, 1, 6 * D), dtype=f)
    m["g1c"] = np.ascontiguousarray(inp["norm1_g"][:L].reshape(L, 8, 128).transpose(0, 2, 1), dtype=f)
    m["g2c"] = np.ascontiguousarray(inp["norm2_g"][:L].reshape(L, 8, 128).transpose(0, 2, 1), dtype=f)
    m["w_in"] = np.ascontiguousarray(inp["w_in"][:L], dtype=f)
    m["w_out"] = np.ascontiguousarray(inp["w_out"][:L], dtype=f)
    m["lamv"] = np.ascontiguousarray(inp["attn_lambda"][:L].reshape(L, 1, 256), dtype=f)
    m["sublng"] = np.ascontiguousarray(inp["attn_subln_g"][:L].reshape(L, 128, 1), dtype=f)
    mu = inp["rwkv_shift_mu"][:L]
    muc = np.zeros((L, 128, 14), f)
    muc[:, :, 0:12] = mu[:, 0:1536].reshape(L, 12, 128).transpose(0, 2, 1)
    muc[:, 0:64, 12] = mu[:, 1536:1600]
    muc[:, 0:96, 13] = mu[:, 1600:1696]
    m["mu_c"] = muc
    cols = []
    for k in ("rwkv_w0", "rwkv_a0", "rwkv_k_k", "rwkv_k_a", "rwkv_r_k", "rwkv_lnx_g", "rwkv_lnx_b"):
        cols.append(inp[k][:L].reshape(L, 4, 128).transpose(0, 2, 1))
    m["rw_cols"] = np.ascontiguousarray(np.concatenate(cols, axis=2), dtype=f)
    m["lw"] = np.ascontiguousarray(np.concatenate([inp["rwkv_w_up"][:L], inp["rwkv_a_up"][:L]], axis=1), dtype=f)
    m["gu"] = np.ascontiguousarray(inp["rwkv_g_up"][:L], dtype=f)
    m["wr"] = np.ascontiguousarray(np.concatenate([inp["moe_w_group"][:L], inp["moe_w_router"][:L]], axis=2), dtype=f)
    m["br"] = np.ascontiguousarray(np.concatenate([inp["moe_b_group"][:L], inp["moe_b_router"][:L]], axis=1).reshape(L, 1, 36), dtype=f)
    m["wg"] = np.ascontiguousarray(inp["moe_w_gate"][:L], dtype=f)
    m["wu"] = np.ascontiguousarray(inp["moe_w_up"][:L], dtype=f)
    m["wd"] = np.ascontiguousarray(inp["moe_w_down"][:L], dtype=f)
    m["fg"] = np.ascontiguousarray(inp["final_g"].reshape(1, D), dtype=f)
    for k, v in host_consts().items():
        m["k_" + k] = v
    return m


_CACHE = {}


def kernel(**inputs):
    inputs = {k: np.asarray(v) for k, v in inputs.items()}
    if "prog" not in _CACHE:
        _CACHE["prog"] = build_program()[0]
    nc = _CACHE["prog"]
    in_maps = [host_layout(inputs, b) for b in range(NCORES)]
    res = run_bass_kernel_spmd(nc, in_maps, core_ids=list(range(NCORES)))
    out = np.stack([np.asarray(res.results[b]["y"], dtype=np.float32) for b in range(NCORES)], axis=0)
    return out
```

```python
import math
from contextlib import ExitStack

import numpy as np
import concourse.bass as bass
import concourse.mybir as mybir
from concourse.bass_utils import run_bass_kernel_spmd

F32 = mybir.dt.float32
BF16 = mybir.dt.bfloat16
I32 = mybir.dt.int32
ALU = mybir.AluOpType
AF = mybir.ActivationFunctionType
AX = mybir.AxisListType

D = 1024
NCORES = 8
SEQ = 4096
DEPTH = 4
INW = 3232
NE = 32
DE = 256
LDC = 0.6065306597126334
ROPE_THETA = 500000.0

ENGS = ("pe", "act", "dve", "pool", "sp")
NDSEM = 12
SEM_ROLL = 30000


class Prog:
    def __init__(self, nc, same_engine_sync=True):
        self.nc = nc
        self.stack = None
        self.q = {e: [] for e in ENGS}
        self.cnt = {e: 0 for e in ENGS}
        self.seen = {e: {} for e in ENGS}
        self.last_w = {}
        self.readers = {}
        self.same = same_engine_sync
        self.esem = {}
        self.dsem = {}
        self.dcnt = {}
        self.dnext = {e: 0 for e in ENGS}
        self.nroll = 0
        self.n_inst = 0

    def setup(self, stack):
        self.root = stack
        self.stack = stack
        nc = self.nc
        for e in ENGS:
            self.esem[e] = stack.enter_context(nc.semaphore("es_" + e))
        for e in ("sp", "act", "pool"):
            self.dsem[e] = [stack.enter_context(nc.semaphore("ds_%s_%d" % (e, j))) for j in range(NDSEM)]
            self.dcnt[e] = [0] * NDSEM

    def sb(self, name, shape, dt=F32):
        self.uid = getattr(self, "uid", 0) + 1
        return self.stack.enter_context(self.nc.sbuf_tensor("%s_%d" % (name, self.uid), list(shape), dt))

    def ps(self, name, shape, dt=F32):
        return self.stack.enter_context(self.nc.psum_tensor(name, list(shape), dt))

    @staticmethod
    def key(x):
        if isinstance(x, (str, tuple)):
            return x
        t = getattr(x, "tensor", x)
        return t.name

    def _deps(self, eng, reads, writes, tokname=None):
        deps = {}
        tokname = tokname or eng

        def add(tok):
            if tok is None:
                return
            s, v, e = tok
            if e == tokname and (eng == "pe" or not self.same):
                return
            if deps.get(id(s), (None, 0))[1] < v:
                deps[id(s)] = (s, v)

        rk = [self.key(r) for r in reads]
        wk = [self.key(w) for w in writes]
        for k in rk:
            if isinstance(k, tuple) and k[0] == "bank" and k not in wk:
                wk.append(k)
        for k in rk:
            add(self.last_w.get(k))
        for k in wk:
            add(self.last_w.get(k))
            for t in self.readers.get(k, ()):
                add(t)
        waits = []
        for sid, (s, v) in deps.items():
            if self.seen[eng].get(sid, 0) < v:
                self.seen[eng][sid] = v
                waits.append((s, v))
        return rk, wk, waits

    def _commit(self, rk, wk, tok):
        for k in wk:
            self.last_w[k] = tok
            self.readers[k] = []
        for k in rk:
            if k not in wk:
                lst = self.readers.setdefault(k, [])
                lst[:] = [t for t in lst if t[0] is not tok[0]]
                lst.append(tok)

    def op(self, eng, fn, reads=(), writes=(), cls=None):
        tokname = eng if cls is None else eng + cls
        rk, wk, waits = self._deps(eng, reads, writes, tokname)
        if self.cnt[eng] >= SEM_ROLL:
            self.nroll += 1
            self.esem[eng] = self.root.enter_context(self.nc.semaphore("es_%s_%d" % (eng, self.nroll)))
            self.cnt[eng] = 0
        self.cnt[eng] += 1
        tok = (self.esem[eng], self.cnt[eng], tokname)
        self.q[eng].append((waits, fn, self.esem[eng], 1))
        self._commit(rk, wk, tok)
        self.n_inst += 1
        return tok

    def dma(self, eng, out, in_, reads=None, writes=None, **kw):
        reads = [in_] if reads is None else reads
        writes = [out] if writes is None else writes
        rk, wk, waits = self._deps(eng, reads, writes)
        j = self.dnext[eng]
        self.dnext[eng] = (j + 1) % NDSEM
        s = self.dsem[eng][j]
        prev = self.dcnt[eng][j]
        if prev > 0 and self.seen[eng].get(id(s), 0) < prev:
            self.seen[eng][id(s)] = prev
            waits.append((s, prev))
        self.dcnt[eng][j] = prev + 16
        tok = (s, prev + 16, "dma_" + eng)
        self.q[eng].append((waits, lambda e: e.dma_start(out=out, in_=in_, **kw), s, 16))
        self._commit(rk, wk, tok)
        self.n_inst += 1
        return tok

    def barrier(self):
        toks = [(self.esem[e], self.cnt[e]) for e in ENGS if self.cnt[e] > 0]
        for e in ("sp", "act", "pool"):
            for j in range(NDSEM):
                if self.dcnt[e][j] > 0:
                    toks.append((self.dsem[e][j], self.dcnt[e][j]))
        for e in ENGS:
            waits = []
            for s, v in toks:
                if s is self.esem[e]:
                    continue
                if self.seen[e].get(id(s), 0) < v:
                    self.seen[e][id(s)] = v
                    waits.append((s, v))
            if waits:
                self.q[e].append((waits, None, None, 0))

    def finish_wait(self, eng, keys):
        rk, wk, waits = self._deps(eng, keys, [])
        self.q[eng].append((waits, None, None, 0))

    def emit(self):
        nc = self.nc
        names = {"pe": "tensor", "act": "scalar", "dve": "vector", "pool": "gpsimd", "sp": "sync"}
        with nc.Block() as block:
            for e in ENGS:
                lst = self.q[e]
                if not lst:
                    continue

                def body(engobj, lst=lst):
                    for waits, fn, sem, inc in lst:
                        for s, v in waits:
                            engobj.wait_ge(s, v)
                        if fn is not None:
                            fn(engobj).then_inc(sem, inc)

                getattr(block, names[e])(body)

    def mm(self, out, lhsT, rhs, start=True, stop=True, r=None, w=None):
        kp = lhsT.partition_size()
        cls = None if kp > 64 else "_%d_%d" % (lhsT.base_partition(), 32 if kp <= 32 else 64)
        return self.op("pe", lambda e: e.matmul(out, lhsT=lhsT, rhs=rhs, start=start, stop=stop),
                       r if r is not None else [lhsT, rhs], w if w is not None else [out], cls=cls)

    def tr(self, out, in_, ident, r=None, w=None):
        kp = in_.partition_size()
        cls = None if kp > 64 else "_%d_%d" % (in_.base_partition(), 32 if kp <= 32 else 64)
        return self.op("pe", lambda e: e.transpose(out, in_, ident),
                       r if r is not None else [in_, ident], w if w is not None else [out], cls=cls)

    def act(self, out, in_, func, bias=None, scale=None, accum=None, r=None, w=None, eng="act"):
        kw = {}
        rr = [in_]
        if bias is not None:
            kw["bias"] = bias
            if not isinstance(bias, (int, float)):
                rr.append(bias)
        if scale is not None:
            kw["scale"] = scale
            if not isinstance(scale, (int, float)):
                rr.append(scale)
        ww = [out]
        if accum is not None:
            kw["accum_out"] = accum
            ww.append(accum)
        return self.op("act", lambda e: e.activation(out=out, in_=in_, func=func, **kw),
                       r if r is not None else rr, w if w is not None else ww)

    def tt(self, eng, out, in0, in1, op, r=None, w=None):
        return self.op(eng, lambda e: e.tensor_tensor(out=out, in0=in0, in1=in1, op=op),
                       r if r is not None else [in0, in1], w if w is not None else [out])

    def ts(self, eng, out, in0, s1, op0, s2=None, op1=None, r=None, w=None):
        rr = [in0]
        for s in (s1, s2):
            if s is not None and not isinstance(s, (int, float)):
                rr.append(s)
        if op1 is None:
            fn = lambda e: e.tensor_scalar(out=out, in0=in0, scalar1=s1, scalar2=None, op0=op0)
        else:
            fn = lambda e: e.tensor_scalar(out=out, in0=in0, scalar1=s1, scalar2=s2, op0=op0, op1=op1)
        return self.op(eng, fn, r if r is not None else rr, w if w is not None else [out])

    def stt(self, eng, out, in0, scalar, in1, op0, op1, r=None, w=None):
        rr = [in0, in1]
        if not isinstance(scalar, (int, float)):
            rr.append(scalar)
        return self.op(eng, lambda e: e.scalar_tensor_tensor(out=out, in0=in0, scalar=scalar, in1=in1, op0=op0, op1=op1),
                       r if r is not None else rr, w if w is not None else [out])

    def cp(self, eng, out, in_, r=None, w=None):
        if eng == "act":
            return self.act(out, in_, AF.Copy, r=r, w=w)
        return self.op(eng, lambda e: e.tensor_copy(out=out, in_=in_),
                       r if r is not None else [in_], w if w is not None else [out])

    def memset(self, eng, out, val, w=None):
        return self.op(eng, lambda e: e.memset(out, val), [], w if w is not None else [out])

    def recip(self, out, in_, r=None, w=None):
        return self.op("dve", lambda e: e.reciprocal(out=out, in_=in_),
                       r if r is not None else [in_], w if w is not None else [out])

    def red(self, out, in_, op, r=None, w=None):
        return self.op("dve", lambda e: e.tensor_reduce(out=out, in_=in_, axis=AX.X, op=op),
                       r if r is not None else [in_], w if w is not None else [out])


def host_consts():
    c = {}
    c["idn"] = np.eye(128, dtype=np.float32)
    rp = np.zeros((128, 128), np.float32)
    for f in range(128):
        d = f % 64
        if d < 8:
            rp[f + 8, f] = 1.0
        elif d < 16:
            rp[f - 8, f] = 1.0
    c["rperm"] = rp
    invf = (ROPE_THETA ** (-np.arange(0, 16, 2, dtype=np.float32) / np.float32(16))).astype(np.float32)
    col = np.zeros((128, 4), np.float32)
    for f in range(128):
        d = f % 64
        if d < 16:
            col[f, 0] = invf[d % 8]
            col[f, 1] = -1.0 if d < 8 else 1.0
    c["ropecol"] = col
    bd = np.zeros((128, 128), np.float32)
    bd[:64, :64] = 1.0
    bd[64:, 64:] = 1.0
    c["bd64"] = bd
    c["ones"] = np.ones((128, 128), np.float32)
    s = np.arange(64)[:, None]
    t = np.arange(64)[None, :]
    m = np.zeros((64, 192), np.float32)
    m[:, 0:64] = (s < t)
    m[:, 64:128] = (s <= t)
    m[:, 128:192] = (t < s)
    c["masks"] = m
    rm = np.ones((128, 512), np.float32)
    rm[:, ::64] = 0.0
    c["rmask"] = rm
    c["idn64x8"] = np.tile(np.eye(64, dtype=np.float32), (1, 8))
    return c


CONST_SHAPES = {"idn": [128, 128], "rperm": [128, 128], "ropecol": [128, 4], "bd64": [128, 128],
                "ones": [128, 128], "masks": [64, 192], "rmask": [128, 512], "idn64x8": [64, 512]}


def lambda_init_of(layer):
    return 0.8 - 0.6 * math.exp(-0.3 * layer)


def build_program(S=SEQ, L=DEPTH, dbg=False, same_sync=True, do_a=True, do_b=True, do_c=True, GB=256, bstop=99):
    assert S % 512 == 0
    NG = S // 512
    NT = S // 128
    SG = min(2048, S)
    NSG = S // SG
    NTS = SG // 128
    NGB = S // GB
    NCB = GB // 64
    nc = bass.Bass("TRN2", target_bir_lowering=False)
    P = Prog(nc, same_engine_sync=same_sync)

    def din(name, shape, dt=F32):
        return nc.dram_tensor(name, list(shape), dt, kind="ExternalInput").ap()

    def dscr(name, shape, dt=F32):
        return nc.dram_tensor(name, list(shape), dt).ap()

    x_in = din("x", [S, D])
    pos_in = din("pos", [1, S], I32)
    c_in = din("c_t", [128, 8])
    ada_w = din("ada_w", [L, D, 6 * D])
    ada_b = din("ada_b", [L, 1, 6 * D])
    g1c = din("g1c", [L, 128, 8])
    g2c = din("g2c", [L, 128, 8])
    w_in = din("w_in", [L, D, INW])
    w_out = din("w_out", [L, D, D])
    lamv = din("lamv", [L, 1, 256])
    sublng = din("sublng", [L, 128, 1])
    mu_c = din("mu_c", [L, 128, 14])
    rw_cols = din("rw_cols", [L, 128, 28])
    lw_in = din("lw", [L, 64, 512])
    gu_in = din("gu", [L, 96, 512])
    wr_in = din("wr", [L, D, 36])
    br_in = din("br", [L, 1, 36])
    wg_in = din("wg", [L, NE, D, DE])
    wu_in = din("wu", [L, NE, D, DE])
    wd_in = din("wd", [L, NE, DE, D])
    fg_in = din("fg", [1, D])
    cst = {k: din("k_" + k, v) for k, v in CONST_SHAPES.items()}
    y_out = nc.dram_tensor("y", [S, D], F32, kind="ExternalOutput").ap()

    xcur = dscr("xcur", [S, D])
    x1_d = dscr("x1_d", [S, D])
    hT_d = dscr("hT_d", [128, 8, S], BF16)
    ao_d = dscr("ao_d", [128, 4, S], BF16)
    tabC_d = dscr("tabC_d", [128, S])
    tabS_d = dscr("tabS_d", [128, S])
    dbg_outs = {}
    if dbg:
        dbg_outs["d_ao"] = nc.dram_tensor("d_ao", [128, 4, S], BF16, kind="ExternalOutput").ap()
        dbg_outs["d_x1"] = nc.dram_tensor("d_x1", [S, D], F32, kind="ExternalOutput").ap()
        dbg_outs["d_x2"] = nc.dram_tensor("d_x2", [S, D], F32, kind="ExternalOutput").ap()

    root = ExitStack()
    with root:
        P.setup(root)
        DB = [P.ps("db%d" % i, [128, 1024]) for i in range(4)]

        def bank(i):
            return DB[i // 2][:, (i % 2) * 512:(i % 2) * 512 + 512]

        def bk(i):
            return ("bank", i)

        idn = P.sb("idn", [128, 128])
        ones = P.sb("ones", [128, 128])
        ones_bf = P.sb("ones_bf", [128, 128], BF16)
        rperm = P.sb("rperm", [128, 128])
        bd64 = P.sb("bd64", [128, 128])
        masks = P.sb("masks", [64, 192])
        rmask = P.sb("rmask", [128, 512])
        idn8 = P.sb("idn8", [64, 512])
        ropecol = P.sb("ropecol", [128, 4])
        onesm = P.sb("onesm", [128, 128])
        cact = P.sb("cact", [128, 8])
        fgbc = P.sb("fgbc", [128, D])
        for t, k in ((idn, "idn"), (ones, "ones"), (rperm, "rperm"), (bd64, "bd64"), (masks, "masks"),
                     (rmask, "rmask"), (idn8, "idn64x8"), (ropecol, "ropecol")):
            P.dma("sp", t[:], cst[k])
        P.cp("dve", ones_bf[:], ones[:])
        P.ts("dve", onesm[:], ones[:], 1.0 / 128.0, ALU.mult)
        P.dma("sp", cact[:], c_in)
        P.act(cact[:], cact[:], AF.Silu)
        P.dma("sp", fgbc[:], fg_in.partition_broadcast(128))
        A1 = P.sb("A1", [128, 8]); B1 = P.sb("B1", [128, 8])
        A2 = P.sb("A2", [128, 8]); B2 = P.sb("B2", [128, 8])
        G1bc = P.sb("G1bc", [128, D]); G2bc = P.sb("G2bc", [128, D])
        lam = P.sb("lam", [1, 4])
        sgcol = P.sb("sgcol", [128, 1])

        def b3(ap, a, b):
            return ap.unsqueeze(2).to_broadcast([ap.shape[0], a, b])

        def v3(ap, a):
            return ap.rearrange("p (a b) -> p a b", a=a)

        def rms_rstd(xtt, junk, stt_):
            P.memset("pool", stt_[:, 0:1], 0.0, w=[stt_])
            P.act(junk[:], xtt[:], AF.Square, accum=stt_[:, 0:1], r=[xtt, stt_], w=[junk, stt_])
            P.ts("dve", stt_[:, 1:2], stt_[:, 0:1], 1.0 / D, ALU.mult, 1e-6, ALU.add, r=[stt_], w=[stt_])
            P.act(stt_[:, 2:3], stt_[:, 1:2], AF.Sqrt, r=[stt_], w=[stt_])
            P.recip(stt_[:, 3:4], stt_[:, 2:3], r=[stt_], w=[stt_])

        def norm_transpose(xtt, xhh, stt_, Acol, Bcol, htmp, out3, out_eng="pool"):
            rms_rstd(xtt, xhh, stt_)
            P.ts("pool", xhh[:], xtt[:], stt_[:, 3:4], ALU.mult, r=[xtt, stt_], w=[xhh])
            for k in range(8):
                P.tr(DB[0][:, k * 128:(k + 1) * 128], xhh[:, k * 128:(k + 1) * 128], idn[:], w=[bk(k // 4)])
            pv = v3(DB[0][:], 8)
            P.tt("dve", htmp[:], pv, b3(Acol[:], 8, 128), ALU.mult, r=[bk(0), bk(1), Acol], w=[htmp])
            P.tt(out_eng, out3, htmp[:], b3(Bcol[:], 8, 128), ALU.add, r=[htmp, Bcol], w=[out3])

        with ExitStack() as sc:
            P.stack = sc
            posi = P.sb("posi", [128, S], I32)
            ang = P.sb("ang", [128, S])
            kf_ = P.sb("kf_", [128, S])
            ki_ = P.sb("ki_", [128, S], I32)
            kg_ = P.sb("kg_", [128, S])
            P.dma("sp", posi[:], pos_in.partition_broadcast(128))
            P.cp("dve", ang[:], posi[:])
            P.ts("pool", ang[:], ang[:], ropecol[:, 0:1], ALU.mult)
            for which, shift, dst in (("sin", 0.5, tabS_d), ("cos", 0.75, tabC_d)):
                P.ts("dve", kf_[:], ang[:], 1.0 / (2.0 * math.pi), ALU.mult, shift, ALU.add)
                P.cp("dve", ki_[:], kf_[:])
                P.cp("pool", kg_[:], ki_[:])
                P.tt("dve", kf_[:], kf_[:], kg_[:], ALU.subtract)
                P.ts("pool", kg_[:], kf_[:], 0.0, ALU.is_lt)
                P.tt("dve", kf_[:], kf_[:], kg_[:], ALU.add)
                P.ts("dve", kf_[:], kf_[:], 2.0 * math.pi, ALU.mult, -math.pi, ALU.add)
                P.ts("dve", kf_[:], kf_[:], 3.14159, ALU.min, -3.14159, ALU.max)
                P.act(kf_[:], kf_[:], AF.Sin)
                if which == "sin":
                    P.ts("dve", kf_[:], kf_[:], ropecol[:, 1:2], ALU.mult)
                P.dma("sp", dst, kf_[:])
        P.barrier()

        def modulation(l):
            linit = lambda_init_of(l)
            awb = [P.sb("awb%d" % i, [128, 8, 512]) for i in range(2)]
            brow = P.sb("brow", [1, 6 * D])
            mrow = P.sb("mrow", [1, 512])
            colt = P.sb("colt", [128, 48])
            gc1 = P.sb("gc1", [128, 8]); gc2 = P.sb("gc2", [128, 8])
            lrow = P.sb("lrow", [1, 256]); ltmp = P.sb("ltmp", [1, 128]); lsum = P.sb("lsum", [1, 2])
            sgl = P.sb("sgl", [128, 1])
            P.dma("sp", brow[:], ada_b[l])
            P.dma("sp", gc1[:], g1c[l]); P.dma("sp", gc2[:], g2c[l])
            awv = ada_w[l].rearrange("(k p) n -> p k n", p=128)
            for cc in range(12):
                wb = awb[cc % 2]
                P.dma("sp" if cc % 2 == 0 else "act", wb[:], awv[:, :, cc * 512:(cc + 1) * 512])
                for k in range(8):
                    P.mm(bank(0)[0:1, :], cact[:, k:k + 1], wb[:, k, :], start=(k == 0), stop=(k == 7), w=[bk(0)])
                P.tt("dve", mrow[:], bank(0)[0:1, :], brow[:, cc * 512:(cc + 1) * 512], ALU.add,
                     r=[bk(0), brow], w=[mrow])
                if cc in (4, 5, 10, 11):
                    P.mm(bank(1), ones[0:1, :], mrow[:], w=[bk(1)])
                    dstg = G1bc if cc < 6 else G2bc
                    off = (cc % 2) * 512
                    P.cp("act", dstg[:, off:off + 512], bank(1), r=[bk(1)], w=[dstg])
                else:
                    for j in range(4):
                        P.mm(bank(1)[:, j:j + 1], mrow[0:1, j * 128:(j + 1) * 128], ones[0:1, 0:1], w=[bk(1)])
                    P.cp("act", colt[:, cc * 4:cc * 4 + 4], bank(1)[:, 0:4], r=[bk(1)], w=[colt])
            P.stt("dve", A1[:], colt[:, 8:16], 1.0, gc1[:], ALU.add, ALU.mult)
            P.cp("dve", B1[:], colt[:, 0:8])
            P.stt("dve", A2[:], colt[:, 32:40], 1.0, gc2[:], ALU.add, ALU.mult)
            P.cp("dve", B2[:], colt[:, 24:32])
            P.dma("sp", lrow[:], lamv[l])
            lv = lrow[:].rearrange("o (a b d) -> o a b d", a=2, b=2)
            P.tt("dve", v3(ltmp[:], 2), lv[:, :, 0, :], lv[:, :, 1, :], ALU.mult, r=[lrow], w=[ltmp])
            P.red(lsum[:], v3(ltmp[:], 2), ALU.add, r=[ltmp], w=[lsum])
            P.act(lsum[:], lsum[:], AF.Exp)
            P.tt("dve", lam[:, 0:1], lsum[:, 0:1], lsum[:, 1:2], ALU.subtract, r=[lsum], w=[lam])
            P.ts("dve", lam[:, 0:1], lam[:, 0:1], float(linit), ALU.add)
            P.dma("sp", sgl[:], sublng[l])
            P.ts("dve", sgcol[:], sgl[:], float(1.0 - linit), ALU.mult)

        def pass_a(l):
            xsrc = x_in if l == 0 else xcur
            wqkv = P.sb("wqkv", [128, 8, 1536], BF16)
            kc = P.sb("kc", [128, 4, S], BF16)
            vc = P.sb("vc", [128, NT, 512], BF16)
            xt = [P.sb("a_xt%d" % i, [128, D]) for i in range(2)]
            xh = P.sb("a_xh", [128, D])
            st = [P.sb("a_st%d" % i, [128, 4]) for i in range(2)]
            htmp = P.sb("a_htmp", [128, 8, 128])
            hT = [P.sb("a_hT%d" % i, [128, 8, 512], BF16) for i in range(2)]
            tC = [P.sb("a_tC%d" % i, [128, 512]) for i in range(2)]
            tS = [P.sb("a_tS%d" % i, [128, 512]) for i in range(2)]
            qf = [P.sb("a_qf%d" % i, [128, 512]) for i in range(2)]
            t1 = [P.sb("a_t1%d" % i, [128, 512]) for i in range(2)]
            t2 = [P.sb("a_t2%d" % i, [128, 512]) for i in range(2)]
            qr = [P.sb("a_qr%d" % i, [128, 4, 512], BF16) for i in range(2)]
            PT = [P.sb("a_PT%d" % i, [128, 512], BF16) for i in range(4)]
            rs = P.sb("a_rs", [1, 512])
            bcs = P.sb("a_bcs", [128, 512])
            om = [P.sb("a_om%d" % i, [128, 512]) for i in range(2)]
            osq = P.sb("a_osq", [128, 512])
            rstd = P.sb("a_rstd", [128, 512])
            aoT = [P.sb("a_aoT%d" % i, [128, 4, 512], BF16) for i in range(2)]
            wv = w_in[l].rearrange("(k p) n -> p k n", p=128)
            P.dma("pool", wqkv[:], wv[:, :, 0:1536])
            for g in range(NG):
                hTg = hT[g % 2]
                for t in range(4):
                    ti = g * 4 + t
                    xtt = xt[ti % 2]
                    P.dma("sp", xtt[:], xsrc[ti * 128:(ti + 1) * 128, :], reads=[("xcur", ti)])
                    norm_transpose(xtt, xh, st[ti % 2], A1, B1, htmp, hTg[:, :, t * 128:(t + 1) * 128])
                P.dma("sp", hT_d[:, :, g * 512:(g + 1) * 512], hTg[:], writes=[("hT_d", g)])
                tCg = tC[g % 2]; tSg = tS[g % 2]
                P.dma("act", tCg[:], tabC_d[:, g * 512:(g + 1) * 512])
                P.dma("act", tSg[:], tabS_d[:, g * 512:(g + 1) * 512])
                qrg = qr[g % 2]
                for c in range(8):
                    pb = 2 + (c % 2)
                    sb2 = 4 + (c % 2)
                    for k in range(8):
                        P.mm(bank(pb), wqkv[:, k, c * 128:(c + 1) * 128], hTg[:, k, :], start=(k == 0),
                             stop=(k == 7), w=[bk(pb)])
                    qff = qf[c % 2]; t1c = t1[c % 2]; t2c = t2[c % 2]
                    P.cp("act", qff[:], bank(pb), r=[bk(pb)], w=[qff])
                    P.mm(bank(sb2), rperm[:], qff[:], w=[bk(sb2)])
                    P.tt("pool", t1c[:], qff[:], tCg[:], ALU.mult)
                    P.tt("dve", t2c[:], bank(sb2), tSg[:], ALU.mult, r=[bk(sb2), tSg], w=[t2c])
                    if c < 4:
                        P.tt("pool", qrg[:, c, :], t1c[:], t2c[:], ALU.add, w=[qrg])
                    else:
                        P.tt("pool", kc[:, c - 4, g * 512:(g + 1) * 512], t1c[:], t2c[:], ALU.add, w=[("kc", g)])
                for t in range(4):
                    pb = 2 + (t % 2)
                    for k in range(8):
                        P.mm(bank(pb), hTg[:, k, t * 128:(t + 1) * 128], wqkv[:, k, 1024:1536], start=(k == 0),
                             stop=(k == 7), w=[bk(pb)])
                    P.cp("act", vc[:, g * 4 + t, :], bank(pb), r=[bk(pb)], w=[("vc", g)])
                aog = aoT[g % 2]
                nkt = 4 * g + 4
                steps = [(h, m, j) for h in range(4) for m in range(2) for j in range(nkt)]
                SBK = [4, 5, 2, 3]

                def qk(i):
                    h, m, j = steps[i]
                    prt = slice(64 * m, 64 * m + 64)
                    jl = j - 4 * g
                    qs = 128 * jl if jl > 0 else 0
                    sb_ = SBK[i % 4]
                    PTt = PT[i % 4]
                    P.mm(bank(sb_)[:, qs:512], kc[prt, h, j * 128:(j + 1) * 128], qrg[prt, h, qs:512],
                         r=[("kc", j // 4), qrg], w=[bk(sb_)])
                    P.act(PTt[:, qs:512], bank(sb_)[:, qs:512], AF.Exp, scale=0.125, r=[bk(sb_)], w=[PTt])
                    if jl >= 0:
                        P.memset("pool", PTt[64:128, qs:qs + 64], 0.0, w=[PTt])

                def pv(i):
                    h, m, j = steps[i]
                    jl = j - 4 * g
                    qs = 128 * jl if jl > 0 else 0
                    PTt = PT[i % 4]
                    P.mm(bank(6)[:, qs:512], vc[:, j, h * 128:(h + 1) * 128], PTt[:, qs:512],
                         start=(j == 0), stop=(j == nkt - 1), r=[("vc", j // 4), PTt], w=[bk(6)])
                    P.mm(bank(7)[0:1, qs:512], ones_bf[:, 0:1], PTt[:, qs:512],
                         start=(j == 0), stop=(j == nkt - 1), r=[PTt], w=[bk(7)])
                    if j != nkt - 1:
                        return
                    P.recip(rs[:], bank(7)[0:1, :], r=[bk(7)], w=[rs])
                    if m == 1:
                        P.ts("dve", rs[:], rs[:], lam[0:1, 0:1], ALU.mult)
                    P.mm(bank(0), ones[0:1, :], rs[:], w=[bk(0)])
                    P.cp("act", bcs[:], bank(0), r=[bk(0)], w=[bcs])
                    P.tt("dve", om[m][:], bank(6), bcs[:], ALU.mult, r=[bk(6), bcs], w=[om[m]])
                    if m == 0:
                        return
                    P.tt("pool", om[0][:], om[0][:], om[1][:], ALU.subtract)
                    P.tt("pool", osq[:], om[0][:], om[0][:], ALU.mult)
                    P.mm(bank(1), onesm[:], osq[:], w=[bk(1)])
                    P.ts("dve", rstd[:], bank(1), 1e-5, ALU.add, r=[bk(1)], w=[rstd])
                    P.act(rstd[:], rstd[:], AF.Sqrt)
                    P.recip(rstd[:], rstd[:])
                    P.tt("dve", om[0][:], om[0][:], rstd[:], ALU.mult)
                    P.ts("pool", aog[:, h, :], om[0][:], sgcol[:, 0:1], ALU.mult, r=[om[0], sgcol], w=[aog])

                LOOK = 2
                for i in range(len(steps) + LOOK):
                    if i < len(steps):
                        qk(i)
                    if i >= LOOK:
                        pv(i - LOOK)
                P.dma("sp", ao_d[:, :, g * 512:(g + 1) * 512], aog[:], writes=[("ao_d", g)])

        def pass_b(l):
            xsrc = x_in if l == 0 else xcur
            wrw = P.sb("wrw", [128, 8, 1696], BF16)
            wout = P.sb("wout", [128, 8, D], BF16)
            LW = P.sb("LW", [64, 512]); GU = P.sb("GU", [96, 512])
            muc = P.sb("muc", [128, 14]); rwc = P.sb("rwc", [128, 28]); omka = P.sb("omka", [128, 4])
            ST = P.sb("ST", [128, 4, 64])
            bnd = P.sb("bnd", [128, 14])
            hTb = [P.sb("b_hT%d" % i, [128, 8, GB], BF16) for i in range(2)]
            pcb = [P.sb("b_pc%d" % i, [128, GB + 1]) for i in range(2)]
            lt = P.sb("b_lt", [128, GB])
            PMr = P.sb("PMr", [128, GB]); PMk = P.sb("PMk", [128, GB]); PMv = P.sb("PMv", [128, 4, GB])
            PL1 = P.sb("PL1", [64, GB]); PL2 = P.sb("PL2", [96, GB])
            T = [P.sb("b_T%d" % i, [128, GB]) for i in range(8)]
            gT = [P.sb("b_gT%d" % i, [128, GB]) for i in range(4)]
            bonusT = [P.sb("b_bo%d" % i, [128, GB]) for i in range(4)]
            ARt = P.sb("ARt", [128, 4, NCB, 2, 64], BF16)
            BKt = P.sb("BKt", [128, 4, NCB, 2, 64])
            BKb = P.sb("BKb", [128, 4, NCB, 2, 64], BF16)
            STb = P.sb("STb", [128, 4, 64], BF16)
            PC = P.sb("PC", [128, 4, NCB])
            Vtok = [P.sb("Vtok%d" % i, [64, 512], BF16) for i in range(NCB)]
            Btok = [P.sb("Btok%d" % i, [64, 512], BF16) for i in range(NCB)]
            Ktok = [P.sb("Ktok%d" % i, [64, 512], BF16) for i in range(NCB)]
            M1 = [P.sb("M1_%d" % i, [64, 8, 128], BF16) for i in range(NCB)]; M2 = [P.sb("M2_%d" % i, [64, 8, 128], BF16) for i in range(NCB)]
            X = [P.sb("Xm_%d" % i, [64, 8, 64], BF16) for i in range(NCB)]; YT = [P.sb("YT_%d" % i, [64, 8, 128], BF16) for i in range(NCB)]
            X0s = P.sb("X0s", [64, 512], BF16); Us = P.sb("Us", [64, 512], BF16)
            Ysl = [P.sb("Ys_%d" % i, [64, 512]) for i in range(NCB)]; Ysq = P.sb("Ysq", [64, 512]); gs = P.sb("gs", [64, 16])
            yT = P.sb("yT", [128, 4, GB])
            fo = P.sb("fo", [128, GB])
            catT = [P.sb("catT%d" % i, [128, 8, GB], BF16) for i in range(1)] * 2
            xr = [P.sb("b_xr%d" % i, [128, D]) for i in range(1)] * 2
            mix = [P.sb("b_mix%d" % i, [128, D]) for i in range(1)] * 2
            wv = w_in[l].rearrange("(k p) n -> p k n", p=128)
            P.dma("pool", wrw[:], wv[:, :, 1536:INW])
            P.dma("pool", wout[:], w_out[l].rearrange("(k p) n -> p k n", p=128))
            P.dma("sp", LW[:], lw_in[l]); P.dma("sp", GU[:], gu_in[l])
            P.dma("sp", muc[:], mu_c[l]); P.dma("sp", rwc[:], rw_cols[l])
            P.ts("dve", omka[:], rwc[:, 12:16], -1.0, ALU.mult, 1.0, ALU.add)
            P.memset("pool", ST[:], 0.0)
            P.memset("pool", STb[:], 0.0)
            P.memset("pool", bnd[:], 0.0)
            rmk = rmask[:, 0:GB]

            def proj_chunk(hTg, c, dst, nchunk):
                if c < 12:
                    cols = slice(c * 128, (c + 1) * 128); M = 128
                elif c == 12:
                    cols = slice(1536, 1600); M = 64
                else:
                    cols = slice(1600, 1696); M = 96
                pb = nchunk % 2
                pc = pcb[nchunk % 2]
                for k in range(8):
                    P.mm(bank(pb)[0:M, 0:GB], wrw[:, k, cols], hTg[:, k, :], start=(k == 0), stop=(k == 7), w=[bk(pb)])
                P.cp("act", pc[0:M, 1:GB + 1], bank(pb)[0:M, 0:GB], r=[bk(pb)], w=[pc])
                P.cp("act", pc[0:M, 0:1], bnd[0:M, c:c + 1], r=[bnd], w=[pc])
                P.tt("pool", lt[0:M, :], pc[0:M, 0:GB], pc[0:M, 1:GB + 1], ALU.subtract, r=[pc], w=[lt])
                P.stt("dve", dst, lt[0:M, :], muc[0:M, c:c + 1], pc[0:M, 1:GB + 1], ALU.mult, ALU.add,
                      r=[lt, muc, pc], w=[dst])
                P.cp("act", bnd[0:M, c:c + 1], pc[0:M, GB:GB + 1], r=[pc], w=[bnd])

            if bstop == 0:
                return
            nch = 0
            for g in range(NGB):
                hTg = hTb[g % 2]
                P.dma("sp", hTg[:], hT_d[:, :, g * GB:(g + 1) * GB], reads=[("hT_d", (g * GB) // 512)])
                proj_chunk(hTg, 12, PL1[:], nch); nch += 1
                proj_chunk(hTg, 13, PL2[:], nch); nch += 1
                if bstop == 1:
                    return
                P.act(PL1[0:32, :], PL1[0:32, :], AF.Tanh)
                P.act(PL2[:], PL2[:], AF.Sigmoid)
                for hp in range(4):
                    proj_chunk(hTg, hp, PMr[:], nch); nch += 1
                    proj_chunk(hTg, 4 + hp, PMk[:], nch); nch += 1
                    proj_chunk(hTg, 8 + hp, PMv[:, hp, :], nch); nch += 1
                    cs = slice(hp * 128, (hp + 1) * 128)

                    def col(pi, hp=hp):
                        return rwc[:, pi * 4 + hp:pi * 4 + hp + 1]
                    ta, tb_, tc_, td, te, tf, tg, th = T
                    r_ = PMr[:]; k_ = PMk[:]; v_ = PMv[:, hp, :]
                    P.mm(bank(2)[:, 0:GB], LW[0:32, cs], PL1[0:32, :], w=[bk(2)])
                    P.act(ta[:], bank(2)[:, 0:GB], AF.Sigmoid, bias=col(0), r=[bk(2), rwc], w=[ta])
                    P.op("dve", lambda e, o=tb_, a=ta: e.tensor_tensor_scan(out=o[:], data0=rmk, data1=a[:], initial=0.0,
                                                                          op0=ALU.mult, op1=ALU.add), [rmask, ta], [tb_])
                    P.act(tc_[:], tb_[:], AF.Exp, scale=-LDC)
                    P.act(td[:], tb_[:], AF.Exp, scale=LDC)
                    P.tt("pool", te[:], tb_[:], ta[:], ALU.subtract)
                    P.act(te[:], te[:], AF.Exp, scale=-LDC)
                    P.mm(bank(3)[:, 0:GB], LW[32:64, cs], PL1[32:64, :], w=[bk(3)])
                    P.act(tf[:], bank(3)[:, 0:GB], AF.Sigmoid, bias=col(1), r=[bk(3), rwc], w=[tf])
                    P.mm(bank(2)[:, 0:GB], GU[0:96, cs], PL2[0:96, :], w=[bk(2)])
                    P.cp("act", gT[hp][:], bank(2)[:, 0:GB], r=[bk(2)], w=[gT[hp]])
                    P.ts("pool", tg[:], k_, col(2), ALU.mult, r=[PMk, rwc], w=[tg])
                    P.act(th[:], tg[:], AF.Square)
                    P.mm(bank(3)[:, 0:GB], bd64[:], th[:], w=[bk(3)])
                    P.act(th[:], bank(3)[:, 0:GB], AF.Sqrt, r=[bk(3)], w=[th])
                    P.ts("dve", th[:], th[:], 1e-12, ALU.max)
                    P.recip(th[:], th[:])
                    P.tt("pool", tg[:], tg[:], th[:], ALU.mult)
                    P.ts("dve", th[:], tf[:], col(3), ALU.mult, omka[:, hp:hp + 1], ALU.add, r=[tf, rwc, omka], w=[th])
                    P.tt("pool", th[:], k_, th[:], ALU.mult, r=[PMk, th], w=[th])
                    P.tt("pool", ta[:], tg[:], tf[:], ALU.mult)
                    P.stt("dve", tb_[:], r_, col(4), th[:], ALU.mult, ALU.mult, r=[PMr, rwc, th], w=[tb_])
                    P.mm(bank(2)[:, 0:GB], bd64[:], tb_[:], w=[bk(2)])
                    P.tt("dve", bonusT[hp][:], bank(2)[:, 0:GB], v_, ALU.mult, r=[bk(2), PMv], w=[bonusT[hp]])
                    P.stt("dve", ARt[:, hp, :, 0, :], v3(tg[:], NCB), -1.0, v3(te[:], NCB), ALU.mult, ALU.mult,
                          r=[tg, te], w=[("ARt", hp)])
                    P.tt("pool", ARt[:, hp, :, 1, :], v3(r_, NCB), v3(tc_[:], NCB), ALU.mult, r=[PMr, tc_], w=[("ARt", hp)])
                    P.tt("pool", BKt[:, hp, :, 0, :], v3(ta[:], NCB), v3(td[:], NCB), ALU.mult, r=[ta, td], w=[("BKt", hp)])
                    P.tt("dve", BKt[:, hp, :, 1, :], v3(th[:], NCB), v3(td[:], NCB), ALU.mult, r=[th, td], w=[("BKt", hp)])
                    P.cp("act", PC[:, hp, :], v3(tc_[:], NCB)[:, :, 63], r=[tc_], w=[PC])
                    P.cp("act", BKb[:, hp, :, :, :], BKt[:, hp, :, :, :], r=[("BKt", hp)], w=[("BKb", hp)])

                if bstop == 2:
                    return
                for c in range(NCB):
                    Vt = Vtok[c]; Bt = Btok[c]; Kt = Ktok[c]
                    XK2 = [("X", c, 0), ("X", c, 1)]
                    YK2 = [("Y", c, 0), ("Y", c, 1)]
                    TK2 = [("TTb", c, 0), ("TTb", c, 1)]
                    T32K = [("TT32", c, 0), ("TT32", c, 1)]
                    for hp in range(4):
                        P.tr(bank(3)[0:64, hp * 128:(hp + 1) * 128], PMv[:, hp, c * 64:(c + 1) * 64], idn[:],
                             r=[PMv, idn], w=[bk(3)])
                        P.tr(bank(0)[0:64, hp * 128:(hp + 1) * 128], BKt[:, hp, c, 0, :], idn[:],
                             r=[("BKt", hp), idn], w=[bk(0)])
                        P.tr(bank(1)[0:64, hp * 128:(hp + 1) * 128], BKt[:, hp, c, 1, :], idn[:],
                             r=[("BKt", hp), idn], w=[bk(1)])
                    P.cp("act", Vt[:], bank(3)[0:64, :], r=[bk(3)], w=[Vt])
                    P.cp("dve", Bt[:], bank(0)[0:64, :], r=[bk(0)], w=[Bt])
                    P.cp("act", Kt[:], bank(1)[0:64, :], r=[bk(1)], w=[Kt])
                    for h in range(8):
                        hp, j = divmod(h, 2)
                        prt = slice(64 * j, 64 * j + 64)
                        arr = ARt[prt, hp, c, :, :].rearrange("p a t -> p (a t)")
                        P.mm(DB[2][0:64, h * 128:(h + 1) * 128], BKb[prt, hp, c, 0, :], arr,
                             r=[("BKb", hp), ("ARt", hp)], w=[bk(4 + h // 4)])
                        P.mm(DB[3][0:64, h * 128:(h + 1) * 128], BKb[prt, hp, c, 1, :], arr,
                             r=[("BKb", hp), ("ARt", hp)], w=[bk(6 + h // 4)])
                        P.mm(bank(2)[0:64, h * 64:(h + 1) * 64], ARt[prt, hp, c, 0, :], BKb[prt, hp, c, 0, :],
                             r=[("BKb", hp), ("ARt", hp)], w=[bk(2)])
                    mk2 = masks[:, 0:128].unsqueeze(1).to_broadcast([64, 8, 128])
                    mk3 = masks[:, 128:192].unsqueeze(1).to_broadcast([64, 8, 64])
                    P.tt("dve", M1[c][:], v3(DB[2][0:64, :], 8), mk2, ALU.mult, r=[bk(4), bk(5), masks], w=[M1[c]])
                    P.tt("dve", M2[c][:], v3(DB[3][0:64, :], 8), mk2, ALU.mult, r=[bk(6), bk(7), masks], w=[M2[c]])
                    P.tt("dve", X[c][:], v3(bank(2)[0:64, :], 8), mk3, ALU.mult, r=[bk(2), masks], w=XK2)
                    P.cp("act", YT[c][:, :, 0:64], M1[c][:, :, 0:64], r=[M1[c]], w=YK2)
                    P.cp("pool", YT[c][:, :, 64:128], v3(idn8[:], 8), r=[idn8], w=TK2)
                for rnd in range(NCB // 2):
                    streams = [(2 * rnd + ci, half) for ci in range(2) for half in range(2)]
                    for lvl in range(6):
                        last = lvl == 5
                        for si, (c, half) in enumerate(streams):
                            pyb = 2 + si
                            pxb = 6 + si // 2
                            pxo = (si % 2) * 256
                            kx = ("X", c, half); ky = ("Y", c, half); kt = ("TTb", c, half)
                            for hh in range(4):
                                h = half * 4 + hh
                                if not last:
                                    P.mm(bank(pyb)[0:64, hh * 128:(hh + 1) * 128], X[c][:, h, :], YT[c][:, h, :],
                                         r=[kx, ky, kt], w=[bk(pyb)])
                                    P.mm(bank(pxb)[0:64, pxo + hh * 64:pxo + (hh + 1) * 64], YT[c][:, h, 0:64], X[c][:, h, :],
                                         r=[kx, ky], w=[bk(pxb)])
                                else:
                                    P.mm(bank(pyb)[0:64, hh * 128 + 64:(hh + 1) * 128], X[c][:, h, :], YT[c][:, h, 64:128],
                                         r=[kx, kt], w=[bk(pyb)])
                        for si, (c, half) in enumerate(streams):
                            pyb = 2 + si
                            pxb = 6 + si // 2
                            pxo = (si % 2) * 256
                            kx = ("X", c, half); ky = ("Y", c, half); kt = ("TTb", c, half); k32 = ("TT32", c, half)
                            hsl = slice(half * 4, half * 4 + 4)
                            PYv = v3(bank(pyb)[0:64, :], 4)
                            if not last:
                                P.cp("act", YT[c][:, hsl, 0:64], PYv[:, :, 0:64], r=[bk(pyb)], w=[ky])
                            P.tt("dve", YT[c][:, hsl, 64:128], YT[c][:, hsl, 64:128], PYv[:, :, 64:128], ALU.add,
                                 r=[kt, bk(pyb)], w=[kt])
                            if not last:
                                P.cp("act", X[c][:, hsl, :], v3(bank(pxb)[0:64, pxo:pxo + 256], 4), r=[bk(pxb)], w=[kx])
                for c in range(NCB):
                    Vt = Vtok[c]; Bt = Btok[c]; Kt = Ktok[c]
                    TK2 = [("TTb", c, 0), ("TTb", c, 1)]
                    for h in range(8):
                        hp, j = divmod(h, 2)
                        prt = slice(64 * j, 64 * j + 64)
                        hs = slice(h * 64, (h + 1) * 64)
                        P.mm(bank(3)[0:64, hs], ARt[prt, hp, c, 0, :], STb[prt, hp, :], start=True, stop=False,
                             r=[("ARt", hp), STb], w=[bk(3)])
                        P.mm(bank(3)[0:64, hs], M2[c][:, h, 0:64], Vt[:, hs], start=False, stop=True, w=[bk(3)])
                    P.cp("act", X0s[:], bank(3)[0:64, :], r=[bk(3)], w=[X0s])
                    for h in range(8):
                        hs = slice(h * 64, (h + 1) * 64)
                        P.mm(bank(0)[0:64, hs], YT[c][:, h, 64:128], X0s[:, hs], r=TK2 + [X0s], w=[bk(0)])
                    P.cp("dve", Us[:], bank(0)[0:64, :], r=[bk(0)], w=[Us])
                    for h in range(8):
                        hp, j = divmod(h, 2)
                        prt = slice(64 * j, 64 * j + 64)
                        hs = slice(h * 64, (h + 1) * 64)
                        P.mm(bank(1)[0:64, hs], ARt[prt, hp, c, 1, :], STb[prt, hp, :], start=True, stop=False,
                             r=[("ARt", hp), STb], w=[bk(1)])
                        P.mm(bank(1)[0:64, hs], M1[c][:, h, 64:128], Us[:, hs], start=False, stop=False, w=[bk(1)])
                        P.mm(bank(1)[0:64, hs], M2[c][:, h, 64:128], Vt[:, hs], start=False, stop=True, w=[bk(1)])
                        P.mm(bank(3)[:, hs], Bt[:, hp * 128:(hp + 1) * 128], Us[:, hs], start=True, stop=False, w=[bk(3)])
                        P.mm(bank(3)[:, hs], Kt[:, hp * 128:(hp + 1) * 128], Vt[:, hs], start=False, stop=True, w=[bk(3)])
                    for j in range(2):
                        prt = slice(64 * j, 64 * j + 64)
                        psv = bank(3)[prt, :].rearrange("p (hp jj v) -> p hp jj v", hp=4, jj=2)[:, :, j, :]
                        P.tt("dve", ST[prt, :, :], ST[prt, :, :], psv, ALU.add, r=[ST, bk(3)], w=[ST])
                        P.tt("pool", ST[prt, :, :], ST[prt, :, :], PC[prt, :, c:c + 1].to_broadcast([64, 4, 64]),
                             ALU.mult, r=[ST, PC], w=[ST])
                    P.cp("act", STb[:], ST[:], r=[ST], w=[STb])
                    P.cp("dve", Ysl[c][:], bank(1)[0:64, :], r=[bk(1)], w=[Ysl[c]])
                for c in range(NCB):
                    Ys = Ysl[c]
                    Yv = v3(Ys[:], 8)
                    P.red(gs[:, 0:8], Yv, ALU.add, r=[Ys], w=[gs])
                    P.ts("dve", gs[:, 0:8], gs[:, 0:8], 1.0 / 64.0, ALU.mult, r=[gs], w=[gs])
                    P.tt("dve", Yv, Yv, b3(gs[:, 0:8], 8, 64), ALU.subtract, r=[Ys, gs], w=[Ys])
                    P.tt("pool", Ysq[:], Ys[:], Ys[:], ALU.mult)
                    P.red(gs[:, 8:16], v3(Ysq[:], 8), ALU.add, r=[Ysq], w=[gs])
                    P.ts("dve", gs[:, 8:16], gs[:, 8:16], 1.0 / 64.0, ALU.mult, 64e-5, ALU.add, r=[gs], w=[gs])
                    P.act(gs[:, 8:16], gs[:, 8:16], AF.Sqrt, r=[gs], w=[gs])
                    P.recip(gs[:, 8:16], gs[:, 8:16], r=[gs], w=[gs])
                    P.tt("dve", Yv, Yv, b3(gs[:, 8:16], 8, 64), ALU.mult, r=[Ys, gs], w=[Ys])
                    for hp in range(4):
                        P.tr(bank(2)[:, hp * 64:(hp + 1) * 64], Ys[:, hp * 128:(hp + 1) * 128], idn[0:64, 0:64],
                             r=[Ys, idn], w=[bk(2)])
                    P.cp("act", yT[:, :, c * 64:(c + 1) * 64], v3(bank(2)[:, 0:256], 4), r=[bk(2)], w=[yT])
                if bstop == 6:
                    return
                ct = catT[g % 2]
                for hp in range(4):
                    P.ts("dve", fo[:], yT[:, hp, :], rwc[:, 20 + hp:21 + hp], ALU.mult, rwc[:, 24 + hp:25 + hp], ALU.add,
                         r=[yT, rwc], w=[fo])
                    P.tt("pool", fo[:], fo[:], bonusT[hp][:], ALU.add)
                    P.tt("dve", ct[:, 4 + hp, :], fo[:], gT[hp][:], ALU.mult, r=[fo, gT[hp]], w=[("ct", 0, 1)])
                P.dma("act", ct[:, 0:4, :], ao_d[:, :, g * GB:(g + 1) * GB], reads=[("ao_d", (g * GB) // 512)],
                      writes=[("ct", 0, 0)])
                for t in range(GB // 128):
                    ti = g * (GB // 128) + t
                    xrr = xr[ti % 2]; mx = mix[ti % 2]
                    for half in range(2):
                        for k in range(8):
                            P.mm(DB[0][:, half * 512:(half + 1) * 512], ct[:, k, t * 128:(t + 1) * 128],
                                 wout[:, k, half * 512:(half + 1) * 512], start=(k == 0), stop=(k == 7),
                                 r=[("ct", 0, 0), ("ct", 0, 1), wout], w=[bk(half)])
                    P.dma("sp", xrr[:], xsrc[ti * 128:(ti + 1) * 128, :], reads=[("xcur", ti)])
                    P.tt("dve", mx[:], DB[0][:], G1bc[:], ALU.mult, r=[bk(0), bk(1), G1bc], w=[mx])
                    P.tt("pool", mx[:], mx[:], xrr[:], ALU.add)
                    P.dma("sp", x1_d[ti * 128:(ti + 1) * 128, :], mx[:], writes=[("x1", ti)])

        def pass_c(l):
            h2T = P.sb("h2T", [128, 8, SG], BF16)
            yacc = P.sb("yacc", [128, NTS, D])
            Gm = P.sb("Gm", [128, NTS, NE])
            wr = P.sb("wr", [128, 8, 36]); brbc = P.sb("brbc", [128, 36])
            wg = [P.sb("wg%d" % i, [128, 8, DE], BF16) for i in range(2)]
            wu = [P.sb("wu%d" % i, [128, 8, DE], BF16) for i in range(2)]
            wd = [P.sb("wd%d" % i, [128, 2, D], BF16) for i in range(2)]
            xt = [P.sb("c_xt%d" % i, [128, D]) for i in range(2)]
            xh = P.sb("c_xh", [128, D])
            st = [P.sb("c_st%d" % i, [128, 4]) for i in range(2)]
            htmp = P.sb("c_htmp", [128, 8, 128])
            h2f = [P.sb("c_h2f%d" % i, [128, 8, 128]) for i in range(2)]
            lg = P.sb("c_lg", [128, 36]); sm = P.sb("c_sm", [128, 16])
            ge = P.sb("c_ge", [128, 4]); gsel = P.sb("c_gsel", [128, 4]); pen = P.sb("c_pen", [128, 4])
            mm_ = P.sb("c_m", [128, 32]); m2 = P.sb("c_m2", [128, 32])
            sel1 = P.sb("c_sel1", [128, 32]); sel2 = P.sb("c_sel2", [128, 32])
            sil = [P.sb("c_sil%d" % i, [128, 512]) for i in range(2)]
            hid = [P.sb("c_hid%d" % i, [128, 2, 512], BF16) for i in range(2)]
            ob = [P.sb("c_ob%d" % i, [128, D]) for i in range(2)]
            P.dma("sp", wr[:], wr_in[l].rearrange("(k p) n -> p k n", p=128))
            P.dma("sp", brbc[:], br_in[l].partition_broadcast(128))
            nhid = 0
            for sg in range(NSG):
                for tl in range(NTS):
                    ti = sg * NTS + tl
                    xtt = xt[ti % 2]; hf = h2f[ti % 2]
                    P.dma("sp", xtt[:], x1_d[ti * 128:(ti + 1) * 128, :], reads=[("x1", ti)])
                    norm_transpose(xtt, xh, st[ti % 2], A2, B2, htmp, hf[:], out_eng="dve")
                    P.cp("act", h2T[:, :, tl * 128:(tl + 1) * 128], hf[:], w=[("h2T", tl // 4)])
                    for k in range(8):
                        P.mm(bank(2)[:, 0:36], hf[:, k, :], wr[:, k, :], start=(k == 0), stop=(k == 7), w=[bk(2)])
                    P.tt("dve", lg[:], bank(2)[:, 0:36], brbc[:], ALU.add, r=[bk(2), brbc], w=[lg])
                    P.red(sm[:, 0:1], lg[:, 0:4], ALU.max, r=[lg], w=[sm])
                    P.ts("dve", sm[:, 1:2], sm[:, 0:1], -1.0, ALU.mult, r=[sm], w=[sm])
                    P.memset("dve", sm[:, 2:3], 0.0, w=[sm])
                    P.act(ge[:], lg[:, 0:4], AF.Exp, bias=sm[:, 1:2], accum=sm[:, 2:3], r=[lg, sm], w=[ge, sm])
                    P.recip(sm[:, 3:4], sm[:, 2:3], r=[sm], w=[sm])
                    P.ts("dve", gsel[:], lg[:, 0:4], sm[:, 0:1], ALU.is_equal, r=[lg, sm], w=[gsel])
                    P.ts("dve", pen[:], gsel[:], 1e30, ALU.mult, -1e30, ALU.add)
                    P.tt("dve", v3(mm_[:], 4), v3(lg[:, 4:36], 4), b3(pen[:], 4, 8), ALU.add, r=[lg, pen], w=[mm_])
                    P.red(sm[:, 4:5], mm_[:], ALU.max, r=[mm_], w=[sm])
                    P.ts("dve", sel1[:], mm_[:], sm[:, 4:5], ALU.is_equal, r=[mm_, sm], w=[sel1])
                    P.stt("dve", m2[:], sel1[:], -1e30, mm_[:], ALU.mult, ALU.add)
                    P.red(sm[:, 5:6], m2[:], ALU.max, r=[m2], w=[sm])
                    P.ts("dve", sel2[:], m2[:], sm[:, 5:6], ALU.is_equal, r=[m2, sm], w=[sel2])
                    P.tt("dve", sm[:, 6:7], sm[:, 5:6], sm[:, 4:5], ALU.subtract, r=[sm], w=[sm])
                    P.act(sm[:, 7:8], sm[:, 6:7], AF.Exp, r=[sm], w=[sm])
                    P.ts("dve", sm[:, 8:9], sm[:, 7:8], 1.0, ALU.add, r=[sm], w=[sm])
                    P.recip(sm[:, 8:9], sm[:, 8:9], r=[sm], w=[sm])
                    P.tt("dve", sm[:, 9:10], sm[:, 8:9], sm[:, 3:4], ALU.mult, r=[sm], w=[sm])
                    P.tt("dve", sm[:, 10:11], sm[:, 3:4], sm[:, 9:10], ALU.subtract, r=[sm], w=[sm])
                    P.ts("dve", Gm[:, tl, :], sel1[:], sm[:, 9:10], ALU.mult, r=[sel1, sm], w=[("Gm", tl)])
                    P.stt("dve", Gm[:, tl, :], sel2[:], sm[:, 10:11], Gm[:, tl, :], ALU.mult, ALU.add,
                          r=[sel2, sm, ("Gm", tl)], w=[("Gm", tl)])
                for e in range(NE):
                    wgb = wg[e % 2]; wub = wu[e % 2]; wdb = wd[e % 2]
                    P.dma("pool", wgb[:], wg_in[l, e].rearrange("(k p) f -> p k f", p=128))
                    P.dma("pool", wub[:], wu_in[l, e].rearrange("(k p) f -> p k f", p=128))
                    P.dma("pool", wdb[:], wd_in[l, e].rearrange("(c p) n -> p c n", p=128))
                    for tb in range(SG // 512):
                        hd = hid[nhid % 2]
                        nhid += 1
                        for fc in range(2):
                            gb = 2 + fc
                            ub = 4 + fc
                            for k in range(8):
                                P.mm(bank(gb), wgb[:, k, fc * 128:(fc + 1) * 128], h2T[:, k, tb * 512:(tb + 1) * 512],
                                     start=(k == 0), stop=(k == 7), r=[wgb, ("h2T", tb)], w=[bk(gb)])
                            for k in range(8):
                                P.mm(bank(ub), wub[:, k, fc * 128:(fc + 1) * 128], h2T[:, k, tb * 512:(tb + 1) * 512],
                                     start=(k == 0), stop=(k == 7), r=[wub, ("h2T", tb)], w=[bk(ub)])
                            P.act(sil[fc][:], bank(gb), AF.Silu, r=[bk(gb)], w=[sil[fc]])
                            P.tt("dve", hd[:, fc, :], sil[fc][:], bank(ub), ALU.mult, r=[sil[fc], bk(ub)], w=[hd])
                        for t in range(4):
                            tl = tb * 4 + t
                            dbi = 0 if t % 2 == 0 else 3
                            for half in range(2):
                                for fc in range(2):
                                    P.mm(DB[dbi][:, half * 512:(half + 1) * 512], hd[:, fc, t * 128:(t + 1) * 128],
                                         wdb[:, fc, half * 512:(half + 1) * 512], start=(fc == 0), stop=(fc == 1),
                                         w=[bk(2 * dbi + half)])
                            if e == 0:
                                P.ts("dve", yacc[:, tl, :], DB[dbi][:], Gm[:, tl, e:e + 1], ALU.mult,
                                     r=[bk(2 * dbi), bk(2 * dbi + 1), ("Gm", tl)], w=[("yacc", tl)])
                            else:
                                P.stt("dve", yacc[:, tl, :], DB[dbi][:], Gm[:, tl, e:e + 1], yacc[:, tl, :], ALU.mult,
                                      ALU.add, r=[bk(2 * dbi), bk(2 * dbi + 1), ("Gm", tl), ("yacc", tl)],
                                      w=[("yacc", tl)])
                for tl in range(NTS):
                    ti = sg * NTS + tl
                    xtt = xt[ti % 2]; o = ob[ti % 2]
                    P.dma("sp", xtt[:], x1_d[ti * 128:(ti + 1) * 128, :], reads=[("x1", ti)])
                    P.tt("dve", o[:], yacc[:, tl, :], G2bc[:], ALU.mult, r=[("yacc", tl), G2bc], w=[o])
                    P.tt("pool", o[:], o[:], xtt[:], ALU.add)
                    if l < L - 1 or dbg:
                        P.dma("sp", xcur[ti * 128:(ti + 1) * 128, :], o[:], writes=[("xcur", ti)])
                    if l == L - 1:
                        stt_ = st[ti % 2]
                        rms_rstd(o, xh, stt_)
                        P.ts("dve", xh[:], o[:], stt_[:, 3:4], ALU.mult, r=[o, stt_], w=[xh])
                        P.tt("pool", o[:], xh[:], fgbc[:], ALU.mult)
                        P.dma("sp", y_out[ti * 128:(ti + 1) * 128, :], o[:], writes=[("y", ti)])

        for l in range(L):
            with ExitStack() as sc:
                P.stack = sc
                modulation(l)
            P.barrier()
            if do_a:
                with ExitStack() as sc:
                    P.stack = sc
                    pass_a(l)
                P.barrier()
            if do_b:
                with ExitStack() as sc:
                    P.stack = sc
                    pass_b(l)
                P.barrier()
            if do_c:
                with ExitStack() as sc:
                    P.stack = sc
                    pass_c(l)
                P.barrier()

        fin = [("y", ti) for ti in range(NT)]
        if dbg:
            if do_a:
                P.dma("sp", dbg_outs["d_ao"], ao_d, reads=[("ao_d", g) for g in range(NG)])
            if do_b:
                P.dma("sp", dbg_outs["d_x1"], x1_d, reads=[("x1", ti) for ti in range(NT)])
            if do_c:
                P.dma("sp", dbg_outs["d_x2"], xcur, reads=[("xcur", ti) for ti in range(NT)])
            fin += list(dbg_outs.values())
        P.finish_wait("sp", fin)
        P.emit()
    return nc, P


def host_layout(inp, b, S=SEQ, L=DEPTH):
    f = np.float32
    m = {}
    m["x"] = np.ascontiguousarray(inp["x"][b, :S], dtype=f)
    m["pos"] = np.ascontiguousarray(inp["positions"][b:b + 1, :S], dtype=np.int32)
    m["c_t"] = np.ascontiguousarray(inp["c"][b].reshape(8, 128).T, dtype=f)
    m["ada_w"] = np.ascontiguousarray(inp["ada_w"][:L], dtype=f)
    m["ada_b"] = np.ascontiguousarray(inp["ada_b"][:L].reshape(L, 1, 6 * D), dtype=f)
    m["g1c"] = np.ascontiguousarray(inp["norm1_g"][:L].reshape(L, 8, 128).transpose(0, 2, 1), dtype=f)
    m["g2c"] = np.ascontiguousarray(inp["norm2_g"][:L].reshape(L, 8, 128).transpose(0, 2, 1), dtype=f)
    m["w_in"] = np.ascontiguousarray(inp["w_in"][:L], dtype=f)
    m["w_out"] = np.ascontiguousarray(inp["w_out"][:L], dtype=f)
    m["lamv"] = np.ascontiguousarray(inp["attn_lambda"][:L].reshape(L, 1, 256), dtype=f)
    m["sublng"] = np.ascontiguousarray(inp["attn_subln_g"][:L].reshape(L, 128, 1), dtype=f)
    mu = inp["rwkv_shift_mu"][:L]
    muc = np.zeros((L, 128, 14), f)
    muc[:, :, 0:12] = mu[:, 0:1536].reshape(L, 12, 128).transpose(0, 2, 1)
    muc[:, 0:64, 12] = mu[:, 1536:1600]
    muc[:, 0:96, 13] = mu[:, 1600:1696]
    m["mu_c"] = muc
    cols = []
    for k in ("rwkv_w0", "rwkv_a0", "rwkv_k_k", "rwkv_k_a", "rwkv_r_k", "rwkv_lnx_g", "rwkv_lnx_b"):
        cols.append(inp[k][:L].reshape(L, 4, 128).transpose(0, 2, 1))
    m["rw_cols"] = np.ascontiguousarray(np.concatenate(cols, axis=2), dtype=f)
    m["lw"] = np.ascontiguousarray(np.concatenate([inp["rwkv_w_up"][:L], inp["rwkv_a_up"][:L]], axis=1), dtype=f)
    m["gu"] = np.ascontiguousarray(inp["rwkv_g_up"][:L], dtype=f)
    m["wr"] = np.ascontiguousarray(np.concatenate([inp["moe_w_group"][:L], inp["moe_w_router"][:L]], axis=2), dtype=f)
    m["br"] = np.ascontiguousarray(np.concatenate([inp["moe_b_group"][:L], inp["moe_b_router"][:L]], axis=1).reshape(L, 1, 36), dtype=f)
    m["wg"] = np.ascontiguousarray(inp["moe_w_gate"][:L], dtype=f)
    m["wu"] = np.ascontiguousarray(inp["moe_w_up"][:L], dtype=f)
    m["wd"] = np.ascontiguousarray(inp["moe_w_down"][:L], dtype=f)
    m["fg"] = np.ascontiguousarray(inp["final_g"].reshape(1, D), dtype=f)
    for k, v in host_consts().items():
        m["k_" + k] = v
    return m


_CACHE = {}


def kernel(**inputs):
    inputs = {k: np.asarray(v) for k, v in inputs.items()}
    if "prog" not in _CACHE:
        _CACHE["prog"] = build_program()[0]
    nc = _CACHE["prog"]
    in_maps = [host_layout(inputs, b) for b in range(NCORES)]
    res = run_bass_kernel_spmd(nc, in_maps, core_ids=list(range(NCORES)))
    out = np.stack([np.asarray(res.results[b]["y"], dtype=np.float32) for b in range(NCORES)], axis=0)
    return out
```

```python
import math
from contextlib import ExitStack

import numpy as np
import concourse.bass as bass
import concourse.mybir as mybir
from concourse.bass_utils import run_bass_kernel_spmd

F32 = mybir.dt.float32
BF16 = mybir.dt.bfloat16
I32 = mybir.dt.int32
ALU = mybir.AluOpType
AF = mybir.ActivationFunctionType
AX = mybir.AxisListType

D = 1024
NCORES = 8
SEQ = 4096
DEPTH = 4
INW = 3232
NE = 32
DE = 256
LDC = 0.6065306597126334
ROPE_THETA = 500000.0

ENGS = ("pe", "act", "dve", "pool", "sp")
NDSEM = 12
SEM_ROLL = 30000
ATT_LOOK = 2


class Prog:
    def __init__(self, nc, same_engine_sync=True):
        self.nc = nc
        self.stack = None
        self.q = {e: [] for e in ENGS}
        self.cnt = {e: 0 for e in ENGS}
        self.seen = {e: {} for e in ENGS}
        self.last_w = {}
        self.readers = {}
        self.same = same_engine_sync
        self.esem = {}
        self.dsem = {}
        self.dcnt = {}
        self.dnext = {e: 0 for e in ENGS}
        self.nroll = 0
        self.n_inst = 0

    def setup(self, stack):
        self.root = stack
        self.stack = stack
        nc = self.nc
        for e in ENGS:
            self.esem[e] = stack.enter_context(nc.semaphore("es_" + e))
        for e in ("sp", "act", "pool"):
            self.dsem[e] = [stack.enter_context(nc.semaphore("ds_%s_%d" % (e, j))) for j in range(NDSEM)]
            self.dcnt[e] = [0] * NDSEM

    def sb(self, name, shape, dt=F32):
        self.uid = getattr(self, "uid", 0) + 1
        return self.stack.enter_context(self.nc.sbuf_tensor("%s_%d" % (name, self.uid), list(shape), dt))

    def ps(self, name, shape, dt=F32):
        return self.stack.enter_context(self.nc.psum_tensor(name, list(shape), dt))

    @staticmethod
    def key(x):
        if isinstance(x, (str, tuple)):
            return x
        t = getattr(x, "tensor", x)
        return t.name

    def _deps(self, eng, reads, writes, tokname=None):
        deps = {}
        tokname = tokname or eng

        def add(tok):
            if tok is None:
                return
            s, v, e = tok
            if e == tokname and (eng == "pe" or not self.same):
                return
            if deps.get(id(s), (None, 0))[1] < v:
                deps[id(s)] = (s, v)

        rk = [self.key(r) for r in reads]
        wk = [self.key(w) for w in writes]
        for k in rk:
            if isinstance(k, tuple) and k[0] == "bank" and k not in wk:
                wk.append(k)
        for k in rk:
            add(self.last_w.get(k))
        for k in wk:
            add(self.last_w.get(k))
            for t in self.readers.get(k, ()):
                add(t)
        waits = []
        for sid, (s, v) in deps.items():
            if self.seen[eng].get(sid, 0) < v:
                self.seen[eng][sid] = v
                waits.append((s, v))
        return rk, wk, waits

    def _commit(self, rk, wk, tok):
        for k in wk:
            self.last_w[k] = tok
            self.readers[k] = []
        for k in rk:
            if k not in wk:
                lst = self.readers.setdefault(k, [])
                lst[:] = [t for t in lst if t[0] is not tok[0]]
                lst.append(tok)

    def op(self, eng, fn, reads=(), writes=(), cls=None):
        tokname = eng if cls is None else eng + cls
        rk, wk, waits = self._deps(eng, reads, writes, tokname)
        if self.cnt[eng] >= SEM_ROLL:
            self.nroll += 1
            self.esem[eng] = self.root.enter_context(self.nc.semaphore("es_%s_%d" % (eng, self.nroll)))
            self.cnt[eng] = 0
        self.cnt[eng] += 1
        tok = (self.esem[eng], self.cnt[eng], tokname)
        self.q[eng].append((waits, fn, self.esem[eng], 1))
        self._commit(rk, wk, tok)
        self.n_inst += 1
        return tok

    def dma(self, eng, out, in_, reads=None, writes=None, **kw):
        reads = [in_] if reads is None else reads
        writes = [out] if writes is None else writes
        rk, wk, waits = self._deps(eng, reads, writes)
        j = self.dnext[eng]
        self.dnext[eng] = (j + 1) % NDSEM
        s = self.dsem[eng][j]
        prev = self.dcnt[eng][j]
        if prev > 0 and self.seen[eng].get(id(s), 0) < prev:
            self.seen[eng][id(s)] = prev
            waits.append((s, prev))
        self.dcnt[eng][j] = prev + 16
        tok = (s, prev + 16, "dma_" + eng)
        self.q[eng].append((waits, lambda e: e.dma_start(out=out, in_=in_, **kw), s, 16))
        self._commit(rk, wk, tok)
        self.n_inst += 1
        return tok

    def barrier(self):
        toks = [(self.esem[e], self.cnt[e]) for e in ENGS if self.cnt[e] > 0]
        for e in ("sp", "act", "pool"):
            for j in range(NDSEM):
                if self.dcnt[e][j] > 0:
                    toks.append((self.dsem[e][j], self.dcnt[e][j]))
        for e in ENGS:
            waits = []
            for s, v in toks:
                if s is self.esem[e]:
                    continue
                if self.seen[e].get(id(s), 0) < v:
                    self.seen[e][id(s)] = v
                    waits.append((s, v))
            if waits:
                self.q[e].append((waits, None, None, 0))

    def finish_wait(self, eng, keys):
        rk, wk, waits = self._deps(eng, keys, [])
        self.q[eng].append((waits, None, None, 0))

    def emit(self):
        nc = self.nc
        names = {"pe": "tensor", "act": "scalar", "dve": "vector", "pool": "gpsimd", "sp": "sync"}
        with nc.Block() as block:
            for e in ENGS:
                lst = self.q[e]
                if not lst:
                    continue

                def body(engobj, lst=lst):
                    for waits, fn, sem, inc in lst:
                        for s, v in waits:
                            engobj.wait_ge(s, v)
                        if fn is not None:
                            fn(engobj).then_inc(sem, inc)

                getattr(block, names[e])(body)

    def mm(self, out, lhsT, rhs, start=True, stop=True, r=None, w=None):
        kp = lhsT.partition_size()
        cls = None if kp > 64 else "_%d_%d" % (lhsT.base_partition(), 32 if kp <= 32 else 64)
        return self.op("pe", lambda e: e.matmul(out, lhsT=lhsT, rhs=rhs, start=start, stop=stop),
                       r if r is not None else [lhsT, rhs], w if w is not None else [out], cls=cls)

    def tr(self, out, in_, ident, r=None, w=None):
        kp = in_.partition_size()
        cls = None if kp > 64 else "_%d_%d" % (in_.base_partition(), 32 if kp <= 32 else 64)
        return self.op("pe", lambda e: e.transpose(out, in_, ident),
                       r if r is not None else [in_, ident], w if w is not None else [out], cls=cls)

    def act(self, out, in_, func, bias=None, scale=None, accum=None, r=None, w=None, eng="act"):
        kw = {}
        rr = [in_]
        if bias is not None:
            kw["bias"] = bias
            if not isinstance(bias, (int, float)):
                rr.append(bias)
        if scale is not None:
            kw["scale"] = scale
            if not isinstance(scale, (int, float)):
                rr.append(scale)
        ww = [out]
        if accum is not None:
            kw["accum_out"] = accum
            ww.append(accum)
        return self.op("act", lambda e: e.activation(out=out, in_=in_, func=func, **kw),
                       r if r is not None else rr, w if w is not None else ww)

    def tt(self, eng, out, in0, in1, op, r=None, w=None):
        return self.op(eng, lambda e: e.tensor_tensor(out=out, in0=in0, in1=in1, op=op),
                       r if r is not None else [in0, in1], w if w is not None else [out])

    def ts(self, eng, out, in0, s1, op0, s2=None, op1=None, r=None, w=None):
        rr = [in0]
        for s in (s1, s2):
            if s is not None and not isinstance(s, (int, float)):
                rr.append(s)
        if op1 is None:
            fn = lambda e: e.tensor_scalar(out=out, in0=in0, scalar1=s1, scalar2=None, op0=op0)
        else:
            fn = lambda e: e.tensor_scalar(out=out, in0=in0, scalar1=s1, scalar2=s2, op0=op0, op1=op1)
        return self.op(eng, fn, r if r is not None else rr, w if w is not None else [out])

    def stt(self, eng, out, in0, scalar, in1, op0, op1, r=None, w=None):
        rr = [in0, in1]
        if not isinstance(scalar, (int, float)):
            rr.append(scalar)
        return self.op(eng, lambda e: e.scalar_tensor_tensor(out=out, in0=in0, scalar=scalar, in1=in1, op0=op0, op1=op1),
                       r if r is not None else rr, w if w is not None else [out])

    def cp(self, eng, out, in_, r=None, w=None):
        if eng == "act":
            return self.act(out, in_, AF.Copy, r=r, w=w)
        return self.op(eng, lambda e: e.tensor_copy(out=out, in_=in_),
                       r if r is not None else [in_], w if w is not None else [out])

    def memset(self, eng, out, val, w=None):
        return self.op(eng, lambda e: e.memset(out, val), [], w if w is not None else [out])

    def recip(self, out, in_, r=None, w=None):
        return self.op("dve", lambda e: e.reciprocal(out=out, in_=in_),
                       r if r is not None else [in_], w if w is not None else [out])

    def red(self, out, in_, op, r=None, w=None):
        return self.op("dve", lambda e: e.tensor_reduce(out=out, in_=in_, axis=AX.X, op=op),
                       r if r is not None else [in_], w if w is not None else [out])


def host_consts():
    c = {}
    c["idn"] = np.eye(128, dtype=np.float32)
    rp = np.zeros((128, 128), np.float32)
    for f in range(128):
        d = f % 64
        if d < 8:
            rp[f + 8, f] = 1.0
        elif d < 16:
            rp[f - 8, f] = 1.0
    c["rperm"] = rp
    invf = (ROPE_THETA ** (-np.arange(0, 16, 2, dtype=np.float32) / np.float32(16))).astype(np.float32)
    col = np.zeros((128, 4), np.float32)
    for f in range(128):
        d = f % 64
        if d < 16:
            col[f, 0] = invf[d % 8]
            col[f, 1] = -1.0 if d < 8 else 1.0
    c["ropecol"] = col
    bd = np.zeros((128, 128), np.float32)
    bd[:64, :64] = 1.0
    bd[64:, 64:] = 1.0
    c["bd64"] = bd
    c["ones"] = np.ones((128, 128), np.float32)
    s = np.arange(64)[:, None]
    t = np.arange(64)[None, :]
    m = np.zeros((64, 192), np.float32)
    m[:, 0:64] = (s < t)
    m[:, 64:128] = (s <= t)
    m[:, 128:192] = (t < s)
    c["masks"] = m
    rm = np.ones((128, 512), np.float32)
    rm[:, ::64] = 0.0
    c["rmask"] = rm
    c["idn64x8"] = np.tile(np.eye(64, dtype=np.float32), (1, 8))
    return c


CONST_SHAPES = {"idn": [128, 128], "rperm": [128, 128], "ropecol": [128, 4], "bd64": [128, 128],
                "ones": [128, 128], "masks": [64, 192], "rmask": [128, 512], "idn64x8": [64, 512]}


def lambda_init_of(layer):
    return 0.8 - 0.6 * math.exp(-0.3 * layer)


def build_program(S=SEQ, L=DEPTH, dbg=False, same_sync=True, do_a=True, do_b=True, do_c=True, GB=256, bstop=99):
    assert S % 512 == 0
    NG = S // 512
    NT = S // 128
    SG = min(2048, S)
    NSG = S // SG
    NTS = SG // 128
    NGB = S // GB
    NCB = GB // 64
    nc = bass.Bass("TRN2", target_bir_lowering=False)
    P = Prog(nc, same_engine_sync=same_sync)

    def din(name, shape, dt=F32):
        return nc.dram_tensor(name, list(shape), dt, kind="ExternalInput").ap()

    def dscr(name, shape, dt=F32):
        return nc.dram_tensor(name, list(shape), dt).ap()

    x_in = din("x", [S, D])
    pos_in = din("pos", [1, S], I32)
    c_in = din("c_t", [128, 8])
    ada_w = din("ada_w", [L, D, 6 * D])
    ada_b = din("ada_b", [L, 1, 6 * D])
    g1c = din("g1c", [L, 128, 8])
    g2c = din("g2c", [L, 128, 8])
    w_in = din("w_in", [L, D, INW])
    w_out = din("w_out", [L, D, D])
    lamv = din("lamv", [L, 1, 256])
    sublng = din("sublng", [L, 128, 1])
    mu_c = din("mu_c", [L, 128, 14])
    rw_cols = din("rw_cols", [L, 128, 28])
    lw_in = din("lw", [L, 64, 512])
    gu_in = din("gu", [L, 96, 512])
    wr_in = din("wr", [L, D, 36])
    br_in = din("br", [L, 1, 36])
    wg_in = din("wg", [L, NE, D, DE])
    wu_in = din("wu", [L, NE, D, DE])
    wd_in = din("wd", [L, NE, DE, D])
    fg_in = din("fg", [1, D])
    cst = {k: din("k_" + k, v) for k, v in CONST_SHAPES.items()}
    y_out = nc.dram_tensor("y", [S, D], F32, kind="ExternalOutput").ap()

    xcur = dscr("xcur", [S, D])
    x1_d = dscr("x1_d", [S, D])
    hT_d = dscr("hT_d", [128, 8, S], BF16)
    ao_d = dscr("ao_d", [128, 4, S], BF16)
    tabC_d = dscr("tabC_d", [128, S])
    tabS_d = dscr("tabS_d", [128, S])
    dbg_outs = {}
    if dbg:
        dbg_outs["d_ao"] = nc.dram_tensor("d_ao", [128, 4, S], BF16, kind="ExternalOutput").ap()
        dbg_outs["d_x1"] = nc.dram_tensor("d_x1", [S, D], F32, kind="ExternalOutput").ap()
        dbg_outs["d_x2"] = nc.dram_tensor("d_x2", [S, D], F32, kind="ExternalOutput").ap()

    root = ExitStack()
    with root:
        P.setup(root)
        DB = [P.ps("db%d" % i, [128, 1024]) for i in range(4)]

        def bank(i):
            return DB[i // 2][:, (i % 2) * 512:(i % 2) * 512 + 512]

        def bk(i):
            return ("bank", i)

        idn = P.sb("idn", [128, 128])
        ones = P.sb("ones", [128, 128])
        ones_bf = P.sb("ones_bf", [128, 128], BF16)
        rperm = P.sb("rperm", [128, 128])
        bd64 = P.sb("bd64", [128, 128])
        masks = P.sb("masks", [64, 192])
        rmask = P.sb("rmask", [128, 512])
        idn8 = P.sb("idn8", [64, 512])
        ropecol = P.sb("ropecol", [128, 4])
        onesm = P.sb("onesm", [128, 128])
        cact = P.sb("cact", [128, 8])
        fgbc = P.sb("fgbc", [128, D])
        for t, k in ((idn, "idn"), (ones, "ones"), (rperm, "rperm"), (bd64, "bd64"), (masks, "masks"),
                     (rmask, "rmask"), (idn8, "idn64x8"), (ropecol, "ropecol")):
            P.dma("sp", t[:], cst[k])
        P.cp("dve", ones_bf[:], ones[:])
        P.ts("dve", onesm[:], ones[:], 1.0 / 128.0, ALU.mult)
        P.dma("sp", cact[:], c_in)
        P.act(cact[:], cact[:], AF.Silu)
        P.dma("sp", fgbc[:], fg_in.partition_broadcast(128))
        A1 = P.sb("A1", [128, 8]); B1 = P.sb("B1", [128, 8])
        A2 = P.sb("A2", [128, 8]); B2 = P.sb("B2", [128, 8])
        G1bc = P.sb("G1bc", [128, D]); G2bc = P.sb("G2bc", [128, D])
        lam = P.sb("lam", [128, 4])
        sgcol = P.sb("sgcol", [128, 1])

        def b3(ap, a, b):
            return ap.unsqueeze(2).to_broadcast([ap.shape[0], a, b])

        def v3(ap, a):
            return ap.rearrange("p (a b) -> p a b", a=a)

        def rms_rstd(xtt, junk, stt_):
            P.memset("pool", stt_[:, 0:1], 0.0, w=[stt_])
            P.act(junk[:], xtt[:], AF.Square, accum=stt_[:, 0:1], r=[xtt, stt_], w=[junk, stt_])
            P.ts("dve", stt_[:, 1:2], stt_[:, 0:1], 1.0 / D, ALU.mult, 1e-6, ALU.add, r=[stt_], w=[stt_])
            P.act(stt_[:, 2:3], stt_[:, 1:2], AF.Sqrt, r=[stt_], w=[stt_])
            P.recip(stt_[:, 3:4], stt_[:, 2:3], r=[stt_], w=[stt_])

        def norm_transpose(xtt, xhh, stt_, Acol, Bcol, htmp, out3, out_eng="pool"):
            rms_rstd(xtt, xhh, stt_)
            P.ts("dve", xhh[:], xtt[:], stt_[:, 3:4], ALU.mult, r=[xtt, stt_], w=[xhh])
            for k in range(8):
                P.tr(DB[0][:, k * 128:(k + 1) * 128], xhh[:, k * 128:(k + 1) * 128], idn[:], w=[bk(k // 4)])
            pv = v3(DB[0][:], 8)
            P.tt("dve", htmp[:], pv, b3(Acol[:], 8, 128), ALU.mult, r=[bk(0), bk(1), Acol], w=[htmp])
            P.tt(out_eng, out3, htmp[:], b3(Bcol[:], 8, 128), ALU.add, r=[htmp, Bcol], w=[out3])

        with ExitStack() as sc:
            P.stack = sc
            posi = P.sb("posi", [128, S], I32)
            ang = P.sb("ang", [128, S])
            kf_ = P.sb("kf_", [128, S])
            ki_ = P.sb("ki_", [128, S], I32)
            kg_ = P.sb("kg_", [128, S])
            P.dma("sp", posi[:], pos_in.partition_broadcast(128))
            P.cp("dve", ang[:], posi[:])
            P.ts("pool", ang[:], ang[:], ropecol[:, 0:1], ALU.mult)
            for which, shift, dst in (("sin", 0.5, tabS_d), ("cos", 0.75, tabC_d)):
                P.ts("dve", kf_[:], ang[:], 1.0 / (2.0 * math.pi), ALU.mult, shift, ALU.add)
                P.cp("dve", ki_[:], kf_[:])
                P.cp("pool", kg_[:], ki_[:])
                P.tt("dve", kf_[:], kf_[:], kg_[:], ALU.subtract)
                P.ts("pool", kg_[:], kf_[:], 0.0, ALU.is_lt)
                P.tt("dve", kf_[:], kf_[:], kg_[:], ALU.add)
                P.ts("dve", kf_[:], kf_[:], 2.0 * math.pi, ALU.mult, -math.pi, ALU.add)
                P.ts("dve", kf_[:], kf_[:], 3.14159, ALU.min, -3.14159, ALU.max)
                P.act(kf_[:], kf_[:], AF.Sin)
                if which == "sin":
                    P.ts("dve", kf_[:], kf_[:], ropecol[:, 1:2], ALU.mult)
                P.dma("sp", dst, kf_[:])
        P.barrier()

        def modulation(l):
            linit = lambda_init_of(l)
            awb = [P.sb("awb%d" % i, [128, 8, 512]) for i in range(2)]
            brow = P.sb("brow", [1, 6 * D])
            mrow = P.sb("mrow", [1, 512])
            colt = P.sb("colt", [128, 48])
            gc1 = P.sb("gc1", [128, 8]); gc2 = P.sb("gc2", [128, 8])
            lrow = P.sb("lrow", [128, 256]); ltmp = P.sb("ltmp", [128, 128]); lsum = P.sb("lsum", [128, 2])
            sgl = P.sb("sgl", [128, 1])
            P.dma("sp", brow[:], ada_b[l])
            P.dma("sp", gc1[:], g1c[l]); P.dma("sp", gc2[:], g2c[l])
            awv = ada_w[l].rearrange("(k p) n -> p k n", p=128)
            for cc in range(12):
                wb = awb[cc % 2]
                P.dma("sp" if cc % 2 == 0 else "act", wb[:], awv[:, :, cc * 512:(cc + 1) * 512])
                for k in range(8):
                    P.mm(bank(0)[0:1, :], cact[:, k:k + 1], wb[:, k, :], start=(k == 0), stop=(k == 7), w=[bk(0)])
                P.tt("dve", mrow[:], bank(0)[0:1, :], brow[:, cc * 512:(cc + 1) * 512], ALU.add,
                     r=[bk(0), brow], w=[mrow])
                if cc in (4, 5, 10, 11):
                    P.mm(bank(1), ones[0:1, :], mrow[:], w=[bk(1)])
                    dstg = G1bc if cc < 6 else G2bc
                    off = (cc % 2) * 512
                    P.cp("act", dstg[:, off:off + 512], bank(1), r=[bk(1)], w=[dstg])
                else:
                    for j in range(4):
                        P.mm(bank(1)[:, j:j + 1], mrow[0:1, j * 128:(j + 1) * 128], ones[0:1, 0:1], w=[bk(1)])
                    P.cp("act", colt[:, cc * 4:cc * 4 + 4], bank(1)[:, 0:4], r=[bk(1)], w=[colt])
            P.stt("dve", A1[:], colt[:, 8:16], 1.0, gc1[:], ALU.add, ALU.mult)
            P.cp("dve", B1[:], colt[:, 0:8])
            P.stt("dve", A2[:], colt[:, 32:40], 1.0, gc2[:], ALU.add, ALU.mult)
            P.cp("dve", B2[:], colt[:, 24:32])
            P.dma("sp", lrow[:], lamv[l].partition_broadcast(128))
            lv = lrow[:].rearrange("o (a b d) -> o a b d", a=2, b=2)
            P.tt("dve", v3(ltmp[:], 2), lv[:, :, 0, :], lv[:, :, 1, :], ALU.mult, r=[lrow], w=[ltmp])
            P.red(lsum[:], v3(ltmp[:], 2), ALU.add, r=[ltmp], w=[lsum])
            P.act(lsum[:], lsum[:], AF.Exp)
            P.tt("dve", lam[:, 0:1], lsum[:, 0:1], lsum[:, 1:2], ALU.subtract, r=[lsum], w=[lam])
            P.ts("dve", lam[:, 0:1], lam[:, 0:1], float(linit), ALU.add)
            P.dma("sp", sgl[:], sublng[l])
            P.ts("dve", sgcol[:], sgl[:], float(1.0 - linit), ALU.mult)

        def pass_a(l):
            xsrc = x_in if l == 0 else xcur
            wqkv = P.sb("wqkv", [128, 8, 1536], BF16)
            kc = P.sb("kc", [128, 4, S], BF16)
            vc = P.sb("vc", [128, NT, 512], BF16)
            xt = [P.sb("a_xt%d" % i, [128, D]) for i in range(2)]
            xh = [P.sb("a_xh%d" % i, [128, D]) for i in range(2)]
            st = [P.sb("a_st%d" % i, [128, 4]) for i in range(2)]
            htmp = [P.sb("a_htmp%d" % i, [128, 8, 128]) for i in range(2)]
            hT = [P.sb("a_hT%d" % i, [128, 8, 512], BF16) for i in range(2)]
            tC = [P.sb("a_tC%d" % i, [128, 512]) for i in range(2)]
            tS = [P.sb("a_tS%d" % i, [128, 512]) for i in range(2)]
            qf = [P.sb("a_qf%d" % i, [128, 512]) for i in range(2)]
            t1 = [P.sb("a_t1%d" % i, [128, 512]) for i in range(2)]
            t2 = [P.sb("a_t2%d" % i, [128, 512]) for i in range(2)]
            qz = [P.sb("a_qz%d" % i, [128, 4, 512], BF16) for i in range(2)]
            rsb = P.sb("a_rsb", [128, 512])
            PT = [P.sb("a_PT%d" % i, [128, 512], BF16) for i in range(4)]
            rs = P.sb("a_rs", [1, 512])
            bcs = P.sb("a_bcs", [128, 512])
            om = [P.sb("a_om%d" % i, [128, 512]) for i in range(2)]
            osq = P.sb("a_osq", [128, 512])
            rstd = P.sb("a_rstd", [128, 512])
            aoT = [P.sb("a_aoT%d" % i, [128, 4, 512], BF16) for i in range(2)]
            wv = w_in[l].rearrange("(k p) n -> p k n", p=128)
            P.dma("pool", wqkv[:], wv[:, :, 0:1536])
            P.memset("pool", qz[0][64:128, :, :], 0.0, w=[qz[0]])
            P.memset("pool", qz[1][0:64, :, :], 0.0, w=[qz[1]])
            for g in range(NG):
                hTg = hT[g % 2]
                for t in range(4):
                    ti = g * 4 + t
                    xtt = xt[ti % 2]
                    P.dma("sp", xtt[:], xsrc[ti * 128:(ti + 1) * 128, :], reads=[("xcur", ti)])
                    norm_transpose(xtt, xh[ti % 2], st[ti % 2], A1, B1, htmp[ti % 2], hTg[:, :, t * 128:(t + 1) * 128])
                P.dma("sp", hT_d[:, :, g * 512:(g + 1) * 512], hTg[:], writes=[("hT_d", g)])
                tCg = tC[g % 2]; tSg = tS[g % 2]
                P.dma("act", tCg[:], tabC_d[:, g * 512:(g + 1) * 512])
                P.dma("act", tSg[:], tabS_d[:, g * 512:(g + 1) * 512])
                for c in range(8):
                    pb = 2 + (c % 2)
                    sb2 = 4 + (c % 2)
                    for k in range(8):
                        P.mm(bank(pb), wqkv[:, k, c * 128:(c + 1) * 128], hTg[:, k, :], start=(k == 0),
                             stop=(k == 7), w=[bk(pb)])
                    qff = qf[c % 2]; t1c = t1[c % 2]; t2c = t2[c % 2]
                    P.cp("act", qff[:], bank(pb), r=[bk(pb)], w=[qff])
                    P.mm(bank(sb2), rperm[:], qff[:], w=[bk(sb2)])
                    P.tt("pool", t1c[:], qff[:], tCg[:], ALU.mult)
                    P.tt("dve", t2c[:], bank(sb2), tSg[:], ALU.mult, r=[bk(sb2), tSg], w=[t2c])
                    if c < 4:
                        P.tt("pool", qz[0][0:64, c, :], t1c[0:64, :], t2c[0:64, :], ALU.add, r=[t1c, t2c], w=[qz[0]])
                        P.tt("pool", qz[1][64:128, c, :], t1c[64:128, :], t2c[64:128, :], ALU.add, r=[t1c, t2c], w=[qz[1]])
                    else:
                        P.tt("pool", kc[:, c - 4, g * 512:(g + 1) * 512], t1c[:], t2c[:], ALU.add, w=[("kc", g)])
                for t in range(4):
                    pb = 2 + (t % 2)
                    for k in range(8):
                        P.mm(bank(pb), hTg[:, k, t * 128:(t + 1) * 128], wqkv[:, k, 1024:1536], start=(k == 0),
                             stop=(k == 7), w=[bk(pb)])
                    P.cp("act", vc[:, g * 4 + t, :], bank(pb), r=[bk(pb)], w=[("vc", g)])
                aog = aoT[g % 2]
                nkt = 4 * g + 4
                steps = [(h, m, j) for h in range(4) for m in range(2) for j in range(nkt)]
                SBK = [4, 5, 2, 3]

                def qk(i):
                    h, m, j = steps[i]
                    prt = slice(64 * m, 64 * m + 64)
                    jl = j - 4 * g
                    qs = 128 * jl if jl > 0 else 0
                    sb_ = SBK[i % 4]
                    PTt = PT[i % 4]
                    P.mm(bank(sb_)[:, qs:512], kc[:, h, j * 128:(j + 1) * 128], qz[m][:, h, qs:512],
                         r=[("kc", j // 4), qz[m]], w=[bk(sb_)])
                    P.act(PTt[:, qs:512], bank(sb_)[:, qs:512], AF.Exp, scale=0.125, r=[bk(sb_)], w=[PTt])
                    if jl >= 0:
                        P.memset("pool", PTt[64:128, qs:qs + 64], 0.0, w=[PTt])

                def pv(i):
                    h, m, j = steps[i]
                    jl = j - 4 * g
                    qs = 128 * jl if jl > 0 else 0
                    PTt = PT[i % 4]
                    P.mm(bank(6)[:, qs:512], vc[:, j, h * 128:(h + 1) * 128], PTt[:, qs:512],
                         start=(j == 0), stop=(j == nkt - 1), r=[("vc", j // 4), PTt], w=[bk(6)])
                    P.mm(bank(7)[:, qs:512], ones_bf[:, :], PTt[:, qs:512],
                         start=(j == 0), stop=(j == nkt - 1), r=[PTt], w=[bk(7)])
                    if j != nkt - 1:
                        return
                    P.recip(rsb[:], bank(7), r=[bk(7)], w=[rsb])
                    if m == 1:
                        P.ts("dve", rsb[:], rsb[:], lam[:, 0:1], ALU.mult)
                    P.tt("dve", om[m][:], bank(6), rsb[:], ALU.mult, r=[bk(6), rsb], w=[om[m]])
                    if m == 0:
                        return
                    P.tt("pool", om[0][:], om[0][:], om[1][:], ALU.subtract)
                    P.tt("pool", osq[:], om[0][:], om[0][:], ALU.mult)
                    P.mm(bank(1), onesm[:], osq[:], w=[bk(1)])
                    P.ts("dve", rstd[:], bank(1), 1e-5, ALU.add, r=[bk(1)], w=[rstd])
                    P.act(rstd[:], rstd[:], AF.Sqrt)
                    P.recip(rstd[:], rstd[:])
                    P.tt("dve", om[0][:], om[0][:], rstd[:], ALU.mult)
                    P.ts("pool", aog[:, h, :], om[0][:], sgcol[:, 0:1], ALU.mult, r=[om[0], sgcol], w=[aog])

                LOOK = ATT_LOOK
                for i in range(len(steps) + LOOK):
                    if i < len(steps):
                        qk(i)
                    if i >= LOOK:
                        pv(i - LOOK)
                P.dma("sp", ao_d[:, :, g * 512:(g + 1) * 512], aog[:], writes=[("ao_d", g)])

        def pass_b(l):
            xsrc = x_in if l == 0 else xcur
            wrw = P.sb("wrw", [128, 8, 1696], BF16)
            wout = P.sb("wout", [128, 8, D], BF16)
            LW = P.sb("LW", [64, 512]); GU = P.sb("GU", [96, 512])
            muc = P.sb("muc", [128, 14]); rwc = P.sb("rwc", [128, 28]); omka = P.sb("omka", [128, 4])
            ST = P.sb("ST", [128, 4, 64])
            bnd = P.sb("bnd", [128, 14])
            hTb = [P.sb("b_hT%d" % i, [128, 8, GB], BF16) for i in range(2)]
            pcb = [P.sb("b_pc%d" % i, [128, GB + 1]) for i in range(2)]
            lt = P.sb("b_lt", [128, GB])
            PMr = P.sb("PMr", [128, GB]); PMk = P.sb("PMk", [128, GB]); PMv = P.sb("PMv", [128, 4, GB])
            PL1 = P.sb("PL1", [64, GB]); PL2 = P.sb("PL2", [96, GB])
            T = [P.sb("b_T%d" % i, [128, GB]) for i in range(8)]
            gT = [P.sb("b_gT%d" % i, [128, GB]) for i in range(4)]
            bonusT = [P.sb("b_bo%d" % i, [128, GB]) for i in range(4)]
            ARt = P.sb("ARt", [128, 4, NCB, 2, 64], BF16)
            BKt = P.sb("BKt", [128, 4, NCB, 2, 64])
            BKb = P.sb("BKb", [128, 4, NCB, 2, 64], BF16)
            STb = P.sb("STb", [128, 4, 64], BF16)
            PC = P.sb("PC", [128, 4, NCB])
            Vtok = [P.sb("Vtok%d" % i, [64, 512], BF16) for i in range(NCB)]
            Btok = [P.sb("Btok%d" % i, [64, 512], BF16) for i in range(NCB)]
            Ktok = [P.sb("Ktok%d" % i, [64, 512], BF16) for i in range(NCB)]
            M1 = [P.sb("M1_%d" % i, [64, 8, 128], BF16) for i in range(NCB)]; M2 = [P.sb("M2_%d" % i, [64, 8, 128], BF16) for i in range(NCB)]
            X = [P.sb("Xm_%d" % i, [64, 8, 64], BF16) for i in range(NCB)]; YT = [P.sb("YT_%d" % i, [64, 8, 128], BF16) for i in range(NCB)]
            X0s = P.sb("X0s", [64, 512], BF16); Us = P.sb("Us", [64, 512], BF16)
            Ysl = [P.sb("Ys_%d" % i, [64, 512]) for i in range(NCB)]; Ysq = P.sb("Ysq", [64, 512]); gs = P.sb("gs", [64, 16])
            yT = P.sb("yT", [128, 4, GB])
            fo = P.sb("fo", [128, GB])
            catT = [P.sb("catT%d" % i, [128, 8, GB], BF16) for i in range(1)] * 2
            xr = [P.sb("b_xr%d" % i, [128, D]) for i in range(1)] * 2
            mix = [P.sb("b_mix%d" % i, [128, D]) for i in range(1)] * 2
            wv = w_in[l].rearrange("(k p) n -> p k n", p=128)
            P.dma("pool", wrw[:], wv[:, :, 1536:INW])
            P.dma("pool", wout[:], w_out[l].rearrange("(k p) n -> p k n", p=128))
            P.dma("sp", LW[:], lw_in[l]); P.dma("sp", GU[:], gu_in[l])
            P.dma("sp", muc[:], mu_c[l]); P.dma("sp", rwc[:], rw_cols[l])
            P.ts("dve", omka[:], rwc[:, 12:16], -1.0, ALU.mult, 1.0, ALU.add)
            P.memset("pool", ST[:], 0.0)
            P.memset("pool", STb[:], 0.0)
            P.memset("pool", bnd[:], 0.0)
            rmk = rmask[:, 0:GB]

            def proj_chunk(hTg, c, dst, nchunk):
                if c < 12:
                    cols = slice(c * 128, (c + 1) * 128); M = 128
                elif c == 12:
                    cols = slice(1536, 1600); M = 64
                else:
                    cols = slice(1600, 1696); M = 96
                pb = nchunk % 2
                pc = pcb[nchunk % 2]
                for k in range(8):
                    P.mm(bank(pb)[0:M, 0:GB], wrw[:, k, cols], hTg[:, k, :], start=(k == 0), stop=(k == 7), w=[bk(pb)])
                P.cp("act", pc[0:M, 1:GB + 1], bank(pb)[0:M, 0:GB], r=[bk(pb)], w=[pc])
                P.cp("act", pc[0:M, 0:1], bnd[0:M, c:c + 1], r=[bnd], w=[pc])
                P.tt("pool", lt[0:M, :], pc[0:M, 0:GB], pc[0:M, 1:GB + 1], ALU.subtract, r=[pc], w=[lt])
                P.stt("dve", dst, lt[0:M, :], muc[0:M, c:c + 1], pc[0:M, 1:GB + 1], ALU.mult, ALU.add,
                      r=[lt, muc, pc], w=[dst])
                P.cp("act", bnd[0:M, c:c + 1], pc[0:M, GB:GB + 1], r=[pc], w=[bnd])

            if bstop == 0:
                return
            nch = 0
            for g in range(NGB):
                hTg = hTb[g % 2]
                P.dma("sp", hTg[:], hT_d[:, :, g * GB:(g + 1) * GB], reads=[("hT_d", (g * GB) // 512)])
                proj_chunk(hTg, 12, PL1[:], nch); nch += 1
                proj_chunk(hTg, 13, PL2[:], nch); nch += 1
                if bstop == 1:
                    return
                P.act(PL1[0:32, :], PL1[0:32, :], AF.Tanh)
                P.act(PL2[:], PL2[:], AF.Sigmoid)
                for hp in range(4):
                    proj_chunk(hTg, hp, PMr[:], nch); nch += 1
                    proj_chunk(hTg, 4 + hp, PMk[:], nch); nch += 1
                    proj_chunk(hTg, 8 + hp, PMv[:, hp, :], nch); nch += 1
                    cs = slice(hp * 128, (hp + 1) * 128)

                    def col(pi, hp=hp):
                        return rwc[:, pi * 4 + hp:pi * 4 + hp + 1]
                    ta, tb_, tc_, td, te, tf, tg, th = T
                    r_ = PMr[:]; k_ = PMk[:]; v_ = PMv[:, hp, :]
                    P.mm(bank(2)[:, 0:GB], LW[0:32, cs], PL1[0:32, :], w=[bk(2)])
                    P.act(ta[:], bank(2)[:, 0:GB], AF.Sigmoid, bias=col(0), r=[bk(2), rwc], w=[ta])
                    P.op("dve", lambda e, o=tb_, a=ta: e.tensor_tensor_scan(out=o[:], data0=rmk, data1=a[:], initial=0.0,
                                                                          op0=ALU.mult, op1=ALU.add), [rmask, ta], [tb_])
                    P.act(tc_[:], tb_[:], AF.Exp, scale=-LDC)
                    P.act(td[:], tb_[:], AF.Exp, scale=LDC)
                    P.tt("pool", te[:], tb_[:], ta[:], ALU.subtract)
                    P.act(te[:], te[:], AF.Exp, scale=-LDC)
                    P.mm(bank(3)[:, 0:GB], LW[32:64, cs], PL1[32:64, :], w=[bk(3)])
                    P.act(tf[:], bank(3)[:, 0:GB], AF.Sigmoid, bias=col(1), r=[bk(3), rwc], w=[tf])
                    P.mm(bank(2)[:, 0:GB], GU[0:96, cs], PL2[0:96, :], w=[bk(2)])
                    P.cp("act", gT[hp][:], bank(2)[:, 0:GB], r=[bk(2)], w=[gT[hp]])
                    P.ts("pool", tg[:], k_, col(2), ALU.mult, r=[PMk, rwc], w=[tg])
                    P.act(th[:], tg[:], AF.Square)
                    P.mm(bank(3)[:, 0:GB], bd64[:], th[:], w=[bk(3)])
                    P.act(th[:], bank(3)[:, 0:GB], AF.Sqrt, r=[bk(3)], w=[th])
                    P.ts("dve", th[:], th[:], 1e-12, ALU.max)
                    P.recip(th[:], th[:])
                    P.tt("pool", tg[:], tg[:], th[:], ALU.mult)
                    P.ts("dve", th[:], tf[:], col(3), ALU.mult, omka[:, hp:hp + 1], ALU.add, r=[tf, rwc, omka], w=[th])
                    P.tt("pool", th[:], k_, th[:], ALU.mult, r=[PMk, th], w=[th])
                    P.tt("pool", ta[:], tg[:], tf[:], ALU.mult)
                    P.stt("dve", tb_[:], r_, col(4), th[:], ALU.mult, ALU.mult, r=[PMr, rwc, th], w=[tb_])
                    P.mm(bank(2)[:, 0:GB], bd64[:], tb_[:], w=[bk(2)])
                    P.tt("dve", bonusT[hp][:], bank(2)[:, 0:GB], v_, ALU.mult, r=[bk(2), PMv], w=[bonusT[hp]])
                    P.stt("dve", ARt[:, hp, :, 0, :], v3(tg[:], NCB), -1.0, v3(te[:], NCB), ALU.mult, ALU.mult,
                          r=[tg, te], w=[("ARt", hp)])
                    P.tt("pool", ARt[:, hp, :, 1, :], v3(r_, NCB), v3(tc_[:], NCB), ALU.mult, r=[PMr, tc_], w=[("ARt", hp)])
                    P.tt("pool", BKt[:, hp, :, 0, :], v3(ta[:], NCB), v3(td[:], NCB), ALU.mult, r=[ta, td], w=[("BKt", hp)])
                    P.tt("dve", BKt[:, hp, :, 1, :], v3(th[:], NCB), v3(td[:], NCB), ALU.mult, r=[th, td], w=[("BKt", hp)])
                    P.cp("act", PC[:, hp, :], v3(tc_[:], NCB)[:, :, 63], r=[tc_], w=[PC])
                    P.cp("act", BKb[:, hp, :, :, :], BKt[:, hp, :, :, :], r=[("BKt", hp)], w=[("BKb", hp)])

                if bstop == 2:
                    return
                for c in range(NCB):
                    Vt = Vtok[c]; Bt = Btok[c]; Kt = Ktok[c]
                    XK2 = [("X", c, 0), ("X", c, 1)]
                    YK2 = [("Y", c, 0), ("Y", c, 1)]
                    TK2 = [("TTb", c, 0), ("TTb", c, 1)]
                    T32K = [("TT32", c, 0), ("TT32", c, 1)]
                    for hp in range(4):
                        P.tr(bank(3)[0:64, hp * 128:(hp + 1) * 128], PMv[:, hp, c * 64:(c + 1) * 64], idn[:],
                             r=[PMv, idn], w=[bk(3)])
                        P.tr(bank(0)[0:64, hp * 128:(hp + 1) * 128], BKt[:, hp, c, 0, :], idn[:],
                             r=[("BKt", hp), idn], w=[bk(0)])
                        P.tr(bank(1)[0:64, hp * 128:(hp + 1) * 128], BKt[:, hp, c, 1, :], idn[:],
                             r=[("BKt", hp), idn], w=[bk(1)])
                    P.cp("act", Vt[:], bank(3)[0:64, :], r=[bk(3)], w=[Vt])
                    P.cp("dve", Bt[:], bank(0)[0:64, :], r=[bk(0)], w=[Bt])
                    P.cp("act", Kt[:], bank(1)[0:64, :], r=[bk(1)], w=[Kt])
                    for h in range(8):
                        hp, j = divmod(h, 2)
                        prt = slice(64 * j, 64 * j + 64)
                        arr = ARt[prt, hp, c, :, :].rearrange("p a t -> p (a t)")
                        P.mm(DB[2][0:64, h * 128:(h + 1) * 128], BKb[prt, hp, c, 0, :], arr,
                             r=[("BKb", hp), ("ARt", hp)], w=[bk(4 + h // 4)])
                        P.mm(DB[3][0:64, h * 128:(h + 1) * 128], BKb[prt, hp, c, 1, :], arr,
                             r=[("BKb", hp), ("ARt", hp)], w=[bk(6 + h // 4)])
                        P.mm(bank(2)[0:64, h * 64:(h + 1) * 64], ARt[prt, hp, c, 0, :], BKb[prt, hp, c, 0, :],
                             r=[("BKb", hp), ("ARt", hp)], w=[bk(2)])
                    mk2 = masks[:, 0:128].unsqueeze(1).to_broadcast([64, 8, 128])
                    mk3 = masks[:, 128:192].unsqueeze(1).to_broadcast([64, 8, 64])
                    P.tt("dve", M1[c][:], v3(DB[2][0:64, :], 8), mk2, ALU.mult, r=[bk(4), bk(5), masks], w=[M1[c]])
                    P.tt("dve", M2[c][:], v3(DB[3][0:64, :], 8), mk2, ALU.mult, r=[bk(6), bk(7), masks], w=[M2[c]])
                    P.tt("dve", X[c][:], v3(bank(2)[0:64, :], 8), mk3, ALU.mult, r=[bk(2), masks], w=XK2)
                    P.cp("act", YT[c][:, :, 0:64], M1[c][:, :, 0:64], r=[M1[c]], w=YK2)
                    P.cp("pool", YT[c][:, :, 64:128], v3(idn8[:], 8), r=[idn8], w=TK2)
                for rnd in range(NCB // 2):
                    streams = [(2 * rnd + ci, half) for ci in range(2) for half in range(2)]
                    for lvl in range(6):
                        last = lvl == 5
                        for si, (c, half) in enumerate(streams):
                            pyb = 2 + si
                            pxb = 6 + si // 2
                            pxo = (si % 2) * 256
                            kx = ("X", c, half); ky = ("Y", c, half); kt = ("TTb", c, half)
                            for hh in range(4):
                                h = half * 4 + hh
                                if not last:
                                    P.mm(bank(pyb)[0:64, hh * 128:(hh + 1) * 128], X[c][:, h, :], YT[c][:, h, :],
                                         r=[kx, ky, kt], w=[bk(pyb)])
                                    P.mm(bank(pxb)[0:64, pxo + hh * 64:pxo + (hh + 1) * 64], YT[c][:, h, 0:64], X[c][:, h, :],
                                         r=[kx, ky], w=[bk(pxb)])
                                else:
                                    P.mm(bank(pyb)[0:64, hh * 128 + 64:(hh + 1) * 128], X[c][:, h, :], YT[c][:, h, 64:128],
                                         r=[kx, kt], w=[bk(pyb)])
                        for si, (c, half) in enumerate(streams):
                            pyb = 2 + si
                            pxb = 6 + si // 2
                            pxo = (si % 2) * 256
                            kx = ("X", c, half); ky = ("Y", c, half); kt = ("TTb", c, half); k32 = ("TT32", c, half)
                            hsl = slice(half * 4, half * 4 + 4)
                            PYv = v3(bank(pyb)[0:64, :], 4)
                            if not last:
                                P.cp("act", YT[c][:, hsl, 0:64], PYv[:, :, 0:64], r=[bk(pyb)], w=[ky])
                            P.tt("dve", YT[c][:, hsl, 64:128], YT[c][:, hsl, 64:128], PYv[:, :, 64:128], ALU.add,
                                 r=[kt, bk(pyb)], w=[kt])
                            if not last:
                                P.cp("act", X[c][:, hsl, :], v3(bank(pxb)[0:64, pxo:pxo + 256], 4), r=[bk(pxb)], w=[kx])
                for c in range(NCB):
                    Vt = Vtok[c]; Bt = Btok[c]; Kt = Ktok[c]
                    TK2 = [("TTb", c, 0), ("TTb", c, 1)]
                    for h in range(8):
                        hp, j = divmod(h, 2)
                        prt = slice(64 * j, 64 * j + 64)
                        hs = slice(h * 64, (h + 1) * 64)
                        P.mm(bank(3)[0:64, hs], ARt[prt, hp, c, 0, :], STb[prt, hp, :], start=True, stop=False,
                             r=[("ARt", hp), STb], w=[bk(3)])
                        P.mm(bank(3)[0:64, hs], M2[c][:, h, 0:64], Vt[:, hs], start=False, stop=True, w=[bk(3)])
                    P.cp("act", X0s[:], bank(3)[0:64, :], r=[bk(3)], w=[X0s])
                    for h in range(8):
                        hs = slice(h * 64, (h + 1) * 64)
                        P.mm(bank(0)[0:64, hs], YT[c][:, h, 64:128], X0s[:, hs], r=TK2 + [X0s], w=[bk(0)])
                    P.cp("dve", Us[:], bank(0)[0:64, :], r=[bk(0)], w=[Us])
                    for h in range(8):
                        hp, j = divmod(h, 2)
                        prt = slice(64 * j, 64 * j + 64)
                        hs = slice(h * 64, (h + 1) * 64)
                        P.mm(bank(1)[0:64, hs], ARt[prt, hp, c, 1, :], STb[prt, hp, :], start=True, stop=False,
                             r=[("ARt", hp), STb], w=[bk(1)])
                        P.mm(bank(1)[0:64, hs], M1[c][:, h, 64:128], Us[:, hs], start=False, stop=False, w=[bk(1)])
                        P.mm(bank(1)[0:64, hs], M2[c][:, h, 64:128], Vt[:, hs], start=False, stop=True, w=[bk(1)])
                        P.mm(bank(3)[:, hs], Bt[:, hp * 128:(hp + 1) * 128], Us[:, hs], start=True, stop=False, w=[bk(3)])
                        P.mm(bank(3)[:, hs], Kt[:, hp * 128:(hp + 1) * 128], Vt[:, hs], start=False, stop=True, w=[bk(3)])
                    for j in range(2):
                        prt = slice(64 * j, 64 * j + 64)
                        psv = bank(3)[prt, :].rearrange("p (hp jj v) -> p hp jj v", hp=4, jj=2)[:, :, j, :]
                        P.tt("dve", ST[prt, :, :], ST[prt, :, :], psv, ALU.add, r=[ST, bk(3)], w=[ST])
                        P.tt("pool", ST[prt, :, :], ST[prt, :, :], PC[prt, :, c:c + 1].to_broadcast([64, 4, 64]),
                             ALU.mult, r=[ST, PC], w=[ST])
                    P.cp("act", STb[:], ST[:], r=[ST], w=[STb])
                    P.cp("dve", Ysl[c][:], bank(1)[0:64, :], r=[bk(1)], w=[Ysl[c]])
                for c in range(NCB):
                    Ys = Ysl[c]
                    Yv = v3(Ys[:], 8)
                    P.red(gs[:, 0:8], Yv, ALU.add, r=[Ys], w=[gs])
                    P.ts("dve", gs[:, 0:8], gs[:, 0:8], 1.0 / 64.0, ALU.mult, r=[gs], w=[gs])
                    P.tt("dve", Yv, Yv, b3(gs[:, 0:8], 8, 64), ALU.subtract, r=[Ys, gs], w=[Ys])
                    P.tt("pool", Ysq[:], Ys[:], Ys[:], ALU.mult)
                    P.red(gs[:, 8:16], v3(Ysq[:], 8), ALU.add, r=[Ysq], w=[gs])
                    P.ts("dve", gs[:, 8:16], gs[:, 8:16], 1.0 / 64.0, ALU.mult, 64e-5, ALU.add, r=[gs], w=[gs])
                    P.act(gs[:, 8:16], gs[:, 8:16], AF.Sqrt, r=[gs], w=[gs])
                    P.recip(gs[:, 8:16], gs[:, 8:16], r=[gs], w=[gs])
                    P.tt("dve", Yv, Yv, b3(gs[:, 8:16], 8, 64), ALU.mult, r=[Ys, gs], w=[Ys])
                    for hp in range(4):
                        P.tr(bank(2)[:, hp * 64:(hp + 1) * 64], Ys[:, hp * 128:(hp + 1) * 128], idn[0:64, 0:64],
                             r=[Ys, idn], w=[bk(2)])
                    P.cp("act", yT[:, :, c * 64:(c + 1) * 64], v3(bank(2)[:, 0:256], 4), r=[bk(2)], w=[yT])
                if bstop == 6:
                    return
                ct = catT[g % 2]
                for hp in range(4):
                    P.ts("dve", fo[:], yT[:, hp, :], rwc[:, 20 + hp:21 + hp], ALU.mult, rwc[:, 24 + hp:25 + hp], ALU.add,
                         r=[yT, rwc], w=[fo])
                    P.tt("pool", fo[:], fo[:], bonusT[hp][:], ALU.add)
                    P.tt("dve", ct[:, 4 + hp, :], fo[:], gT[hp][:], ALU.mult, r=[fo, gT[hp]], w=[("ct", 0, 1)])
                P.dma("act", ct[:, 0:4, :], ao_d[:, :, g * GB:(g + 1) * GB], reads=[("ao_d", (g * GB) // 512)],
                      writes=[("ct", 0, 0)])
                for t in range(GB // 128):
                    ti = g * (GB // 128) + t
                    xrr = xr[ti % 2]; mx = mix[ti % 2]
                    for half in range(2):
                        for k in range(8):
                            P.mm(DB[0][:, half * 512:(half + 1) * 512], ct[:, k, t * 128:(t + 1) * 128],
                                 wout[:, k, half * 512:(half + 1) * 512], start=(k == 0), stop=(k == 7),
                                 r=[("ct", 0, 0), ("ct", 0, 1), wout], w=[bk(half)])
                    P.dma("sp", xrr[:], xsrc[ti * 128:(ti + 1) * 128, :], reads=[("xcur", ti)])
                    P.tt("dve", mx[:], DB[0][:], G1bc[:], ALU.mult, r=[bk(0), bk(1), G1bc], w=[mx])
                    P.tt("pool", mx[:], mx[:], xrr[:], ALU.add)
                    P.dma("sp", x1_d[ti * 128:(ti + 1) * 128, :], mx[:], writes=[("x1", ti)])

        def pass_c(l):
            h2T = P.sb("h2T", [128, 8, SG], BF16)
            yacc = P.sb("yacc", [128, NTS, D])
            Gm = P.sb("Gm", [128, NTS, NE])
            wr = P.sb("wr", [128, 8, 36]); brbc = P.sb("brbc", [128, 36])
            wg = [P.sb("wg%d" % i, [128, 8, DE], BF16) for i in range(2)]
            wu = [P.sb("wu%d" % i, [128, 8, DE], BF16) for i in range(2)]
            wd = [P.sb("wd%d" % i, [128, 2, D], BF16) for i in range(2)]
            xt = [P.sb("c_xt%d" % i, [128, D]) for i in range(2)]
            xh2 = [P.sb("c_xh%d" % i, [128, D]) for i in range(2)]
            xh = xh2[0]
            st = [P.sb("c_st%d" % i, [128, 4]) for i in range(2)]
            htmp2 = [P.sb("c_htmp%d" % i, [128, 8, 128]) for i in range(2)]
            h2f = [P.sb("c_h2f%d" % i, [128, 8, 128]) for i in range(2)]
            rt2 = []
            for i in range(2):
                rt2.append(dict(lg=P.sb("c_lg%d" % i, [128, 36]), sm=P.sb("c_sm%d" % i, [128, 16]),
                                ge=P.sb("c_ge%d" % i, [128, 4]), gsel=P.sb("c_gsel%d" % i, [128, 4]),
                                pen=P.sb("c_pen%d" % i, [128, 4]), mm_=P.sb("c_m%d" % i, [128, 32]),
                                m2=P.sb("c_m2%d" % i, [128, 32]), sel1=P.sb("c_sel1%d" % i, [128, 32]),
                                sel2=P.sb("c_sel2%d" % i, [128, 32])))
            sil = [P.sb("c_sil%d" % i, [128, 512]) for i in range(2)]
            hid = [P.sb("c_hid%d" % i, [128, 2, 512], BF16) for i in range(2)]
            ob = [P.sb("c_ob%d" % i, [128, D]) for i in range(2)]
            P.dma("sp", wr[:], wr_in[l].rearrange("(k p) n -> p k n", p=128))
            P.dma("sp", brbc[:], br_in[l].partition_broadcast(128))
            for sg in range(NSG):
                for tl in range(NTS):
                    ti = sg * NTS + tl
                    xtt = xt[ti % 2]; hf = h2f[ti % 2]
                    P.dma("sp", xtt[:], x1_d[ti * 128:(ti + 1) * 128, :], reads=[("x1", ti)])
                    norm_transpose(xtt, xh2[ti % 2], st[ti % 2], A2, B2, htmp2[ti % 2], hf[:], out_eng="dve")
                    _r = rt2[ti % 2]
                    lg = _r["lg"]; sm = _r["sm"]; ge = _r["ge"]; gsel = _r["gsel"]; pen = _r["pen"]
                    mm_ = _r["mm_"]; m2 = _r["m2"]; sel1 = _r["sel1"]; sel2 = _r["sel2"]
                    P.cp("act", h2T[:, :, tl * 128:(tl + 1) * 128], hf[:], w=[("h2T", tl // 4)])
                    for k in range(8):
                        P.mm(bank(2)[:, 0:36], hf[:, k, :], wr[:, k, :], start=(k == 0), stop=(k == 7), w=[bk(2)])
                    P.tt("dve", lg[:], bank(2)[:, 0:36], brbc[:], ALU.add, r=[bk(2), brbc], w=[lg])
                    P.red(sm[:, 0:1], lg[:, 0:4], ALU.max, r=[lg], w=[sm])
                    P.ts("dve", sm[:, 1:2], sm[:, 0:1], -1.0, ALU.mult, r=[sm], w=[sm])
                    P.memset("dve", sm[:, 2:3], 0.0, w=[sm])
                    P.act(ge[:], lg[:, 0:4], AF.Exp, bias=sm[:, 1:2], accum=sm[:, 2:3], r=[lg, sm], w=[ge, sm])
                    P.recip(sm[:, 3:4], sm[:, 2:3], r=[sm], w=[sm])
                    P.ts("dve", gsel[:], lg[:, 0:4], sm[:, 0:1], ALU.is_equal, r=[lg, sm], w=[gsel])
                    P.ts("dve", pen[:], gsel[:], 1e30, ALU.mult, -1e30, ALU.add)
                    P.tt("dve", v3(mm_[:], 4), v3(lg[:, 4:36], 4), b3(pen[:], 4, 8), ALU.add, r=[lg, pen], w=[mm_])
                    P.red(sm[:, 4:5], mm_[:], ALU.max, r=[mm_], w=[sm])
                    P.ts("dve", sel1[:], mm_[:], sm[:, 4:5], ALU.is_equal, r=[mm_, sm], w=[sel1])
                    P.stt("dve", m2[:], sel1[:], -1e30, mm_[:], ALU.mult, ALU.add)
                    P.red(sm[:, 5:6], m2[:], ALU.max, r=[m2], w=[sm])
                    P.ts("dve", sel2[:], m2[:], sm[:, 5:6], ALU.is_equal, r=[m2, sm], w=[sel2])
                    P.tt("dve", sm[:, 6:7], sm[:, 5:6], sm[:, 4:5], ALU.subtract, r=[sm], w=[sm])
                    P.act(sm[:, 7:8], sm[:, 6:7], AF.Exp, r=[sm], w=[sm])
                    P.ts("dve", sm[:, 8:9], sm[:, 7:8], 1.0, ALU.add, r=[sm], w=[sm])
                    P.recip(sm[:, 8:9], sm[:, 8:9], r=[sm], w=[sm])
                    P.tt("dve", sm[:, 9:10], sm[:, 8:9], sm[:, 3:4], ALU.mult, r=[sm], w=[sm])
                    P.tt("dve", sm[:, 10:11], sm[:, 3:4], sm[:, 9:10], ALU.subtract, r=[sm], w=[sm])
                    P.ts("dve", Gm[:, tl, :], sel1[:], sm[:, 9:10], ALU.mult, r=[sel1, sm], w=[("Gm", tl)])
                    P.stt("dve", Gm[:, tl, :], sel2[:], sm[:, 10:11], Gm[:, tl, :], ALU.mult, ALU.add,
                          r=[sel2, sm, ("Gm", tl)], w=[("Gm", tl)])
                blocks = [(e, tb) for e in range(NE) for tb in range(SG // 512)]

                def gu(i):
                    e, tb = blocks[i]
                    wgb = wg[e % 2]; wub = wu[e % 2]; wdb = wd[e % 2]
                    if tb == 0:
                        P.dma("pool", wgb[:], wg_in[l, e].rearrange("(k p) f -> p k f", p=128))
                        P.dma("pool", wub[:], wu_in[l, e].rearrange("(k p) f -> p k f", p=128))
                        P.dma("pool", wdb[:], wd_in[l, e].rearrange("(c p) n -> p c n", p=128))
                    hd = hid[i % 2]
                    for fc in range(2):
                        gb = 2 + fc
                        ub = 4 + fc
                        for k in range(8):
                            P.mm(bank(gb), wgb[:, k, fc * 128:(fc + 1) * 128], h2T[:, k, tb * 512:(tb + 1) * 512],
                                 start=(k == 0), stop=(k == 7), r=[wgb, ("h2T", tb)], w=[bk(gb)])
                        for k in range(8):
                            P.mm(bank(ub), wub[:, k, fc * 128:(fc + 1) * 128], h2T[:, k, tb * 512:(tb + 1) * 512],
                                 start=(k == 0), stop=(k == 7), r=[wub, ("h2T", tb)], w=[bk(ub)])
                        P.act(sil[fc][:], bank(gb), AF.Silu, r=[bk(gb)], w=[sil[fc]])
                        P.tt("dve", hd[:, fc, :], sil[fc][:], bank(ub), ALU.mult, r=[sil[fc], bk(ub)], w=[hd])

                def down(i):
                    e, tb = blocks[i]
                    wdb = wd[e % 2]
                    hd = hid[i % 2]
                    for t in range(4):
                        tl = tb * 4 + t
                        dbi = 0 if t % 2 == 0 else 3
                        for half in range(2):
                            for fc in range(2):
                                P.mm(DB[dbi][:, half * 512:(half + 1) * 512], hd[:, fc, t * 128:(t + 1) * 128],
                                     wdb[:, fc, half * 512:(half + 1) * 512], start=(fc == 0), stop=(fc == 1),
                                     w=[bk(2 * dbi + half)])
                        if e == 0:
                            P.ts("dve", yacc[:, tl, :], DB[dbi][:], Gm[:, tl, e:e + 1], ALU.mult,
                                 r=[bk(2 * dbi), bk(2 * dbi + 1), ("Gm", tl)], w=[("yacc", tl)])
                        else:
                            P.stt("dve", yacc[:, tl, :], DB[dbi][:], Gm[:, tl, e:e + 1], yacc[:, tl, :], ALU.mult,
                                  ALU.add, r=[bk(2 * dbi), bk(2 * dbi + 1), ("Gm", tl), ("yacc", tl)],
                                  w=[("yacc", tl)])

                gu(0)
                for i in range(len(blocks)):
                    if i + 1 < len(blocks):
                        gu(i + 1)
                    down(i)
                for tl in range(NTS):
                    ti = sg * NTS + tl
                    xtt = xt[ti % 2]; o = ob[ti % 2]
                    P.dma("sp", xtt[:], x1_d[ti * 128:(ti + 1) * 128, :], reads=[("x1", ti)])
                    P.tt("dve", o[:], yacc[:, tl, :], G2bc[:], ALU.mult, r=[("yacc", tl), G2bc], w=[o])
                    P.tt("pool", o[:], o[:], xtt[:], ALU.add)
                    if l < L - 1 or dbg:
                        P.dma("sp", xcur[ti * 128:(ti + 1) * 128, :], o[:], writes=[("xcur", ti)])
                    if l == L - 1:
                        stt_ = st[ti % 2]
                        rms_rstd(o, xh, stt_)
                        P.ts("dve", xh[:], o[:], stt_[:, 3:4], ALU.mult, r=[o, stt_], w=[xh])
                        P.tt("pool", o[:], xh[:], fgbc[:], ALU.mult)
                        P.dma("sp", y_out[ti * 128:(ti + 1) * 128, :], o[:], writes=[("y", ti)])

        for l in range(L):
            with ExitStack() as sc:
                P.stack = sc
                modulation(l)
            P.barrier()
            if do_a:
                with ExitStack() as sc:
                    P.stack = sc
                    pass_a(l)
                P.barrier()
            if do_b:
                with ExitStack() as sc:
                    P.stack = sc
                    pass_b(l)
                P.barrier()
            if do_c:
                with ExitStack() as sc:
                    P.stack = sc
                    pass_c(l)
                P.barrier()

        fin = [("y", ti) for ti in range(NT)]
        if dbg:
            if do_a:
                P.dma("sp", dbg_outs["d_ao"], ao_d, reads=[("ao_d", g) for g in range(NG)])
            if do_b:
                P.dma("sp", dbg_outs["d_x1"], x1_d, reads=[("x1", ti) for ti in range(NT)])
            if do_c:
                P.dma("sp", dbg_outs["d_x2"], xcur, reads=[("xcur", ti) for ti in range(NT)])
            fin += list(dbg_outs.values())
        P.finish_wait("sp", fin)
        P.emit()
    return nc, P


def host_layout(inp, b, S=SEQ, L=DEPTH):
    f = np.float32
    m = {}
    m["x"] = np.ascontiguousarray(inp["x"][b, :S], dtype=f)
    m["pos"] = np.ascontiguousarray(inp["positions"][b:b + 1, :S], dtype=np.int32)
    m["c_t"] = np.ascontiguousarray(inp["c"][b].reshape(8, 128).T, dtype=f)
    m["ada_w"] = np.ascontiguousarray(inp["ada_w"][:L], dtype=f)
    m["ada_b"] = np.ascontiguousarray(inp["ada_b"][:L].reshape(L, 1, 6 * D), dtype=f)
    m["g1c"] = np.ascontiguousarray(inp["norm1_g"][:L].reshape(L, 8, 128).transpose(0, 2, 1), dtype=f)
    m["g2c"] = np.ascontiguousarray(inp["norm2_g"][:L].reshape(L, 8, 128).transpose(0, 2, 1), dtype=f)
    m["w_in"] = np.ascontiguousarray(inp["w_in"][:L], dtype=f)
    m["w_out"] = np.ascontiguousarray(inp["w_out"][:L], dtype=f)
    m["lamv"] = np.ascontiguousarray(inp["attn_lambda"][:L].reshape(L, 1, 256), dtype=f)
    m["sublng"] = np.ascontiguousarray(inp["attn_subln_g"][:L].reshape(L, 128, 1), dtype=f)
    mu = inp["rwkv_shift_mu"][:L]
    muc = np.zeros((L, 128, 14), f)
    muc[:, :, 0:12] = mu[:, 0:1536].reshape(L, 12, 128).transpose(0, 2, 1)
    muc[:, 0:64, 12] = mu[:, 1536:1600]
    muc[:, 0:96, 13] = mu[:, 1600:1696]
    m["mu_c"] = muc
    cols = []
    for k in ("rwkv_w0", "rwkv_a0", "rwkv_k_k", "rwkv_k_a", "rwkv_r_k", "rwkv_lnx_g", "rwkv_lnx_b"):
        cols.append(inp[k][:L].reshape(L, 4, 128).transpose(0, 2, 1))
    m["rw_cols"] = np.ascontiguousarray(np.concatenate(cols, axis=2), dtype=f)
    m["lw"] = np.ascontiguousarray(np.concatenate([inp["rwkv_w_up"][:L], inp["rwkv_a_up"][:L]], axis=1), dtype=f)
    m["gu"] = np.ascontiguousarray(inp["rwkv_g_up"][:L], dtype=f)
    m["wr"] = np.ascontiguousarray(np.concatenate([inp["moe_w_group"][:L], inp["moe_w_router"][:L]], axis=2), dtype=f)
    m["br"] = np.ascontiguousarray(np.concatenate([inp["moe_b_group"][:L], inp["moe_b_router"][:L]], axis=1).reshape(L, 1, 36), dtype=f)
    m["wg"] = np.ascontiguousarray(inp["moe_w_gate"][:L], dtype=f)
    m["wu"] = np.ascontiguousarray(inp["moe_w_up"][:L], dtype=f)
    m["wd"] = np.ascontiguousarray(inp["moe_w_down"][:L], dtype=f)
    m["fg"] = np.ascontiguousarray(inp["final_g"].reshape(1, D), dtype=f)
    for k, v in host_consts().items():
        m["k_" + k] = v
    return m


_CACHE = {}


def kernel(**inputs):
    inputs = {k: np.asarray(v) for k, v in inputs.items()}
    if "prog" not in _CACHE:
        _CACHE["prog"] = build_program()[0]
    nc = _CACHE["prog"]
    in_maps = [host_layout(inputs, b) for b in range(NCORES)]
    res = run_bass_kernel_spmd(nc, in_maps, core_ids=list(range(NCORES)))
    out = np.stack([np.asarray(res.results[b]["y"], dtype=np.float32) for b in range(NCORES)], axis=0)
    return out
```

```python
import math
from contextlib import ExitStack

import numpy as np
import concourse.bass as bass
import concourse.mybir as mybir
from concourse.bass_utils import run_bass_kernel_spmd

F32 = mybir.dt.float32
BF16 = mybir.dt.bfloat16
I32 = mybir.dt.int32
ALU = mybir.AluOpType
AF = mybir.ActivationFunctionType
AX = mybir.AxisListType

D = 1024
NCORES = 8
SEQ = 4096
DEPTH = 4
INW = 3232
NE = 32
DE = 256
LDC = 0.6065306597126334
ROPE_THETA = 500000.0

ENGS = ("pe", "act", "dve", "pool", "sp")
NDSEM = 12
SEM_ROLL = 30000
ATT_LOOK = 2


class Prog:
    def __init__(self, nc, same_engine_sync=True):
        self.nc = nc
        self.stack = None
        self.q = {e: [] for e in ENGS}
        self.cnt = {e: 0 for e in ENGS}
        self.seen = {e: {} for e in ENGS}
        self.last_w = {}
        self.readers = {}
        self.same = same_engine_sync
        self.esem = {}
        self.dsem = {}
        self.dcnt = {}
        self.dnext = {e: 0 for e in ENGS}
        self.nroll = 0
        self.n_inst = 0

    def setup(self, stack):
        self.root = stack
        self.stack = stack
        nc = self.nc
        for e in ENGS:
            self.esem[e] = stack.enter_context(nc.semaphore("es_" + e))
        for e in ("sp", "act", "pool"):
            self.dsem[e] = [stack.enter_context(nc.semaphore("ds_%s_%d" % (e, j))) for j in range(NDSEM)]
            self.dcnt[e] = [0] * NDSEM

    def sb(self, name, shape, dt=F32):
        self.uid = getattr(self, "uid", 0) + 1
        return self.stack.enter_context(self.nc.sbuf_tensor("%s_%d" % (name, self.uid), list(shape), dt))

    def ps(self, name, shape, dt=F32):
        return self.stack.enter_context(self.nc.psum_tensor(name, list(shape), dt))

    @staticmethod
    def key(x):
        if isinstance(x, (str, tuple)):
            return x
        t = getattr(x, "tensor", x)
        return t.name

    def _deps(self, eng, reads, writes, tokname=None):
        deps = {}
        tokname = tokname or eng

        def add(tok):
            if tok is None:
                return
            s, v, e = tok
            if e == tokname and (eng == "pe" or not self.same):
                return
            if deps.get(id(s), (None, 0))[1] < v:
                deps[id(s)] = (s, v)

        rk = [self.key(r) for r in reads]
        wk = [self.key(w) for w in writes]
        for k in rk:
            if isinstance(k, tuple) and k[0] == "bank" and k not in wk:
                wk.append(k)
        for k in rk:
            add(self.last_w.get(k))
        for k in wk:
            add(self.last_w.get(k))
            for t in self.readers.get(k, ()):
                add(t)
        waits = []
        for sid, (s, v) in deps.items():
            if self.seen[eng].get(sid, 0) < v:
                self.seen[eng][sid] = v
                waits.append((s, v))
        return rk, wk, waits

    def _commit(self, rk, wk, tok):
        for k in wk:
            self.last_w[k] = tok
            self.readers[k] = []
        for k in rk:
            if k not in wk:
                lst = self.readers.setdefault(k, [])
                lst[:] = [t for t in lst if t[0] is not tok[0]]
                lst.append(tok)

    def op(self, eng, fn, reads=(), writes=(), cls=None):
        tokname = eng if cls is None else eng + cls
        rk, wk, waits = self._deps(eng, reads, writes, tokname)
        if self.cnt[eng] >= SEM_ROLL:
            self.nroll += 1
            self.esem[eng] = self.root.enter_context(self.nc.semaphore("es_%s_%d" % (eng, self.nroll)))
            self.cnt[eng] = 0
        self.cnt[eng] += 1
        tok = (self.esem[eng], self.cnt[eng], tokname)
        self.q[eng].append((waits, fn, self.esem[eng], 1))
        self._commit(rk, wk, tok)
        self.n_inst += 1
        return tok

    def dma(self, eng, out, in_, reads=None, writes=None, **kw):
        reads = [in_] if reads is None else reads
        writes = [out] if writes is None else writes
        rk, wk, waits = self._deps(eng, reads, writes)
        j = self.dnext[eng]
        self.dnext[eng] = (j + 1) % NDSEM
        s = self.dsem[eng][j]
        prev = self.dcnt[eng][j]
        if prev > 0 and self.seen[eng].get(id(s), 0) < prev:
            self.seen[eng][id(s)] = prev
            waits.append((s, prev))
        self.dcnt[eng][j] = prev + 16
        tok = (s, prev + 16, "dma_" + eng)
        self.q[eng].append((waits, lambda e: e.dma_start(out=out, in_=in_, **kw), s, 16))
        self._commit(rk, wk, tok)
        self.n_inst += 1
        return tok

    def barrier(self):
        toks = [(self.esem[e], self.cnt[e]) for e in ENGS if self.cnt[e] > 0]
        for e in ("sp", "act", "pool"):
            for j in range(NDSEM):
                if self.dcnt[e][j] > 0:
                    toks.append((self.dsem[e][j], self.dcnt[e][j]))
        for e in ENGS:
            waits = []
            for s, v in toks:
                if s is self.esem[e]:
                    continue
                if self.seen[e].get(id(s), 0) < v:
                    self.seen[e][id(s)] = v
                    waits.append((s, v))
            if waits:
                self.q[e].append((waits, None, None, 0))

    def finish_wait(self, eng, keys):
        rk, wk, waits = self._deps(eng, keys, [])
        self.q[eng].append((waits, None, None, 0))

    def emit(self):
        nc = self.nc
        names = {"pe": "tensor", "act": "scalar", "dve": "vector", "pool": "gpsimd", "sp": "sync"}
        with nc.Block() as block:
            for e in ENGS:
                lst = self.q[e]
                if not lst:
                    continue

                def body(engobj, lst=lst):
                    for waits, fn, sem, inc in lst:
                        for s, v in waits:
                            engobj.wait_ge(s, v)
                        if fn is not None:
                            fn(engobj).then_inc(sem, inc)

                getattr(block, names[e])(body)

    def mm(self, out, lhsT, rhs, start=True, stop=True, r=None, w=None):
        kp = lhsT.partition_size()
        cls = None if kp > 64 else "_%d_%d" % (lhsT.base_partition(), 32 if kp <= 32 else 64)
        return self.op("pe", lambda e: e.matmul(out, lhsT=lhsT, rhs=rhs, start=start, stop=stop),
                       r if r is not None else [lhsT, rhs], w if w is not None else [out], cls=cls)

    def tr(self, out, in_, ident, r=None, w=None):
        kp = in_.partition_size()
        cls = None if kp > 64 else "_%d_%d" % (in_.base_partition(), 32 if kp <= 32 else 64)
        return self.op("pe", lambda e: e.transpose(out, in_, ident),
                       r if r is not None else [in_, ident], w if w is not None else [out], cls=cls)

    def act(self, out, in_, func, bias=None, scale=None, accum=None, r=None, w=None, eng="act"):
        kw = {}
        rr = [in_]
        if bias is not None:
            kw["bias"] = bias
            if not isinstance(bias, (int, float)):
                rr.append(bias)
        if scale is not None:
            kw["scale"] = scale
            if not isinstance(scale, (int, float)):
                rr.append(scale)
        ww = [out]
        if accum is not None:
            kw["accum_out"] = accum
            ww.append(accum)
        return self.op("act", lambda e: e.activation(out=out, in_=in_, func=func, **kw),
                       r if r is not None else rr, w if w is not None else ww)

    def tt(self, eng, out, in0, in1, op, r=None, w=None):
        return self.op(eng, lambda e: e.tensor_tensor(out=out, in0=in0, in1=in1, op=op),
                       r if r is not None else [in0, in1], w if w is not None else [out])

    def ts(self, eng, out, in0, s1, op0, s2=None, op1=None, r=None, w=None):
        rr = [in0]
        for s in (s1, s2):
            if s is not None and not isinstance(s, (int, float)):
                rr.append(s)
        if op1 is None:
            fn = lambda e: e.tensor_scalar(out=out, in0=in0, scalar1=s1, scalar2=None, op0=op0)
        else:
            fn = lambda e: e.tensor_scalar(out=out, in0=in0, scalar1=s1, scalar2=s2, op0=op0, op1=op1)
        return self.op(eng, fn, r if r is not None else rr, w if w is not None else [out])

    def stt(self, eng, out, in0, scalar, in1, op0, op1, r=None, w=None):
        rr = [in0, in1]
        if not isinstance(scalar, (int, float)):
            rr.append(scalar)
        return self.op(eng, lambda e: e.scalar_tensor_tensor(out=out, in0=in0, scalar=scalar, in1=in1, op0=op0, op1=op1),
                       r if r is not None else rr, w if w is not None else [out])

    def cp(self, eng, out, in_, r=None, w=None):
        if eng == "act":
            return self.act(out, in_, AF.Copy, r=r, w=w)
        return self.op(eng, lambda e: e.tensor_copy(out=out, in_=in_),
                       r if r is not None else [in_], w if w is not None else [out])

    def memset(self, eng, out, val, w=None):
        return self.op(eng, lambda e: e.memset(out, val), [], w if w is not None else [out])

    def recip(self, out, in_, r=None, w=None):
        return self.op("dve", lambda e: e.reciprocal(out=out, in_=in_),
                       r if r is not None else [in_], w if w is not None else [out])

    def red(self, out, in_, op, r=None, w=None):
        return self.op("dve", lambda e: e.tensor_reduce(out=out, in_=in_, axis=AX.X, op=op),
                       r if r is not None else [in_], w if w is not None else [out])


def host_consts():
    c = {}
    c["idn"] = np.eye(128, dtype=np.float32)
    rp = np.zeros((128, 128), np.float32)
    for f in range(128):
        d = f % 64
        if d < 8:
            rp[f + 8, f] = 1.0
        elif d < 16:
            rp[f - 8, f] = 1.0
    c["rperm"] = rp
    invf = (ROPE_THETA ** (-np.arange(0, 16, 2, dtype=np.float32) / np.float32(16))).astype(np.float32)
    col = np.zeros((128, 4), np.float32)
    for f in range(128):
        d = f % 64
        if d < 16:
            col[f, 0] = invf[d % 8]
            col[f, 1] = -1.0 if d < 8 else 1.0
    c["ropecol"] = col
    bd = np.zeros((128, 128), np.float32)
    bd[:64, :64] = 1.0
    bd[64:, 64:] = 1.0
    c["bd64"] = bd
    c["ones"] = np.ones((128, 128), np.float32)
    s = np.arange(64)[:, None]
    t = np.arange(64)[None, :]
    m = np.zeros((64, 192), np.float32)
    m[:, 0:64] = (s < t)
    m[:, 64:128] = (s <= t)
    m[:, 128:192] = (t < s)
    c["masks"] = m
    rm = np.ones((128, 512), np.float32)
    rm[:, ::64] = 0.0
    c["rmask"] = rm
    c["idn64x8"] = np.tile(np.eye(64, dtype=np.float32), (1, 8))
    return c


CONST_SHAPES = {"idn": [128, 128], "rperm": [128, 128], "ropecol": [128, 4], "bd64": [128, 128],
                "ones": [128, 128], "masks": [64, 192], "rmask": [128, 512], "idn64x8": [64, 512]}


def lambda_init_of(layer):
    return 0.8 - 0.6 * math.exp(-0.3 * layer)


def build_program(S=SEQ, L=DEPTH, dbg=False, same_sync=True, do_a=True, do_b=True, do_c=True, GB=256, bstop=99):
    assert S % 512 == 0
    NG = S // 512
    NT = S // 128
    SG = min(2048, S)
    NSG = S // SG
    NTS = SG // 128
    NGB = S // GB
    NCB = GB // 64
    nc = bass.Bass("TRN2", target_bir_lowering=False)
    P = Prog(nc, same_engine_sync=same_sync)

    def din(name, shape, dt=F32):
        return nc.dram_tensor(name, list(shape), dt, kind="ExternalInput").ap()

    def dscr(name, shape, dt=F32):
        return nc.dram_tensor(name, list(shape), dt).ap()

    x_in = din("x", [S, D])
    pos_in = din("pos", [1, S], I32)
    c_in = din("c_t", [128, 8])
    ada_w = din("ada_w", [L, D, 6 * D])
    ada_b = din("ada_b", [L, 1, 6 * D])
    g1c = din("g1c", [L, 128, 8])
    g2c = din("g2c", [L, 128, 8])
    w_in = din("w_in", [L, D, INW])
    w_out = din("w_out", [L, D, D])
    lamv = din("lamv", [L, 1, 256])
    sublng = din("sublng", [L, 128, 1])
    mu_c = din("mu_c", [L, 128, 14])
    rw_cols = din("rw_cols", [L, 128, 28])
    lw_in = din("lw", [L, 64, 512])
    gu_in = din("gu", [L, 96, 512])
    wr_in = din("wr", [L, D, 36])
    br_in = din("br", [L, 1, 36])
    wg_in = din("wg", [L, NE, D, DE])
    wu_in = din("wu", [L, NE, D, DE])
    wd_in = din("wd", [L, NE, DE, D])
    fg_in = din("fg", [1, D])
    cst = {k: din("k_" + k, v) for k, v in CONST_SHAPES.items()}
    y_out = nc.dram_tensor("y", [S, D], F32, kind="ExternalOutput").ap()

    xcur = dscr("xcur", [S, D])
    x1_d = dscr("x1_d", [S, D])
    hT_d = dscr("hT_d", [128, 8, S], BF16)
    ao_d = dscr("ao_d", [128, 4, S], BF16)
    tabC_d = dscr("tabC_d", [128, S])
    tabS_d = dscr("tabS_d", [128, S])
    dbg_outs = {}
    if dbg:
        dbg_outs["d_ao"] = nc.dram_tensor("d_ao", [128, 4, S], BF16, kind="ExternalOutput").ap()
        dbg_outs["d_x1"] = nc.dram_tensor("d_x1", [S, D], F32, kind="ExternalOutput").ap()
        dbg_outs["d_x2"] = nc.dram_tensor("d_x2", [S, D], F32, kind="ExternalOutput").ap()

    root = ExitStack()
    with root:
        P.setup(root)
        DB = [P.ps("db%d" % i, [128, 1024]) for i in range(4)]

        def bank(i):
            return DB[i // 2][:, (i % 2) * 512:(i % 2) * 512 + 512]

        def bk(i):
            return ("bank", i)

        idn = P.sb("idn", [128, 128])
        ones = P.sb("ones", [128, 128])
        ones_bf = P.sb("ones_bf", [128, 128], BF16)
        rperm = P.sb("rperm", [128, 128])
        bd64 = P.sb("bd64", [128, 128])
        masks = P.sb("masks", [64, 192])
        rmask = P.sb("rmask", [128, 512])
        idn8 = P.sb("idn8", [64, 512])
        ropecol = P.sb("ropecol", [128, 4])
        onesm = P.sb("onesm", [128, 128])
        cact = P.sb("cact", [128, 8])
        fgbc = P.sb("fgbc", [128, D])
        for t, k in ((idn, "idn"), (ones, "ones"), (rperm, "rperm"), (bd64, "bd64"), (masks, "masks"),
                     (rmask, "rmask"), (idn8, "idn64x8"), (ropecol, "ropecol")):
            P.dma("sp", t[:], cst[k])
        P.cp("dve", ones_bf[:], ones[:])
        P.ts("dve", onesm[:], ones[:], 1.0 / 128.0, ALU.mult)
        P.dma("sp", cact[:], c_in)
        P.act(cact[:], cact[:], AF.Silu)
        P.dma("sp", fgbc[:], fg_in.partition_broadcast(128))
        A1 = P.sb("A1", [128, 8]); B1 = P.sb("B1", [128, 8])
        A2 = P.sb("A2", [128, 8]); B2 = P.sb("B2", [128, 8])
        G1bc = P.sb("G1bc", [128, D]); G2bc = P.sb("G2bc", [128, D])
        lam = P.sb("lam", [128, 4])
        sgcol = P.sb("sgcol", [128, 1])

        def b3(ap, a, b):
            return ap.unsqueeze(2).to_broadcast([ap.shape[0], a, b])

        def v3(ap, a):
            return ap.rearrange("p (a b) -> p a b", a=a)

        def rms_rstd(xtt, junk, stt_):
            P.memset("pool", stt_[:, 0:1], 0.0, w=[stt_])
            P.act(junk[:], xtt[:], AF.Square, accum=stt_[:, 0:1], r=[xtt, stt_], w=[junk, stt_])
            P.ts("dve", stt_[:, 1:2], stt_[:, 0:1], 1.0 / D, ALU.mult, 1e-6, ALU.add, r=[stt_], w=[stt_])
            P.act(stt_[:, 2:3], stt_[:, 1:2], AF.Sqrt, r=[stt_], w=[stt_])
            P.recip(stt_[:, 3:4], stt_[:, 2:3], r=[stt_], w=[stt_])

        def norm_transpose(xtt, xhh, stt_, Acol, Bcol, htmp, out3, out_eng="pool"):
            rms_rstd(xtt, xhh, stt_)
            P.ts("dve", xhh[:], xtt[:], stt_[:, 3:4], ALU.mult, r=[xtt, stt_], w=[xhh])
            for k in range(8):
                P.tr(DB[0][:, k * 128:(k + 1) * 128], xhh[:, k * 128:(k + 1) * 128], idn[:], w=[bk(k // 4)])
            pv = v3(DB[0][:], 8)
            P.tt("dve", htmp[:], pv, b3(Acol[:], 8, 128), ALU.mult, r=[bk(0), bk(1), Acol], w=[htmp])
            P.tt(out_eng, out3, htmp[:], b3(Bcol[:], 8, 128), ALU.add, r=[htmp, Bcol], w=[out3])

        with ExitStack() as sc:
            P.stack = sc
            posi = P.sb("posi", [128, S], I32)
            ang = P.sb("ang", [128, S])
            kf_ = P.sb("kf_", [128, S])
            ki_ = P.sb("ki_", [128, S], I32)
            kg_ = P.sb("kg_", [128, S])
            P.dma("sp", posi[:], pos_in.partition_broadcast(128))
            P.cp("dve", ang[:], posi[:])
            P.ts("pool", ang[:], ang[:], ropecol[:, 0:1], ALU.mult)
            for which, shift, dst in (("sin", 0.5, tabS_d), ("cos", 0.75, tabC_d)):
                P.ts("dve", kf_[:], ang[:], 1.0 / (2.0 * math.pi), ALU.mult, shift, ALU.add)
                P.cp("dve", ki_[:], kf_[:])
                P.cp("pool", kg_[:], ki_[:])
                P.tt("dve", kf_[:], kf_[:], kg_[:], ALU.subtract)
                P.ts("pool", kg_[:], kf_[:], 0.0, ALU.is_lt)
                P.tt("dve", kf_[:], kf_[:], kg_[:], ALU.add)
                P.ts("dve", kf_[:], kf_[:], 2.0 * math.pi, ALU.mult, -math.pi, ALU.add)
                P.ts("dve", kf_[:], kf_[:], 3.14159, ALU.min, -3.14159, ALU.max)
                P.act(kf_[:], kf_[:], AF.Sin)
                if which == "sin":
                    P.ts("dve", kf_[:], kf_[:], ropecol[:, 1:2], ALU.mult)
                P.dma("sp", dst, kf_[:])
        P.barrier()

        def modulation(l):
            linit = lambda_init_of(l)
            awb = [P.sb("awb%d" % i, [128, 8, 512]) for i in range(2)]
            brow = P.sb("brow", [1, 6 * D])
            mrow = P.sb("mrow", [1, 512])
            colt = P.sb("colt", [128, 48])
            gc1 = P.sb("gc1", [128, 8]); gc2 = P.sb("gc2", [128, 8])
            lrow = P.sb("lrow", [128, 256]); ltmp = P.sb("ltmp", [128, 128]); lsum = P.sb("lsum", [128, 2])
            sgl = P.sb("sgl", [128, 1])
            P.dma("sp", brow[:], ada_b[l])
            P.dma("sp", gc1[:], g1c[l]); P.dma("sp", gc2[:], g2c[l])
            awv = ada_w[l].rearrange("(k p) n -> p k n", p=128)
            for cc in range(12):
                wb = awb[cc % 2]
                P.dma("sp" if cc % 2 == 0 else "act", wb[:], awv[:, :, cc * 512:(cc + 1) * 512])
                for k in range(8):
                    P.mm(bank(0)[0:1, :], cact[:, k:k + 1], wb[:, k, :], start=(k == 0), stop=(k == 7), w=[bk(0)])
                P.tt("dve", mrow[:], bank(0)[0:1, :], brow[:, cc * 512:(cc + 1) * 512], ALU.add,
                     r=[bk(0), brow], w=[mrow])
                if cc in (4, 5, 10, 11):
                    P.mm(bank(1), ones[0:1, :], mrow[:], w=[bk(1)])
                    dstg = G1bc if cc < 6 else G2bc
                    off = (cc % 2) * 512
                    P.cp("act", dstg[:, off:off + 512], bank(1), r=[bk(1)], w=[dstg])
                else:
                    for j in range(4):
                        P.mm(bank(1)[:, j:j + 1], mrow[0:1, j * 128:(j + 1) * 128], ones[0:1, 0:1], w=[bk(1)])
                    P.cp("act", colt[:, cc * 4:cc * 4 + 4], bank(1)[:, 0:4], r=[bk(1)], w=[colt])
            P.stt("dve", A1[:], colt[:, 8:16], 1.0, gc1[:], ALU.add, ALU.mult)
            P.cp("dve", B1[:], colt[:, 0:8])
            P.stt("dve", A2[:], colt[:, 32:40], 1.0, gc2[:], ALU.add, ALU.mult)
            P.cp("dve", B2[:], colt[:, 24:32])
            P.dma("sp", lrow[:], lamv[l].partition_broadcast(128))
            lv = lrow[:].rearrange("o (a b d) -> o a b d", a=2, b=2)
            P.tt("dve", v3(ltmp[:], 2), lv[:, :, 0, :], lv[:, :, 1, :], ALU.mult, r=[lrow], w=[ltmp])
            P.red(lsum[:], v3(ltmp[:], 2), ALU.add, r=[ltmp], w=[lsum])
            P.act(lsum[:], lsum[:], AF.Exp)
            P.tt("dve", lam[:, 0:1], lsum[:, 0:1], lsum[:, 1:2], ALU.subtract, r=[lsum], w=[lam])
            P.ts("dve", lam[:, 0:1], lam[:, 0:1], float(linit), ALU.add)
            P.dma("sp", sgl[:], sublng[l])
            P.ts("dve", sgcol[:], sgl[:], float(1.0 - linit), ALU.mult)

        def pass_a(l):
            xsrc = x_in if l == 0 else xcur
            wqkv = P.sb("wqkv", [128, 8, 1536], BF16)
            kc = P.sb("kc", [128, 4, S], BF16)
            vc = P.sb("vc", [128, NT, 512], BF16)
            xt = [P.sb("a_xt%d" % i, [128, D]) for i in range(2)]
            xh = [P.sb("a_xh%d" % i, [128, D]) for i in range(2)]
            st = [P.sb("a_st%d" % i, [128, 4]) for i in range(2)]
            htmp = [P.sb("a_htmp%d" % i, [128, 8, 128]) for i in range(2)]
            hT = [P.sb("a_hT%d" % i, [128, 8, 512], BF16) for i in range(2)]
            tC = [P.sb("a_tC%d" % i, [128, 512]) for i in range(2)]
            tS = [P.sb("a_tS%d" % i, [128, 512]) for i in range(2)]
            qf = [P.sb("a_qf%d" % i, [128, 512]) for i in range(2)]
            t1 = [P.sb("a_t1%d" % i, [128, 512]) for i in range(2)]
            t2 = [P.sb("a_t2%d" % i, [128, 512]) for i in range(2)]
            qz = [P.sb("a_qz%d" % i, [128, 4, 512], BF16) for i in range(2)]
            rsb = P.sb("a_rsb", [128, 512])
            PT = [P.sb("a_PT%d" % i, [128, 512], BF16) for i in range(4)]
            rs = P.sb("a_rs", [1, 512])
            bcs = P.sb("a_bcs", [128, 512])
            om = [P.sb("a_om%d" % i, [128, 512]) for i in range(2)]
            osq = P.sb("a_osq", [128, 512])
            rstd = P.sb("a_rstd", [128, 512])
            aoT = [P.sb("a_aoT%d" % i, [128, 4, 512], BF16) for i in range(2)]
            wv = w_in[l].rearrange("(k p) n -> p k n", p=128)
            P.dma("pool", wqkv[:], wv[:, :, 0:1536])
            P.memset("pool", qz[0][64:128, :, :], 0.0, w=[qz[0]])
            P.memset("pool", qz[1][0:64, :, :], 0.0, w=[qz[1]])
            for g in range(NG):
                hTg = hT[g % 2]
                for t in range(4):
                    ti = g * 4 + t
                    xtt = xt[ti % 2]
                    P.dma("sp", xtt[:], xsrc[ti * 128:(ti + 1) * 128, :], reads=[("xcur", ti)])
                    norm_transpose(xtt, xh[ti % 2], st[ti % 2], A1, B1, htmp[ti % 2], hTg[:, :, t * 128:(t + 1) * 128])
                P.dma("sp", hT_d[:, :, g * 512:(g + 1) * 512], hTg[:], writes=[("hT_d", g)])
                tCg = tC[g % 2]; tSg = tS[g % 2]
                P.dma("act", tCg[:], tabC_d[:, g * 512:(g + 1) * 512])
                P.dma("act", tSg[:], tabS_d[:, g * 512:(g + 1) * 512])
                for c in range(8):
                    pb = 2 + (c % 2)
                    sb2 = 4 + (c % 2)
                    for k in range(8):
                        P.mm(bank(pb), wqkv[:, k, c * 128:(c + 1) * 128], hTg[:, k, :], start=(k == 0),
                             stop=(k == 7), w=[bk(pb)])
                    qff = qf[c % 2]; t1c = t1[c % 2]; t2c = t2[c % 2]
                    P.cp("act", qff[:], bank(pb), r=[bk(pb)], w=[qff])
                    P.mm(bank(sb2), rperm[:], qff[:], w=[bk(sb2)])
                    P.tt("pool", t1c[:], qff[:], tCg[:], ALU.mult)
                    P.tt("dve", t2c[:], bank(sb2), tSg[:], ALU.mult, r=[bk(sb2), tSg], w=[t2c])
                    if c < 4:
                        P.tt("pool", qz[0][0:64, c, :], t1c[0:64, :], t2c[0:64, :], ALU.add, r=[t1c, t2c], w=[qz[0]])
                        P.tt("pool", qz[1][64:128, c, :], t1c[64:128, :], t2c[64:128, :], ALU.add, r=[t1c, t2c], w=[qz[1]])
                    else:
                        P.tt("pool", kc[:, c - 4, g * 512:(g + 1) * 512], t1c[:], t2c[:], ALU.add, w=[("kc", g)])
                for t in range(4):
                    pb = 2 + (t % 2)
                    for k in range(8):
                        P.mm(bank(pb), hTg[:, k, t * 128:(t + 1) * 128], wqkv[:, k, 1024:1536], start=(k == 0),
                             stop=(k == 7), w=[bk(pb)])
                    P.cp("act", vc[:, g * 4 + t, :], bank(pb), r=[bk(pb)], w=[("vc", g)])
                aog = aoT[g % 2]
                nkt = 4 * g + 4
                steps = [(h, m, j) for h in range(4) for m in range(2) for j in range(nkt)]
                SBK = [4, 5, 2, 3]

                def qk(i):
                    h, m, j = steps[i]
                    prt = slice(64 * m, 64 * m + 64)
                    jl = j - 4 * g
                    qs = 128 * jl if jl > 0 else 0
                    sb_ = SBK[i % 4]
                    PTt = PT[i % 4]
                    P.mm(bank(sb_)[:, qs:512], kc[:, h, j * 128:(j + 1) * 128], qz[m][:, h, qs:512],
                         r=[("kc", j // 4), qz[m]], w=[bk(sb_)])
                    P.act(PTt[:, qs:512], bank(sb_)[:, qs:512], AF.Exp, scale=0.125, r=[bk(sb_)], w=[PTt])
                    if jl >= 0:
                        P.memset("pool", PTt[64:128, qs:qs + 64], 0.0, w=[PTt])

                def pv(i):
                    h, m, j = steps[i]
                    jl = j - 4 * g
                    qs = 128 * jl if jl > 0 else 0
                    PTt = PT[i % 4]
                    P.mm(bank(6)[:, qs:512], vc[:, j, h * 128:(h + 1) * 128], PTt[:, qs:512],
                         start=(j == 0), stop=(j == nkt - 1), r=[("vc", j // 4), PTt], w=[bk(6)])
                    P.mm(bank(7)[:, qs:512], ones_bf[:, :], PTt[:, qs:512],
                         start=(j == 0), stop=(j == nkt - 1), r=[PTt], w=[bk(7)])
                    if j != nkt - 1:
                        return
                    P.recip(rsb[:], bank(7), r=[bk(7)], w=[rsb])
                    if m == 1:
                        P.ts("dve", rsb[:], rsb[:], lam[:, 0:1], ALU.mult)
                    P.tt("dve", om[m][:], bank(6), rsb[:], ALU.mult, r=[bk(6), rsb], w=[om[m]])
                    if m == 0:
                        return
                    P.tt("pool", om[0][:], om[0][:], om[1][:], ALU.subtract)
                    P.tt("pool", osq[:], om[0][:], om[0][:], ALU.mult)
                    P.mm(bank(1), onesm[:], osq[:], w=[bk(1)])
                    P.ts("dve", rstd[:], bank(1), 1e-5, ALU.add, r=[bk(1)], w=[rstd])
                    P.act(rstd[:], rstd[:], AF.Sqrt)
                    P.recip(rstd[:], rstd[:])
                    P.tt("dve", om[0][:], om[0][:], rstd[:], ALU.mult)
                    P.ts("dve", aog[:, h, :], om[0][:], sgcol[:, 0:1], ALU.mult, r=[om[0], sgcol], w=[aog])

                LOOK = ATT_LOOK
                for i in range(len(steps) + LOOK):
                    if i < len(steps):
                        qk(i)
                    if i >= LOOK:
                        pv(i - LOOK)
                P.dma("sp", ao_d[:, :, g * 512:(g + 1) * 512], aog[:], writes=[("ao_d", g)])

        def pass_b(l):
            xsrc = x_in if l == 0 else xcur
            wrw = P.sb("wrw", [128, 8, 1696], BF16)
            wout = P.sb("wout", [128, 8, D], BF16)
            LW = P.sb("LW", [64, 512]); GU = P.sb("GU", [96, 512])
            muc = P.sb("muc", [128, 14]); rwc = P.sb("rwc", [128, 28]); omka = P.sb("omka", [128, 4])
            ST = P.sb("ST", [128, 4, 64])
            bnd = P.sb("bnd", [128, 14])
            hTb = [P.sb("b_hT%d" % i, [128, 8, GB], BF16) for i in range(2)]
            pcb = [P.sb("b_pc%d" % i, [128, GB + 1]) for i in range(2)]
            lt = P.sb("b_lt", [128, GB])
            PMr = P.sb("PMr", [128, GB]); PMk = P.sb("PMk", [128, GB]); PMv = P.sb("PMv", [128, 4, GB])
            PL1 = P.sb("PL1", [64, GB]); PL2 = P.sb("PL2", [96, GB])
            T = [P.sb("b_T%d" % i, [128, GB]) for i in range(8)]
            gT = [P.sb("b_gT%d" % i, [128, GB]) for i in range(4)]
            bonusT = [P.sb("b_bo%d" % i, [128, GB]) for i in range(4)]
            ARt = P.sb("ARt", [128, 4, NCB, 2, 64], BF16)
            BKt = P.sb("BKt", [128, 4, NCB, 2, 64])
            BKb = P.sb("BKb", [128, 4, NCB, 2, 64], BF16)
            STb = P.sb("STb", [128, 4, 64], BF16)
            PC = P.sb("PC", [128, 4, NCB])
            Vtok = [P.sb("Vtok%d" % i, [64, 512], BF16) for i in range(NCB)]
            Btok = [P.sb("Btok%d" % i, [64, 512], BF16) for i in range(NCB)]
            Ktok = [P.sb("Ktok%d" % i, [64, 512], BF16) for i in range(NCB)]
            M1 = [P.sb("M1_%d" % i, [64, 8, 128], BF16) for i in range(NCB)]; M2 = [P.sb("M2_%d" % i, [64, 8, 128], BF16) for i in range(NCB)]
            X = [P.sb("Xm_%d" % i, [64, 8, 64], BF16) for i in range(NCB)]; YT = [P.sb("YT_%d" % i, [64, 8, 128], BF16) for i in range(NCB)]
            X0s = P.sb("X0s", [64, 512], BF16); Us = P.sb("Us", [64, 512], BF16)
            Ysl = [P.sb("Ys_%d" % i, [64, 512]) for i in range(NCB)]; Ysq = P.sb("Ysq", [64, 512]); gs = P.sb("gs", [64, 16])
            yT = P.sb("yT", [128, 4, GB])
            fo = P.sb("fo", [128, GB])
            catT = [P.sb("catT%d" % i, [128, 8, GB], BF16) for i in range(1)] * 2
            xr = [P.sb("b_xr%d" % i, [128, D]) for i in range(1)] * 2
            mix = [P.sb("b_mix%d" % i, [128, D]) for i in range(1)] * 2
            wv = w_in[l].rearrange("(k p) n -> p k n", p=128)
            P.dma("pool", wrw[:], wv[:, :, 1536:INW])
            P.dma("pool", wout[:], w_out[l].rearrange("(k p) n -> p k n", p=128))
            P.dma("sp", LW[:], lw_in[l]); P.dma("sp", GU[:], gu_in[l])
            P.dma("sp", muc[:], mu_c[l]); P.dma("sp", rwc[:], rw_cols[l])
            P.ts("dve", omka[:], rwc[:, 12:16], -1.0, ALU.mult, 1.0, ALU.add)
            P.memset("pool", ST[:], 0.0)
            P.memset("pool", STb[:], 0.0)
            P.memset("pool", bnd[:], 0.0)
            rmk = rmask[:, 0:GB]

            def proj_chunk(hTg, c, dst, nchunk):
                if c < 12:
                    cols = slice(c * 128, (c + 1) * 128); M = 128
                elif c == 12:
                    cols = slice(1536, 1600); M = 64
                else:
                    cols = slice(1600, 1696); M = 96
                pb = nchunk % 2
                pc = pcb[nchunk % 2]
                for k in range(8):
                    P.mm(bank(pb)[0:M, 0:GB], wrw[:, k, cols], hTg[:, k, :], start=(k == 0), stop=(k == 7), w=[bk(pb)])
                P.cp("act", pc[0:M, 1:GB + 1], bank(pb)[0:M, 0:GB], r=[bk(pb)], w=[pc])
                P.cp("act", pc[0:M, 0:1], bnd[0:M, c:c + 1], r=[bnd], w=[pc])
                P.tt("pool", lt[0:M, :], pc[0:M, 0:GB], pc[0:M, 1:GB + 1], ALU.subtract, r=[pc], w=[lt])
                P.stt("dve", dst, lt[0:M, :], muc[0:M, c:c + 1], pc[0:M, 1:GB + 1], ALU.mult, ALU.add,
                      r=[lt, muc, pc], w=[dst])
                P.cp("act", bnd[0:M, c:c + 1], pc[0:M, GB:GB + 1], r=[pc], w=[bnd])

            if bstop == 0:
                return
            nch = 0
            for g in range(NGB):
                hTg = hTb[g % 2]
                P.dma("sp", hTg[:], hT_d[:, :, g * GB:(g + 1) * GB], reads=[("hT_d", (g * GB) // 512)])
                proj_chunk(hTg, 12, PL1[:], nch); nch += 1
                proj_chunk(hTg, 13, PL2[:], nch); nch += 1
                if bstop == 1:
                    return
                P.act(PL1[0:32, :], PL1[0:32, :], AF.Tanh)
                P.act(PL2[:], PL2[:], AF.Sigmoid)
                for hp in range(4):
                    proj_chunk(hTg, hp, PMr[:], nch); nch += 1
                    proj_chunk(hTg, 4 + hp, PMk[:], nch); nch += 1
                    proj_chunk(hTg, 8 + hp, PMv[:, hp, :], nch); nch += 1
                    cs = slice(hp * 128, (hp + 1) * 128)

                    def col(pi, hp=hp):
                        return rwc[:, pi * 4 + hp:pi * 4 + hp + 1]
                    ta, tb_, tc_, td, te, tf, tg, th = T
                    r_ = PMr[:]; k_ = PMk[:]; v_ = PMv[:, hp, :]
                    P.mm(bank(2)[:, 0:GB], LW[0:32, cs], PL1[0:32, :], w=[bk(2)])
                    P.act(ta[:], bank(2)[:, 0:GB], AF.Sigmoid, bias=col(0), r=[bk(2), rwc], w=[ta])
                    P.op("dve", lambda e, o=tb_, a=ta: e.tensor_tensor_scan(out=o[:], data0=rmk, data1=a[:], initial=0.0,
                                                                          op0=ALU.mult, op1=ALU.add), [rmask, ta], [tb_])
                    P.act(tc_[:], tb_[:], AF.Exp, scale=-LDC)
                    P.act(td[:], tb_[:], AF.Exp, scale=LDC)
                    P.tt("pool", te[:], tb_[:], ta[:], ALU.subtract)
                    P.act(te[:], te[:], AF.Exp, scale=-LDC)
                    P.mm(bank(3)[:, 0:GB], LW[32:64, cs], PL1[32:64, :], w=[bk(3)])
                    P.act(tf[:], bank(3)[:, 0:GB], AF.Sigmoid, bias=col(1), r=[bk(3), rwc], w=[tf])
                    P.mm(bank(2)[:, 0:GB], GU[0:96, cs], PL2[0:96, :], w=[bk(2)])
                    P.cp("act", gT[hp][:], bank(2)[:, 0:GB], r=[bk(2)], w=[gT[hp]])
                    P.ts("dve", tg[:], k_, col(2), ALU.mult, r=[PMk, rwc], w=[tg])
                    P.act(th[:], tg[:], AF.Square)
                    P.mm(bank(3)[:, 0:GB], bd64[:], th[:], w=[bk(3)])
                    P.act(th[:], bank(3)[:, 0:GB], AF.Sqrt, r=[bk(3)], w=[th])
                    P.ts("dve", th[:], th[:], 1e-12, ALU.max)
                    P.recip(th[:], th[:])
                    P.tt("pool", tg[:], tg[:], th[:], ALU.mult)
                    P.ts("dve", th[:], tf[:], col(3), ALU.mult, omka[:, hp:hp + 1], ALU.add, r=[tf, rwc, omka], w=[th])
                    P.tt("pool", th[:], k_, th[:], ALU.mult, r=[PMk, th], w=[th])
                    P.tt("pool", ta[:], tg[:], tf[:], ALU.mult)
                    P.stt("dve", tb_[:], r_, col(4), th[:], ALU.mult, ALU.mult, r=[PMr, rwc, th], w=[tb_])
                    P.mm(bank(2)[:, 0:GB], bd64[:], tb_[:], w=[bk(2)])
                    P.tt("dve", bonusT[hp][:], bank(2)[:, 0:GB], v_, ALU.mult, r=[bk(2), PMv], w=[bonusT[hp]])
                    P.stt("dve", ARt[:, hp, :, 0, :], v3(tg[:], NCB), -1.0, v3(te[:], NCB), ALU.mult, ALU.mult,
                          r=[tg, te], w=[("ARt", hp)])
                    P.tt("pool", ARt[:, hp, :, 1, :], v3(r_, NCB), v3(tc_[:], NCB), ALU.mult, r=[PMr, tc_], w=[("ARt", hp)])
                    P.tt("pool", BKt[:, hp, :, 0, :], v3(ta[:], NCB), v3(td[:], NCB), ALU.mult, r=[ta, td], w=[("BKt", hp)])
                    P.tt("dve", BKt[:, hp, :, 1, :], v3(th[:], NCB), v3(td[:], NCB), ALU.mult, r=[th, td], w=[("BKt", hp)])
                    P.cp("act", PC[:, hp, :], v3(tc_[:], NCB)[:, :, 63], r=[tc_], w=[PC])
                    P.cp("act", BKb[:, hp, :, :, :], BKt[:, hp, :, :, :], r=[("BKt", hp)], w=[("BKb", hp)])

                if bstop == 2:
                    return
                for c in range(NCB):
                    Vt = Vtok[c]; Bt = Btok[c]; Kt = Ktok[c]
                    XK2 = [("X", c, 0), ("X", c, 1)]
                    YK2 = [("Y", c, 0), ("Y", c, 1)]
                    TK2 = [("TTb", c, 0), ("TTb", c, 1)]
                    T32K = [("TT32", c, 0), ("TT32", c, 1)]
                    for hp in range(4):
                        P.tr(bank(3)[0:64, hp * 128:(hp + 1) * 128], PMv[:, hp, c * 64:(c + 1) * 64], idn[:],
                             r=[PMv, idn], w=[bk(3)])
                        P.tr(bank(0)[0:64, hp * 128:(hp + 1) * 128], BKt[:, hp, c, 0, :], idn[:],
                             r=[("BKt", hp), idn], w=[bk(0)])
                        P.tr(bank(1)[0:64, hp * 128:(hp + 1) * 128], BKt[:, hp, c, 1, :], idn[:],
                             r=[("BKt", hp), idn], w=[bk(1)])
                    P.cp("act", Vt[:], bank(3)[0:64, :], r=[bk(3)], w=[Vt])
                    P.cp("dve", Bt[:], bank(0)[0:64, :], r=[bk(0)], w=[Bt])
                    P.cp("act", Kt[:], bank(1)[0:64, :], r=[bk(1)], w=[Kt])
                    for h in range(8):
                        hp, j = divmod(h, 2)
                        prt = slice(64 * j, 64 * j + 64)
                        arr = ARt[prt, hp, c, :, :].rearrange("p a t -> p (a t)")
                        P.mm(DB[2][0:64, h * 128:(h + 1) * 128], BKb[prt, hp, c, 0, :], arr,
                             r=[("BKb", hp), ("ARt", hp)], w=[bk(4 + h // 4)])
                        P.mm(DB[3][0:64, h * 128:(h + 1) * 128], BKb[prt, hp, c, 1, :], arr,
                             r=[("BKb", hp), ("ARt", hp)], w=[bk(6 + h // 4)])
                        P.mm(bank(2)[0:64, h * 64:(h + 1) * 64], ARt[prt, hp, c, 0, :], BKb[prt, hp, c, 0, :],
                             r=[("BKb", hp), ("ARt", hp)], w=[bk(2)])
                    mk2 = masks[:, 0:128].unsqueeze(1).to_broadcast([64, 8, 128])
                    mk3 = masks[:, 128:192].unsqueeze(1).to_broadcast([64, 8, 64])
                    P.tt("dve", M1[c][:], v3(DB[2][0:64, :], 8), mk2, ALU.mult, r=[bk(4), bk(5), masks], w=[M1[c]])
                    P.tt("dve", M2[c][:], v3(DB[3][0:64, :], 8), mk2, ALU.mult, r=[bk(6), bk(7), masks], w=[M2[c]])
                    P.tt("dve", X[c][:], v3(bank(2)[0:64, :], 8), mk3, ALU.mult, r=[bk(2), masks], w=XK2)
                    P.cp("act", YT[c][:, :, 0:64], M1[c][:, :, 0:64], r=[M1[c]], w=YK2)
                    P.cp("pool", YT[c][:, :, 64:128], v3(idn8[:], 8), r=[idn8], w=TK2)
                for rnd in range(NCB // 2):
                    streams = [(2 * rnd + ci, half) for ci in range(2) for half in range(2)]
                    for lvl in range(6):
                        last = lvl == 5
                        for si, (c, half) in enumerate(streams):
                            pyb = 2 + si
                            pxb = 6 + si // 2
                            pxo = (si % 2) * 256
                            kx = ("X", c, half); ky = ("Y", c, half); kt = ("TTb", c, half)
                            for hh in range(4):
                                h = half * 4 + hh
                                if not last:
                                    P.mm(bank(pyb)[0:64, hh * 128:(hh + 1) * 128], X[c][:, h, :], YT[c][:, h, :],
                                         r=[kx, ky, kt], w=[bk(pyb)])
                                    P.mm(bank(pxb)[0:64, pxo + hh * 64:pxo + (hh + 1) * 64], YT[c][:, h, 0:64], X[c][:, h, :],
                                         r=[kx, ky], w=[bk(pxb)])
                                else:
                                    P.mm(bank(pyb)[0:64, hh * 128 + 64:(hh + 1) * 128], X[c][:, h, :], YT[c][:, h, 64:128],
                                         r=[kx, kt], w=[bk(pyb)])
                        for si, (c, half) in enumerate(streams):
                            pyb = 2 + si
                            pxb = 6 + si // 2
                            pxo = (si % 2) * 256
                            kx = ("X", c, half); ky = ("Y", c, half); kt = ("TTb", c, half); k32 = ("TT32", c, half)
                            hsl = slice(half * 4, half * 4 + 4)
                            PYv = v3(bank(pyb)[0:64, :], 4)
                            if not last:
                                P.cp("act", YT[c][:, hsl, 0:64], PYv[:, :, 0:64], r=[bk(pyb)], w=[ky])
                            P.tt("dve", YT[c][:, hsl, 64:128], YT[c][:, hsl, 64:128], PYv[:, :, 64:128], ALU.add,
                                 r=[kt, bk(pyb)], w=[kt])
                            if not last:
                                P.cp("act", X[c][:, hsl, :], v3(bank(pxb)[0:64, pxo:pxo + 256], 4), r=[bk(pxb)], w=[kx])
                for c in range(NCB):
                    Vt = Vtok[c]; Bt = Btok[c]; Kt = Ktok[c]
                    TK2 = [("TTb", c, 0), ("TTb", c, 1)]
                    for h in range(8):
                        hp, j = divmod(h, 2)
                        prt = slice(64 * j, 64 * j + 64)
                        hs = slice(h * 64, (h + 1) * 64)
                        P.mm(bank(3)[0:64, hs], ARt[prt, hp, c, 0, :], STb[prt, hp, :], start=True, stop=False,
                             r=[("ARt", hp), STb], w=[bk(3)])
                        P.mm(bank(3)[0:64, hs], M2[c][:, h, 0:64], Vt[:, hs], start=False, stop=True, w=[bk(3)])
                    P.cp("act", X0s[:], bank(3)[0:64, :], r=[bk(3)], w=[X0s])
                    for h in range(8):
                        hs = slice(h * 64, (h + 1) * 64)
                        P.mm(bank(0)[0:64, hs], YT[c][:, h, 64:128], X0s[:, hs], r=TK2 + [X0s], w=[bk(0)])
                    P.cp("dve", Us[:], bank(0)[0:64, :], r=[bk(0)], w=[Us])
                    for h in range(8):
                        hp, j = divmod(h, 2)
                        prt = slice(64 * j, 64 * j + 64)
                        hs = slice(h * 64, (h + 1) * 64)
                        P.mm(bank(1)[0:64, hs], ARt[prt, hp, c, 1, :], STb[prt, hp, :], start=True, stop=False,
                             r=[("ARt", hp), STb], w=[bk(1)])
                        P.mm(bank(1)[0:64, hs], M1[c][:, h, 64:128], Us[:, hs], start=False, stop=False, w=[bk(1)])
                        P.mm(bank(1)[0:64, hs], M2[c][:, h, 64:128], Vt[:, hs], start=False, stop=True, w=[bk(1)])
                        P.mm(bank(3)[:, hs], Bt[:, hp * 128:(hp + 1) * 128], Us[:, hs], start=True, stop=False, w=[bk(3)])
                        P.mm(bank(3)[:, hs], Kt[:, hp * 128:(hp + 1) * 128], Vt[:, hs], start=False, stop=True, w=[bk(3)])
                    for j in range(2):
                        prt = slice(64 * j, 64 * j + 64)
                        psv = bank(3)[prt, :].rearrange("p (hp jj v) -> p hp jj v", hp=4, jj=2)[:, :, j, :]
                        P.tt("dve", ST[prt, :, :], ST[prt, :, :], psv, ALU.add, r=[ST, bk(3)], w=[ST])
                        P.tt("pool", ST[prt, :, :], ST[prt, :, :], PC[prt, :, c:c + 1].to_broadcast([64, 4, 64]),
                             ALU.mult, r=[ST, PC], w=[ST])
                    P.cp("act", STb[:], ST[:], r=[ST], w=[STb])
                    P.cp("dve", Ysl[c][:], bank(1)[0:64, :], r=[bk(1)], w=[Ysl[c]])
                for c in range(NCB):
                    Ys = Ysl[c]
                    Yv = v3(Ys[:], 8)
                    P.red(gs[:, 0:8], Yv, ALU.add, r=[Ys], w=[gs])
                    P.ts("dve", gs[:, 0:8], gs[:, 0:8], 1.0 / 64.0, ALU.mult, r=[gs], w=[gs])
                    P.tt("dve", Yv, Yv, b3(gs[:, 0:8], 8, 64), ALU.subtract, r=[Ys, gs], w=[Ys])
                    P.tt("pool", Ysq[:], Ys[:], Ys[:], ALU.mult)
                    P.red(gs[:, 8:16], v3(Ysq[:], 8), ALU.add, r=[Ysq], w=[gs])
                    P.ts("dve", gs[:, 8:16], gs[:, 8:16], 1.0 / 64.0, ALU.mult, 64e-5, ALU.add, r=[gs], w=[gs])
                    P.act(gs[:, 8:16], gs[:, 8:16], AF.Sqrt, r=[gs], w=[gs])
                    P.recip(gs[:, 8:16], gs[:, 8:16], r=[gs], w=[gs])
                    P.tt("dve", Yv, Yv, b3(gs[:, 8:16], 8, 64), ALU.mult, r=[Ys, gs], w=[Ys])
                    for hp in range(4):
                        P.tr(bank(2)[:, hp * 64:(hp + 1) * 64], Ys[:, hp * 128:(hp + 1) * 128], idn[0:64, 0:64],
                             r=[Ys, idn], w=[bk(2)])
                    P.cp("act", yT[:, :, c * 64:(c + 1) * 64], v3(bank(2)[:, 0:256], 4), r=[bk(2)], w=[yT])
                if bstop == 6:
                    return
                ct = catT[g % 2]
                for hp in range(4):
                    P.ts("dve", fo[:], yT[:, hp, :], rwc[:, 20 + hp:21 + hp], ALU.mult, rwc[:, 24 + hp:25 + hp], ALU.add,
                         r=[yT, rwc], w=[fo])
                    P.tt("pool", fo[:], fo[:], bonusT[hp][:], ALU.add)
                    P.tt("dve", ct[:, 4 + hp, :], fo[:], gT[hp][:], ALU.mult, r=[fo, gT[hp]], w=[("ct", 0, 1)])
                P.dma("act", ct[:, 0:4, :], ao_d[:, :, g * GB:(g + 1) * GB], reads=[("ao_d", (g * GB) // 512)],
                      writes=[("ct", 0, 0)])
                for t in range(GB // 128):
                    ti = g * (GB // 128) + t
                    xrr = xr[ti % 2]; mx = mix[ti % 2]
                    for half in range(2):
                        for k in range(8):
                            P.mm(DB[0][:, half * 512:(half + 1) * 512], ct[:, k, t * 128:(t + 1) * 128],
                                 wout[:, k, half * 512:(half + 1) * 512], start=(k == 0), stop=(k == 7),
                                 r=[("ct", 0, 0), ("ct", 0, 1), wout], w=[bk(half)])
                    P.dma("sp", xrr[:], xsrc[ti * 128:(ti + 1) * 128, :], reads=[("xcur", ti)])
                    P.tt("dve", mx[:], DB[0][:], G1bc[:], ALU.mult, r=[bk(0), bk(1), G1bc], w=[mx])
                    P.tt("pool", mx[:], mx[:], xrr[:], ALU.add)
                    P.dma("sp", x1_d[ti * 128:(ti + 1) * 128, :], mx[:], writes=[("x1", ti)])

        def pass_c(l):
            h2T = P.sb("h2T", [128, 8, SG], BF16)
            yacc = P.sb("yacc", [128, NTS, D])
            Gm = P.sb("Gm", [128, NTS, NE])
            wr = P.sb("wr", [128, 8, 36]); brbc = P.sb("brbc", [128, 36])
            wg = [P.sb("wg%d" % i, [128, 8, DE], BF16) for i in range(2)]
            wu = [P.sb("wu%d" % i, [128, 8, DE], BF16) for i in range(2)]
            wd = [P.sb("wd%d" % i, [128, 2, D], BF16) for i in range(2)]
            xt = [P.sb("c_xt%d" % i, [128, D]) for i in range(2)]
            xh2 = [P.sb("c_xh%d" % i, [128, D]) for i in range(2)]
            xh = xh2[0]
            st = [P.sb("c_st%d" % i, [128, 4]) for i in range(2)]
            htmp2 = [P.sb("c_htmp%d" % i, [128, 8, 128]) for i in range(2)]
            h2f = [P.sb("c_h2f%d" % i, [128, 8, 128]) for i in range(2)]
            rt2 = []
            for i in range(2):
                rt2.append(dict(lg=P.sb("c_lg%d" % i, [128, 36]), sm=P.sb("c_sm%d" % i, [128, 16]),
                                ge=P.sb("c_ge%d" % i, [128, 4]), gsel=P.sb("c_gsel%d" % i, [128, 4]),
                                pen=P.sb("c_pen%d" % i, [128, 4]), mm_=P.sb("c_m%d" % i, [128, 32]),
                                m2=P.sb("c_m2%d" % i, [128, 32]), sel1=P.sb("c_sel1%d" % i, [128, 32]),
                                sel2=P.sb("c_sel2%d" % i, [128, 32])))
            sil = [P.sb("c_sil%d" % i, [128, 512]) for i in range(2)]
            hid = [P.sb("c_hid%d" % i, [128, 2, 512], BF16) for i in range(2)]
            ob = [P.sb("c_ob%d" % i, [128, D]) for i in range(2)]
            P.dma("sp", wr[:], wr_in[l].rearrange("(k p) n -> p k n", p=128))
            P.dma("sp", brbc[:], br_in[l].partition_broadcast(128))
            for sg in range(NSG):
                for tl in range(NTS):
                    ti = sg * NTS + tl
                    xtt = xt[ti % 2]; hf = h2f[ti % 2]
                    P.dma("sp", xtt[:], x1_d[ti * 128:(ti + 1) * 128, :], reads=[("x1", ti)])
                    norm_transpose(xtt, xh2[ti % 2], st[ti % 2], A2, B2, htmp2[ti % 2], hf[:], out_eng="dve")
                    _r = rt2[ti % 2]
                    lg = _r["lg"]; sm = _r["sm"]; ge = _r["ge"]; gsel = _r["gsel"]; pen = _r["pen"]
                    mm_ = _r["mm_"]; m2 = _r["m2"]; sel1 = _r["sel1"]; sel2 = _r["sel2"]
                    P.cp("act", h2T[:, :, tl * 128:(tl + 1) * 128], hf[:], w=[("h2T", tl // 4)])
                    for k in range(8):
                        P.mm(bank(2)[:, 0:36], hf[:, k, :], wr[:, k, :], start=(k == 0), stop=(k == 7), w=[bk(2)])
                    P.tt("dve", lg[:], bank(2)[:, 0:36], brbc[:], ALU.add, r=[bk(2), brbc], w=[lg])
                    P.red(sm[:, 0:1], lg[:, 0:4], ALU.max, r=[lg], w=[sm])
                    P.ts("dve", sm[:, 1:2], sm[:, 0:1], -1.0, ALU.mult, r=[sm], w=[sm])
                    P.memset("dve", sm[:, 2:3], 0.0, w=[sm])
                    P.act(ge[:], lg[:, 0:4], AF.Exp, bias=sm[:, 1:2], accum=sm[:, 2:3], r=[lg, sm], w=[ge, sm])
                    P.recip(sm[:, 3:4], sm[:, 2:3], r=[sm], w=[sm])
                    P.ts("dve", gsel[:], lg[:, 0:4], sm[:, 0:1], ALU.is_equal, r=[lg, sm], w=[gsel])
                    P.ts("dve", pen[:], gsel[:], 1e30, ALU.mult, -1e30, ALU.add)
                    P.tt("dve", v3(mm_[:], 4), v3(lg[:, 4:36], 4), b3(pen[:], 4, 8), ALU.add, r=[lg, pen], w=[mm_])
                    P.red(sm[:, 4:5], mm_[:], ALU.max, r=[mm_], w=[sm])
                    P.ts("dve", sel1[:], mm_[:], sm[:, 4:5], ALU.is_equal, r=[mm_, sm], w=[sel1])
                    P.stt("dve", m2[:], sel1[:], -1e30, mm_[:], ALU.mult, ALU.add)
                    P.red(sm[:, 5:6], m2[:], ALU.max, r=[m2], w=[sm])
                    P.ts("dve", sel2[:], m2[:], sm[:, 5:6], ALU.is_equal, r=[m2, sm], w=[sel2])
                    P.tt("dve", sm[:, 6:7], sm[:, 5:6], sm[:, 4:5], ALU.subtract, r=[sm], w=[sm])
                    P.act(sm[:, 7:8], sm[:, 6:7], AF.Exp, r=[sm], w=[sm])
                    P.ts("dve", sm[:, 8:9], sm[:, 7:8], 1.0, ALU.add, r=[sm], w=[sm])
                    P.recip(sm[:, 8:9], sm[:, 8:9], r=[sm], w=[sm])
                    P.tt("dve", sm[:, 9:10], sm[:, 8:9], sm[:, 3:4], ALU.mult, r=[sm], w=[sm])
                    P.tt("dve", sm[:, 10:11], sm[:, 3:4], sm[:, 9:10], ALU.subtract, r=[sm], w=[sm])
                    P.ts("dve", Gm[:, tl, :], sel1[:], sm[:, 9:10], ALU.mult, r=[sel1, sm], w=[("Gm", tl)])
                    P.stt("dve", Gm[:, tl, :], sel2[:], sm[:, 10:11], Gm[:, tl, :], ALU.mult, ALU.add,
                          r=[sel2, sm, ("Gm", tl)], w=[("Gm", tl)])
                blocks = [(e, tb) for e in range(NE) for tb in range(SG // 512)]

                def gu(i):
                    e, tb = blocks[i]
                    wgb = wg[e % 2]; wub = wu[e % 2]; wdb = wd[e % 2]
                    if tb == 0:
                        P.dma("pool", wgb[:], wg_in[l, e].rearrange("(k p) f -> p k f", p=128))
                        P.dma("pool", wub[:], wu_in[l, e].rearrange("(k p) f -> p k f", p=128))
                        P.dma("pool", wdb[:], wd_in[l, e].rearrange("(c p) n -> p c n", p=128))
                    hd = hid[i % 2]
                    for fc in range(2):
                        gb = 2 + fc
                        ub = 4 + fc
                        for k in range(8):
                            P.mm(bank(gb), wgb[:, k, fc * 128:(fc + 1) * 128], h2T[:, k, tb * 512:(tb + 1) * 512],
                                 start=(k == 0), stop=(k == 7), r=[wgb, ("h2T", tb)], w=[bk(gb)])
                        for k in range(8):
                            P.mm(bank(ub), wub[:, k, fc * 128:(fc + 1) * 128], h2T[:, k, tb * 512:(tb + 1) * 512],
                                 start=(k == 0), stop=(k == 7), r=[wub, ("h2T", tb)], w=[bk(ub)])
                        P.act(sil[fc][:], bank(gb), AF.Silu, r=[bk(gb)], w=[sil[fc]])
                        P.tt("dve", hd[:, fc, :], sil[fc][:], bank(ub), ALU.mult, r=[sil[fc], bk(ub)], w=[hd])

                def down(i):
                    e, tb = blocks[i]
                    wdb = wd[e % 2]
                    hd = hid[i % 2]
                    for t in range(4):
                        tl = tb * 4 + t
                        dbi = 0 if t % 2 == 0 else 3
                        for half in range(2):
                            for fc in range(2):
                                P.mm(DB[dbi][:, half * 512:(half + 1) * 512], hd[:, fc, t * 128:(t + 1) * 128],
                                     wdb[:, fc, half * 512:(half + 1) * 512], start=(fc == 0), stop=(fc == 1),
                                     w=[bk(2 * dbi + half)])
                        if e == 0:
                            P.ts("dve", yacc[:, tl, :], DB[dbi][:], Gm[:, tl, e:e + 1], ALU.mult,
                                 r=[bk(2 * dbi), bk(2 * dbi + 1), ("Gm", tl)], w=[("yacc", tl)])
                        else:
                            P.stt("dve", yacc[:, tl, :], DB[dbi][:], Gm[:, tl, e:e + 1], yacc[:, tl, :], ALU.mult,
                                  ALU.add, r=[bk(2 * dbi), bk(2 * dbi + 1), ("Gm", tl), ("yacc", tl)],
                                  w=[("yacc", tl)])

                gu(0)
                for i in range(len(blocks)):
                    if i + 1 < len(blocks):
                        gu(i + 1)
                    down(i)
                for tl in range(NTS):
                    ti = sg * NTS + tl
                    xtt = xt[ti % 2]; o = ob[ti % 2]
                    P.dma("sp", xtt[:], x1_d[ti * 128:(ti + 1) * 128, :], reads=[("x1", ti)])
                    P.tt("dve", o[:], yacc[:, tl, :], G2bc[:], ALU.mult, r=[("yacc", tl), G2bc], w=[o])
                    P.tt("pool", o[:], o[:], xtt[:], ALU.add)
                    if l < L - 1 or dbg:
                        P.dma("sp", xcur[ti * 128:(ti + 1) * 128, :], o[:], writes=[("xcur", ti)])
                    if l == L - 1:
                        stt_ = st[ti % 2]
                        rms_rstd(o, xh, stt_)
                        P.ts("dve", xh[:], o[:], stt_[:, 3:4], ALU.mult, r=[o, stt_], w=[xh])
                        P.tt("pool", o[:], xh[:], fgbc[:], ALU.mult)
                        P.dma("sp", y_out[ti * 128:(ti + 1) * 128, :], o[:], writes=[("y", ti)])

        for l in range(L):
            with ExitStack() as sc:
                P.stack = sc
                modulation(l)
            P.barrier()
            if do_a:
                with ExitStack() as sc:
                    P.stack = sc
                    pass_a(l)
                P.barrier()
            if do_b:
                with ExitStack() as sc:
                    P.stack = sc
                    pass_b(l)
                P.barrier()
            if do_c:
                with ExitStack() as sc:
                    P.stack = sc
                    pass_c(l)
                P.barrier()

        fin = [("y", ti) for ti in range(NT)]
        if dbg:
            if do_a:
                P.dma("sp", dbg_outs["d_ao"], ao_d, reads=[("ao_d", g) for g in range(NG)])
            if do_b:
                P.dma("sp", dbg_outs["d_x1"], x1_d, reads=[("x1", ti) for ti in range(NT)])
            if do_c:
                P.dma("sp", dbg_outs["d_x2"], xcur, reads=[("xcur", ti) for ti in range(NT)])
            fin += list(dbg_outs.values())
        P.finish_wait("sp", fin)
        P.emit()
    return nc, P


def host_layout(inp, b, S=SEQ, L=DEPTH):
    f = np.float32
    m = {}
    m["x"] = np.ascontiguousarray(inp["x"][b, :S], dtype=f)
    m["pos"] = np.ascontiguousarray(inp["positions"][b:b + 1, :S], dtype=np.int32)
    m["c_t"] = np.ascontiguousarray(inp["c"][b].reshape(8, 128).T, dtype=f)
    m["ada_w"] = np.ascontiguousarray(inp["ada_w"][:L], dtype=f)
    m["ada_b"] = np.ascontiguousarray(inp["ada_b"][:L].reshape(L, 1, 6 * D), dtype=f)
    m["g1c"] = np.ascontiguousarray(inp["norm1_g"][:L].reshape(L, 8, 128).transpose(0, 2, 1), dtype=f)
    m["g2c"] = np.ascontiguousarray(inp["norm2_g"][:L].reshape(L, 8, 128).transpose(0, 2, 1), dtype=f)
    m["w_in"] = np.ascontiguousarray(inp["w_in"][:L], dtype=f)
    m["w_out"] = np.ascontiguousarray(inp["w_out"][:L], dtype=f)
    m["lamv"] = np.ascontiguousarray(inp["attn_lambda"][:L].reshape(L, 1, 256), dtype=f)
    m["sublng"] = np.ascontiguousarray(inp["attn_subln_g"][:L].reshape(L, 128, 1), dtype=f)
    mu = inp["rwkv_shift_mu"][:L]
    muc = np.zeros((L, 128, 14), f)
    muc[:, :, 0:12] = mu[:, 0:1536].reshape(L, 12, 128).transpose(0, 2, 1)
    muc[:, 0:64, 12] = mu[:, 1536:1600]
    muc[:, 0:96, 13] = mu[:, 1600:1696]
    m["mu_c"] = muc
    cols = []
    for k in ("rwkv_w0", "rwkv_a0", "rwkv_k_k", "rwkv_k_a", "rwkv_r_k", "rwkv_lnx_g", "rwkv_lnx_b"):
        cols.append(inp[k][:L].reshape(L, 4, 128).transpose(0, 2, 1))
    m["rw_cols"] = np.ascontiguousarray(np.concatenate(cols, axis=2), dtype=f)
    m["lw"] = np.ascontiguousarray(np.concatenate([inp["rwkv_w_up"][:L], inp["rwkv_a_up"][:L]], axis=1), dtype=f)
    m["gu"] = np.ascontiguousarray(inp["rwkv_g_up"][:L], dtype=f)
    m["wr"] = np.ascontiguousarray(np.concatenate([inp["moe_w_group"][:L], inp["moe_w_router"][:L]], axis=2), dtype=f)
    m["br"] = np.ascontiguousarray(np.concatenate([inp["moe_b_group"][:L], inp["moe_b_router"][:L]], axis=1).reshape(L, 1, 36), dtype=f)
    m["wg"] = np.ascontiguousarray(inp["moe_w_gate"][:L], dtype=f)
    m["wu"] = np.ascontiguousarray(inp["moe_w_up"][:L], dtype=f)
    m["wd"] = np.ascontiguousarray(inp["moe_w_down"][:L], dtype=f)
    m["fg"] = np.ascontiguousarray(inp["final_g"].reshape(1, D), dtype=f)
    for k, v in host_consts().items():
        m["k_" + k] = v
    return m


_CACHE = {}


def kernel(**inputs):
    inputs = {k: np.asarray(v) for k, v in inputs.items()}
    if "prog" not in _CACHE:
        _CACHE["prog"] = build_program()[0]
    nc = _CACHE["prog"]
    in_maps = [host_layout(inputs, b) for b in range(NCORES)]
    res = run_bass_kernel_spmd(nc, in_maps, core_ids=list(range(NCORES)))
    out = np.stack([np.asarray(res.results[b]["y"], dtype=np.float32) for b in range(NCORES)], axis=0)
    return out
```
